# Optimizing a Trainium2 kernel written in Bass

```python
import math
import jax
import jax.numpy as jnp
from jax import lax
import numpy as np

D_MODEL = 1024
BATCH = 4
SEQ = 4096
DEPTH = 2

CTX_LEN = 256
GRID_W = 64

LRU_WIDTH = 384
LRU_BLOCKS = 6
LRU_BLOCK = LRU_WIDTH // LRU_BLOCKS
LRU_CONV = 4
LRU_C = 8.0
HY_WIDTH = 256
HY_ORDER = 2
HY_SHORT = 3
HY_BANDS = 16
HY_EMB = 2 * HY_BANDS + 1
HY_FILT_HID = 64
HY_MAX_DECAY = math.log(1e-2) / 0.3
HY_MIN_DECAY = math.log(1e-2) / 1.5
NA_HEADS = 6
NA_HEAD_DIM = 64
NA_WIDTH = NA_HEADS * NA_HEAD_DIM
NA_WIN_R = 8
NA_WIN_C = 16
S5_WIDTH = 256
S5_GROUP = 16
S5_GROUPS = S5_WIDTH // S5_GROUP
S5_STATE = 64
N_BRANCH = 4
OFF_A = 0
OFF_B = OFF_A + 2 * LRU_WIDTH
OFF_C = OFF_B + 3 * HY_WIDTH
OFF_D = OFF_C + 3 * NA_WIDTH
OFF_G = OFF_D + S5_WIDTH
IN_COLS = OFF_G + N_BRANCH * D_MODEL
FF_DENSE = 2816
N_EXPERTS = 8
TOP_K = 2
FF_EXPERT = 3584
LN_EPS = 1e-5
F32 = jnp.float32

kernel_name = 'hybrid_gated_mixers_diffusion_block'


def layer_norm(x, g=None, b=None):
    xf = x.astype(F32)
    mu = jnp.mean(xf, axis=-1, keepdims=True)
    var = jnp.mean(jnp.square(xf - mu), axis=-1, keepdims=True)
    y = (xf - mu) * lax.rsqrt(var + LN_EPS)
    if g is not None:
        y = y * g.astype(F32) + b.astype(F32)
    return y.astype(x.dtype)


def modulate(x, shift, scale):
    return layer_norm(x) * (1.0 + scale) + shift


def dw_conv(x, w, b):
    k_w = w.shape[0]
    left = k_w // 2
    L = x.shape[1]
    xp = jnp.pad(x, ((0, 0), (left, k_w - 1 - left), (0, 0)))
    y = xp[:, 0:L] * w[0]
    for k in range(1, k_w):
        y = y + xp[:, k:k + L] * w[k]
    return y + b


def linear_scan(a, b, h0, reverse):
    def combine(e1, e2):
        a1, b1 = e1
        a2, b2 = e2
        return a1 * a2, a2 * b1 + b2
    a_cum, b_cum = lax.associative_scan(combine, (a, b), reverse=reverse, axis=0)
    return b_cum + a_cum * h0


def complex_scan(a_re, a_im, b_re, b_im, h0_re, h0_im, reverse):
    def combine(e1, e2):
        ar1, ai1, br1, bi1 = e1
        ar2, ai2, br2, bi2 = e2
        return (ar1 * ar2 - ai1 * ai2, ar1 * ai2 + ai1 * ar2,
                ar2 * br1 - ai2 * bi1 + br2, ar2 * bi1 + ai2 * br1 + bi2)
    ar, ai, br, bi = lax.associative_scan(combine, (a_re, a_im, b_re, b_im), reverse=reverse, axis=0)
    return br + ar * h0_re - ai * h0_im, bi + ar * h0_im + ai * h0_re


def rglru_scan(xc, w_r, b_r, w_i, b_i, lam, h0, reverse):
    bsz, L, _ = xc.shape
    xb = xc.reshape(bsz, L, LRU_BLOCKS, LRU_BLOCK)
    gate_r = jax.nn.sigmoid(jnp.einsum('blgi,gij->blgj', xb, w_r.astype(F32)).reshape(bsz, L, LRU_WIDTH) + b_r)
    gate_i = jax.nn.sigmoid(jnp.einsum('blgi,gij->blgj', xb, w_i.astype(F32)).reshape(bsz, L, LRU_WIDTH) + b_i)
    log_a = -LRU_C * gate_r * jax.nn.softplus(-lam.astype(F32))
    a = jnp.exp(log_a)
    b = jnp.sqrt(-jnp.expm1(2.0 * log_a)) * gate_i * xc
    h = linear_scan(jnp.swapaxes(a, 0, 1), jnp.swapaxes(b, 0, 1), h0, reverse)
    return jnp.swapaxes(h, 0, 1)


def rglru_mixer(pa_c, pa_l, lp, with_ctx):
    def conv_in(pa):
        return dw_conv(pa[..., LRU_WIDTH:], lp['lru_conv_w'], lp['lru_conv_b']).astype(F32)

    def run(xc, d, h0, reverse):
        return rglru_scan(xc, lp['lru_w_r'][d], lp['lru_b_r'][d], lp['lru_w_i'][d], lp['lru_b_i'][d],
                          lp['lru_lambda'][d], h0, reverse)

    xc_c, xc_l = conv_in(pa_c), conv_in(pa_l)
    hc_f = run(xc_c, 0, 0.0, False)
    hc_b = run(xc_c, 1, 0.0, True)
    hl_f = run(xc_l, 0, hc_f[:, -1], False)
    hl_b = run(xc_l, 1, hc_b[:, 0], True)
    y_l = (jax.nn.gelu(pa_l[..., :LRU_WIDTH].astype(F32)) * (hl_f + hl_b)).astype(pa_l.dtype)
    y_c = None
    if with_ctx:
        y_c = (jax.nn.gelu(pa_c[..., :LRU_WIDTH].astype(F32)) * (hc_f + hc_b)).astype(pa_c.dtype)
    return y_c, y_l


def hyena_filters(L, w1, b1, w2, b2, w3, b3, freq):
    t = jnp.arange(L, dtype=F32)
    t_norm = t / L
    bands = jnp.arange(1, HY_BANDS + 1, dtype=F32)
    ang = (2.0 * math.pi / L) * t[:, None] * bands[None, :]
    feat = jnp.concatenate([t_norm[:, None], jnp.cos(ang), jnp.sin(ang)], axis=-1)
    h = jnp.sin(freq * (feat @ w1 + b1))
    h = jnp.sin(freq * (h @ w2 + b2))
    h = (h @ w3 + b3).reshape(L, 2, HY_ORDER, HY_WIDTH)
    deltas = jnp.abs(jnp.linspace(HY_MIN_DECAY, HY_MAX_DECAY, HY_WIDTH, dtype=F32))
    window = jnp.exp(-t_norm[:, None] * deltas[None, :])
    return h * window[:, None, None, :]


def bidir_kernel(h_f, h_b):
    k = jnp.concatenate([h_f, jnp.zeros_like(h_f[:1]), h_b[:0:-1]], axis=0)
    return k / (jnp.sum(jnp.abs(k), axis=0, keepdims=True) + 1e-6)


def long_conv(z, k, bias):
    L = z.shape[1]
    kf = jnp.fft.rfft(k, axis=0)
    zf = jnp.fft.rfft(z, n=2 * L, axis=1)
    y = jnp.fft.irfft(zf * kf[None], n=2 * L, axis=1)[:, :L]
    return y + z * bias


def hyena_sequence(pb, lp):
    L = pb.shape[1]
    z = dw_conv(pb, lp['hy_conv_w'], lp['hy_conv_b']).astype(F32)
    v, x1, x2 = jnp.split(z, 3, axis=-1)
    h = hyena_filters(L, lp['hy_w1'], lp['hy_b1'], lp['hy_w2'], lp['hy_b2'], lp['hy_w3'], lp['hy_b3'], lp['hy_freq'])
    y = v
    for o, gate in enumerate((x1, x2)):
        y = gate * long_conv(y, bidir_kernel(h[:, 0, o], h[:, 1, o]), lp['hy_bias'][o])
    return y.astype(pb.dtype)


def natten_mixer(pc_c, pc_l, rpb, with_ctx):
    bsz, L, _ = pc_l.shape
    rows = L // GRID_W
    kr = min(NA_WIN_R, rows)
    nw = kr * NA_WIN_C
    scale = NA_HEAD_DIM ** -0.5

    def split_heads(p):
        return [p[..., i * NA_WIDTH:(i + 1) * NA_WIDTH].reshape(p.shape[0], p.shape[1], NA_HEADS, NA_HEAD_DIM)
                for i in range(3)]

    q_l, k_l, v_l = split_heads(pc_l)
    q_c, k_c, v_c = split_heads(pc_c)
    kg = k_l.reshape(bsz, rows, GRID_W, NA_HEADS, NA_HEAD_DIM)
    vg = v_l.reshape(bsz, rows, GRID_W, NA_HEADS, NA_HEAD_DIM)
    qg = jnp.moveaxis((q_l * scale).reshape(bsz, rows, GRID_W, NA_HEADS, NA_HEAD_DIM), 1, 0)
    row_ids = jnp.arange(rows)
    row_start = jnp.clip(row_ids - kr // 2, 0, rows - kr)
    cols = jnp.arange(GRID_W)
    col_idx = jnp.clip(cols - NA_WIN_C // 2, 0, GRID_W - NA_WIN_C)[:, None] + jnp.arange(NA_WIN_C)[None, :]
    dc_idx = col_idx - cols[:, None] + (NA_WIN_C - 1)

    def row_block(args):
        q_r, r, rs = args
        k_win = lax.dynamic_slice_in_dim(kg, rs, kr, axis=1)[:, :, col_idx]
        v_win = lax.dynamic_slice_in_dim(vg, rs, kr, axis=1)[:, :, col_idx]
        dr_idx = rs + jnp.arange(kr) - r + (NA_WIN_R - 1)
        bias = rpb[:, dr_idx[:, None, None], dc_idx[None, :, :]]
        s_win = jnp.einsum('bwhd,biwjhd->bhwij', q_r, k_win) + jnp.transpose(bias, (0, 2, 1, 3))[None]
        s_ctx = jnp.einsum('bwhd,bnhd->bhwn', q_r, k_c)
        s = jnp.concatenate([s_win.reshape(bsz, NA_HEADS, GRID_W, nw), s_ctx], axis=-1)
        pr = jax.nn.softmax(s.astype(F32), axis=-1).astype(v_l.dtype)
        p_win = pr[..., :nw].reshape(bsz, NA_HEADS, GRID_W, kr, NA_WIN_C)
        return (jnp.einsum('bhwij,biwjhd->bwhd', p_win, v_win)
                + jnp.einsum('bhwn,bnhd->bwhd', pr[..., nw:], v_c))

    o_l = lax.map(row_block, (qg, row_ids, row_start))
    y_l = jnp.moveaxis(o_l, 0, 1).reshape(bsz, L, NA_WIDTH)
    y_c = None
    if with_ctx:
        s = jnp.einsum('bqhd,bkhd->bhqk', q_c * scale, k_c)
        pr = jax.nn.softmax(s.astype(F32), axis=-1).astype(v_c.dtype)
        y_c = jnp.einsum('bhqk,bkhd->bqhd', pr, v_c).reshape(bsz, q_c.shape[1], NA_WIDTH)
    return y_c, y_l


def s5_discretise(a_re, a_im, log_dt, b_re, b_im):
    a_re, a_im = a_re.astype(F32), a_im.astype(F32)
    dt = jnp.exp(log_dt.astype(F32))[:, None]
    mag = jnp.exp(dt * a_re)
    ab_re = mag * jnp.cos(dt * a_im)
    ab_im = mag * jnp.sin(dt * a_im)
    den = jnp.square(a_re) + jnp.square(a_im)
    f_re = ((ab_re - 1.0) * a_re + ab_im * a_im) / den
    f_im = (ab_im * a_re - (ab_re - 1.0) * a_im) / den
    b_re, b_im = b_re.astype(F32), b_im.astype(F32)
    bb_re = f_re[..., None] * b_re - f_im[..., None] * b_im
    bb_im = f_re[..., None] * b_im + f_im[..., None] * b_re
    return ab_re, ab_im, bb_re, bb_im


def s5_states(u, disc, h0_re, h0_im, reverse):
    ab_re, ab_im, bb_re, bb_im = disc
    L = u.shape[1]
    bu_re = jnp.einsum('blgi,gpi->lbgp', u, bb_re)
    bu_im = jnp.einsum('blgi,gpi->lbgp', u, bb_im)
    a_re = jnp.broadcast_to(ab_re[None, None], (L, 1) + ab_re.shape)
    a_im = jnp.broadcast_to(ab_im[None, None], (L, 1) + ab_im.shape)
    return complex_scan(a_re, a_im, bu_re, bu_im, h0_re, h0_im, reverse)


def s5_readout(h_re, h_im, c_re, c_im):
    return (jnp.einsum('lbgp,gop->blgo', h_re, c_re.astype(F32))
            - jnp.einsum('lbgp,gop->blgo', h_im, c_im.astype(F32)))


def s5_mixer(pd_c, pd_l, lp, with_ctx):
    def groups(pd):
        return pd.astype(F32).reshape(pd.shape[0], pd.shape[1], S5_GROUPS, S5_GROUP)

    discs = [s5_discretise(lp['s5_a_re'][d], lp['s5_a_im'][d], lp['s5_log_dt'][d],
                           lp['s5_b_re'][d], lp['s5_b_im'][d]) for d in range(2)]
    u_c, u_l = groups(pd_c), groups(pd_l)
    hc_f = s5_states(u_c, discs[0], 0.0, 0.0, False)
    hc_b = s5_states(u_c, discs[1], 0.0, 0.0, True)
    hl_f = s5_states(u_l, discs[0], hc_f[0][-1], hc_f[1][-1], False)
    hl_b = s5_states(u_l, discs[1], hc_b[0][0], hc_b[1][0], True)

    def out(h_f, h_b, pd):
        y = (s5_readout(h_f[0], h_f[1], lp['s5_c_re'][0], lp['s5_c_im'][0])
             + s5_readout(h_b[0], h_b[1], lp['s5_c_re'][1], lp['s5_c_im'][1]))
        y = y.reshape(pd.shape) + lp['s5_d'] * pd.astype(F32)
        g = jax.nn.gelu(y)
        return (g * jax.nn.sigmoid(g @ lp['s5_w_glu'].astype(F32) + lp['s5_b_glu'])).astype(pd.dtype)

    y_l = out(hl_f, hl_b, pd_l)
    y_c = out(hc_f, hc_b, pd_c) if with_ctx else None
    return y_c, y_l


def merge_branches(pg, ys, lp):
    gates = jax.nn.sigmoid(pg.astype(F32)).reshape(pg.shape[:-1] + (N_BRANCH, D_MODEL)).astype(pg.dtype)
    projs = (lp['w_br_a'], lp['w_br_b'], lp['w_br_c'], lp['w_br_d'])
    merged = gates[..., 0, :] * (ys[0] @ projs[0])
    for i in range(1, N_BRANCH):
        merged = merged + gates[..., i, :] * (ys[i] @ projs[i])
    return merged @ lp['w_out']


def token_mixer(u_c, u_l, lp, with_ctx):
    p_l = u_l @ lp['w_in']
    p_c = u_c @ (lp['w_in'] if with_ctx else lp['w_in'][:, :OFF_G])
    ya_c, ya_l = rglru_mixer(p_c[..., OFF_A:OFF_B], p_l[..., OFF_A:OFF_B], lp, with_ctx)
    yb_l = hyena_sequence(p_l[..., OFF_B:OFF_C], lp)
    yc_c, yc_l = natten_mixer(p_c[..., OFF_C:OFF_D], p_l[..., OFF_C:OFF_D], lp['na_rpb'], with_ctx)
    yd_c, yd_l = s5_mixer(p_c[..., OFF_D:OFF_G], p_l[..., OFF_D:OFF_G], lp, with_ctx)
    m_l = merge_branches(p_l[..., OFF_G:], (ya_l, yb_l, yc_l, yd_l), lp)
    if not with_ctx:
        return None, m_l
    yb_c = hyena_sequence(p_c[..., OFF_B:OFF_C], lp)
    m_c = merge_branches(p_c[..., OFF_G:], (ya_c, yb_c, yc_c, yd_c), lp)
    return m_c, m_l


def swiglu(u, w_gate, w_up, w_down):
    return (jax.nn.silu(u @ w_gate) * (u @ w_up)) @ w_down


def moe_swiglu(u, w_router, w_gate, w_up, w_down):
    logits = (u @ w_router).astype(F32)
    top_v, top_i = lax.top_k(logits, TOP_K)
    weights = jax.nn.softmax(top_v, axis=-1)
    gate = jnp.sum(jax.nn.one_hot(top_i, N_EXPERTS, dtype=F32) * weights[..., None], axis=-2).astype(u.dtype)
    out = gate[..., 0:1] * swiglu(u, w_gate[0], w_up[0], w_down[0])
    for e in range(1, N_EXPERTS):
        out = out + gate[..., e:e + 1] * swiglu(u, w_gate[e], w_up[e], w_down[e])
    return out


def setup_inputs(seed: int = 0) -> dict:
    key = jax.random.key(seed)
    keys = iter(jax.random.split(key, 64))

    def nrm(shape, std):
        return std * jax.random.normal(next(keys), shape, jnp.float32)

    D = D_MODEL
    beta = (8.0 * DEPTH) ** -0.25
    n_dense = (DEPTH + 1) // 2
    n_moe = DEPTH // 2
    a_target = jax.random.uniform(next(keys), (DEPTH, 2, LRU_WIDTH), jnp.float32, 0.9, 0.999)
    s_root = a_target ** (1.0 / LRU_C)
    state_n = jnp.arange(S5_STATE, dtype=jnp.float32)
    return {
        'x': nrm((BATCH, SEQ, D), 1.0),
        'c': nrm((BATCH, D), 1.0),
        'ctx': nrm((BATCH, CTX_LEN, D), 1.0),
        'c_ctx': nrm((D,), 1.0),
        'w_mod': nrm((DEPTH, D, 6 * D), 0.5 * D ** -0.5),
        'b_mod': nrm((DEPTH, 6 * D), 0.01),
        'w_in': nrm((DEPTH, D, IN_COLS), D ** -0.5),
        'lru_conv_w': nrm((DEPTH, LRU_CONV, LRU_WIDTH), LRU_CONV ** -0.5),
        'lru_conv_b': nrm((DEPTH, LRU_WIDTH), 0.01),
        'lru_w_r': nrm((DEPTH, 2, LRU_BLOCKS, LRU_BLOCK, LRU_BLOCK), LRU_BLOCK ** -0.5),
        'lru_b_r': nrm((DEPTH, 2, LRU_WIDTH), 0.01),
        'lru_w_i': nrm((DEPTH, 2, LRU_BLOCKS, LRU_BLOCK, LRU_BLOCK), LRU_BLOCK ** -0.5),
        'lru_b_i': nrm((DEPTH, 2, LRU_WIDTH), 0.01),
        'lru_lambda': jnp.log(s_root) - jnp.log1p(-s_root),
        'hy_conv_w': nrm((DEPTH, HY_SHORT, 3 * HY_WIDTH), HY_SHORT ** -0.5),
        'hy_conv_b': nrm((DEPTH, 3 * HY_WIDTH), 0.01),
        'hy_w1': nrm((DEPTH, HY_EMB, HY_FILT_HID), HY_EMB ** -0.5),
        'hy_b1': nrm((DEPTH, HY_FILT_HID), 0.1),
        'hy_w2': nrm((DEPTH, HY_FILT_HID, HY_FILT_HID), HY_FILT_HID ** -0.5),
        'hy_b2': nrm((DEPTH, HY_FILT_HID), 0.1),
        'hy_w3': nrm((DEPTH, HY_FILT_HID, 2 * HY_ORDER * HY_WIDTH), HY_FILT_HID ** -0.5),
        'hy_b3': nrm((DEPTH, 2 * HY_ORDER * HY_WIDTH), 0.01),
        'hy_freq': 1.0 + nrm((DEPTH, HY_FILT_HID), 0.01),
        'hy_bias': nrm((DEPTH, HY_ORDER, HY_WIDTH), 0.5),
        'na_rpb': nrm((DEPTH, NA_HEADS, 2 * NA_WIN_R - 1, 2 * NA_WIN_C - 1), 0.1),
        's5_a_re': -0.5 + nrm((DEPTH, 2, S5_GROUPS, S5_STATE), 0.01),
        's5_a_im': math.pi * state_n + nrm((DEPTH, 2, S5_GROUPS, S5_STATE), 0.01),
        's5_log_dt': jax.random.uniform(next(keys), (DEPTH, 2, S5_GROUPS), jnp.float32, math.log(1e-3), math.log(1e-1)),
        's5_b_re': nrm((DEPTH, 2, S5_GROUPS, S5_STATE, S5_GROUP), (2 * S5_GROUP) ** -0.5),
        's5_b_im': nrm((DEPTH, 2, S5_GROUPS, S5_STATE, S5_GROUP), (2 * S5_GROUP) ** -0.5),
        's5_c_re': nrm((DEPTH, 2, S5_GROUPS, S5_GROUP, S5_STATE), (2 * S5_STATE) ** -0.5),
        's5_c_im': nrm((DEPTH, 2, S5_GROUPS, S5_GROUP, S5_STATE), (2 * S5_STATE) ** -0.5),
        's5_d': nrm((DEPTH, S5_WIDTH), 1.0),
        's5_w_glu': nrm((DEPTH, S5_WIDTH, S5_WIDTH), S5_WIDTH ** -0.5),
        's5_b_glu': nrm((DEPTH, S5_WIDTH), 0.01),
        'w_br_a': nrm((DEPTH, LRU_WIDTH, D), beta * LRU_WIDTH ** -0.5),
        'w_br_b': nrm((DEPTH, HY_WIDTH, D), beta * HY_WIDTH ** -0.5),
        'w_br_c': nrm((DEPTH, NA_WIDTH, D), beta * NA_WIDTH ** -0.5),
        'w_br_d': nrm((DEPTH, S5_WIDTH, D), beta * S5_WIDTH ** -0.5),
        'w_out': nrm((DEPTH, D, D), beta * D ** -0.5),
        'ln1_g': 1.0 + nrm((DEPTH, D), 0.01),
        'ln1_b': nrm((DEPTH, D), 0.01),
        'ln2_g': 1.0 + nrm((DEPTH, D), 0.01),
        'ln2_b': nrm((DEPTH, D), 0.01),
        'ff_w_gate': nrm((n_dense, D, FF_DENSE), D ** -0.5),
        'ff_w_up': nrm((n_dense, D, FF_DENSE), D ** -0.5),
        'ff_w_down': nrm((n_dense, FF_DENSE, D), beta * FF_DENSE ** -0.5),
        'moe_router': nrm((n_moe, D, N_EXPERTS), D ** -0.5),
        'moe_w_gate': nrm((n_moe, N_EXPERTS, D, FF_EXPERT), D ** -0.5),
        'moe_w_up': nrm((n_moe, N_EXPERTS, D, FF_EXPERT), D ** -0.5),
        'moe_w_down': nrm((n_moe, N_EXPERTS, FF_EXPERT, D), beta * FF_EXPERT ** -0.5),
    }


def reference(x, c, ctx, c_ctx, w_mod, b_mod, w_in, lru_conv_w, lru_conv_b, lru_w_r, lru_b_r, lru_w_i, lru_b_i,
              lru_lambda, hy_conv_w, hy_conv_b, hy_w1, hy_b1, hy_w2, hy_b2, hy_w3, hy_b3, hy_freq, hy_bias, na_rpb,
              s5_a_re, s5_a_im, s5_log_dt, s5_b_re, s5_b_im, s5_c_re, s5_c_im, s5_d, s5_w_glu, s5_b_glu,
              w_br_a, w_br_b, w_br_c, w_br_d, w_out, ln1_g, ln1_b, ln2_g, ln2_b,
              ff_w_gate, ff_w_up, ff_w_down, moe_router, moe_w_gate, moe_w_up, moe_w_down):
    alpha = (2.0 * DEPTH) ** 0.25
    for l in range(DEPTH):
        with_ctx = l < DEPTH - 1
        lp = {
            'w_in': w_in[l], 'lru_conv_w': lru_conv_w[l], 'lru_conv_b': lru_conv_b[l],
            'lru_w_r': lru_w_r[l], 'lru_b_r': lru_b_r[l], 'lru_w_i': lru_w_i[l], 'lru_b_i': lru_b_i[l],
            'lru_lambda': lru_lambda[l], 'hy_conv_w': hy_conv_w[l], 'hy_conv_b': hy_conv_b[l],
            'hy_w1': hy_w1[l], 'hy_b1': hy_b1[l], 'hy_w2': hy_w2[l], 'hy_b2': hy_b2[l], 'hy_w3': hy_w3[l],
            'hy_b3': hy_b3[l], 'hy_freq': hy_freq[l], 'hy_bias': hy_bias[l], 'na_rpb': na_rpb[l],
            's5_a_re': s5_a_re[l], 's5_a_im': s5_a_im[l], 's5_log_dt': s5_log_dt[l], 's5_b_re': s5_b_re[l],
            's5_b_im': s5_b_im[l], 's5_c_re': s5_c_re[l], 's5_c_im': s5_c_im[l], 's5_d': s5_d[l],
            's5_w_glu': s5_w_glu[l], 's5_b_glu': s5_b_glu[l], 'w_br_a': w_br_a[l], 'w_br_b': w_br_b[l],
            'w_br_c': w_br_c[l], 'w_br_d': w_br_d[l], 'w_out': w_out[l],
        }
        mod_l = (jax.nn.silu(c) @ w_mod[l] + b_mod[l])[:, None, :]
        mod_c = jax.nn.silu(c_ctx) @ w_mod[l] + b_mod[l]
        sh1, sc1, g1, sh2, sc2, g2 = jnp.split(mod_l, 6, axis=-1)
        csh1, csc1, cg1, csh2, csc2, cg2 = jnp.split(mod_c, 6, axis=-1)
        m_c, m_l = token_mixer(modulate(ctx, csh1, csc1), modulate(x, sh1, sc1), lp, with_ctx)
        x = layer_norm(alpha * x + g1 * m_l, ln1_g[l], ln1_b[l])
        if with_ctx:
            ctx = layer_norm(alpha * ctx + cg1 * m_c, ln1_g[l], ln1_b[l])

        def ffn(u):
            if l % 2 == 0:
                return swiglu(u, ff_w_gate[l // 2], ff_w_up[l // 2], ff_w_down[l // 2])
            return moe_swiglu(u, moe_router[l // 2], moe_w_gate[l // 2], moe_w_up[l // 2], moe_w_down[l // 2])

        x = layer_norm(alpha * x + g2 * ffn(modulate(x, sh2, sc2)), ln2_g[l], ln2_b[l])
        if with_ctx:
            ctx = layer_norm(alpha * ctx + cg2 * ffn(modulate(ctx, csh2, csc2)), ln2_g[l], ln2_b[l])
    return x
```

```python
import math
import os as _os
from contextlib import ExitStack

import numpy as np
import ml_dtypes

import concourse.bass as bass
import concourse.mybir as mybir
from concourse.bass_utils import run_bass_kernel_spmd

F32 = mybir.dt.float32
BF16 = mybir.dt.bfloat16
ALU = mybir.AluOpType
AF = mybir.ActivationFunctionType

D = 1024
NB = 4
SEQ = 4096
CTXL = 256
T = SEQ + CTXL
NT = T // 128
LRU_W = 384
HY_W = 256
NA_W = 384
S5_W = 256
OFF_A = 0
OFF_B = 768
OFF_C = 1536
OFF_D = 2688
OFF_G = 2944
FF_DENSE = 2816
FF_EXP = 3584
NEXP = 8
ALPHA = 4.0 ** 0.25
EPS = 1e-5
MAGIC = 12582912.0
TWO_PI = 2.0 * math.pi

ENGS = ["pe", "act", "dve", "pool", "sp"]
N_DMA_SEMS = 32
SBUF_RESERVE = 16384
N_HW_SEMS = 24


class Sched:
    def __init__(self, nc, es):
        self.nc = nc
        self.ops = {e: [] for e in ENGS}
        self.cnt = {e: 0 for e in ENGS}
        self.res = {}
        self.esem = {e: es.enter_context(nc.semaphore("s_" + e)) for e in ENGS}
        self.dsem = [es.enter_context(nc.semaphore("d_%d" % i)) for i in range(N_DMA_SEMS)]
        self.dval = [0] * N_DMA_SEMS
        self.dnext = 0
        self.gnext = 0
        self.waited = {e: {} for e in ENGS}
        self.final_tokens = []
        self.pending = {e: [] for e in ENGS}

    def barrier(self):
        toks = [("e", e, self.cnt[e]) for e in ENGS if self.cnt[e] > 0]
        toks += [("d", k, self.dval[k]) for k in range(N_DMA_SEMS) if self.dval[k] > 0]
        toks += [("c", k, 16) for k in range(len(getattr(self, "csem", [])))]
        for e in ENGS:
            self.pending[e].extend(toks)

    def _sem(self, tok):
        if tok[0] == "c":
            return self.csem[tok[1]]
        return self.esem[tok[1]] if tok[0] == "e" else self.dsem[tok[1]]

    def coll(self, es, fn, reads=(), writes=()):
        if not hasattr(self, "csem"):
            self.csem = []
        deps = self._deps(reads, writes) + self.pending["pool"]
        self.pending["pool"] = []
        waits = self._waits_for("pool", deps)
        self.csem.append(es.enter_context(self.nc.semaphore("c_%d" % len(self.csem))))
        tok = ("c", len(self.csem) - 1, 16)
        self.ops["pool"].append((fn, waits, tok))
        self._commit(tok, reads, writes)
        return tok

    def _deps(self, reads, writes):
        deps = []
        for r in reads:
            st = self.res.get(r)
            if st and st["w"] is not None:
                deps.append(st["w"])
        for w in writes:
            st = self.res.get(w)
            if st:
                if st["w"] is not None:
                    deps.append(st["w"])
                deps.extend(st["r"])
        return deps

    def _commit(self, tok, reads, writes):
        for r in reads:
            st = self.res.setdefault(r, {"w": None, "r": []})
            st["r"].append(tok)
        for w in writes:
            self.res[w] = {"w": tok, "r": []}

    def _waits_for(self, eng, deps):
        need = {}
        for t in deps:
            if t[0] == "e" and t[1] == eng and eng == "pe":
                continue
            k = (t[0], t[1])
            if need.get(k, -1) < t[2]:
                need[k] = t[2]
        out = []
        for k, v in need.items():
            if self.waited[eng].get(k, -1) >= v:
                continue
            self.waited[eng][k] = v
            out.append((k[0], k[1], v))
        return out

    def op(self, eng, fn, reads=(), writes=()):
        deps = self._deps(reads, writes) + self.pending[eng]
        self.pending[eng] = []
        waits = self._waits_for(eng, deps)
        self.cnt[eng] += 1
        tok = ("e", eng, self.cnt[eng])
        self.ops[eng].append((fn, waits, tok))
        self._commit(tok, reads, writes)
        return tok

    def dma(self, eng, fn, reads=(), writes=(), final=False):
        deps = self._deps(reads, writes) + self.pending[eng]
        self.pending[eng] = []
        if eng == "pool":
            k = N_HW_SEMS + self.gnext
            self.gnext = (self.gnext + 1) % (N_DMA_SEMS - N_HW_SEMS)
        else:
            k = self.dnext
            self.dnext = (self.dnext + 1) % N_HW_SEMS
        if self.dval[k] > 0:
            deps.append(("d", k, self.dval[k]))
        waits = self._waits_for(eng, deps)
        self.dval[k] += 16
        tok = ("d", k, self.dval[k])
        self.ops[eng].append((fn, waits, tok))
        self._commit(tok, reads, writes)
        if final:
            self.final_tokens.append(tok)
        return tok

    def emit(self):
        nc = self.nc
        fin_waits = self._waits_for("sp", self.final_tokens)
        engmap = {"pe": "tensor", "act": "scalar", "dve": "vector", "pool": "gpsimd", "sp": "sync"}
        with nc.Block() as block:
            for e in ENGS:
                ops = self.ops[e]
                extra = fin_waits if e == "sp" else []
                if not ops and not extra:
                    continue

                def body(engine, ops=ops, extra=extra):
                    for fn, waits, tok in ops:
                        for w in waits:
                            engine.wait_ge(self._sem(w), w[2])
                        ins = fn(engine)
                        ins.then_inc(self._sem(tok), 1 if tok[0] == "e" else 16)
                    for w in extra:
                        engine.wait_ge(self._sem(w), w[2])

                getattr(block, engmap[e])(body)


class KB:
    def __init__(self, name="k"):
        self.nc = bass.Bass("TRN2", target_bir_lowering=False)
        self.es = ExitStack()
        self.S = Sched(self.nc, self.es)
        self.rr = 0
        self.in_names = []
        self.out_names = []

    def din(self, name, shape, dt=F32):
        name = self.__dict__.get("pfx", "") + name
        cache = self.__dict__.setdefault("_dins", {})
        if name in cache:
            return cache[name]
        self.in_names.append(name)
        cache[name] = self.nc.dram_tensor(name, list(shape), dt, kind="ExternalInput").ap()
        return cache[name]

    def dout(self, name, shape, dt=F32):
        name = self.__dict__.get("pfx", "") + name
        self.out_names.append(name)
        return self.nc.dram_tensor(name, list(shape), dt, kind="ExternalOutput").ap()

    def dscratch(self, name, shape, dt=F32):
        name = self.__dict__.get("pfx", "") + name
        return self.nc.dram_tensor(name, list(shape), dt).ap()

    def sb(self, name, shape, dt=F32):
        used = self.__dict__.setdefault("_used", {})
        n = used.get(name, 0)
        used[name] = n + 1
        if n:
            name = "%s__%d" % (name, n)
        t_ = self.es.enter_context(self.nc.sbuf_tensor(name, list(shape), dt))
        rem = self.nc.sbuf_bytes_remaining
        self.min_rem = min(self.__dict__.get("min_rem", 1 << 30), rem)
        assert rem >= SBUF_RESERVE, "SBUF budget exceeded at %s: remaining %d" % (name, rem)
        return t_

    def scope(self):
        kb = self

        class _Scope:
            def __enter__(self_):
                self_.old = kb.es
                kb.es = ExitStack()
                return self_

            def __exit__(self_, *a):
                kb.S.barrier()
                kb.es.close()
                kb.es = self_.old
                return False

        return _Scope()

    def ps(self, name, shape=(128, 512), dt=F32):
        used = self.__dict__.setdefault("_usedp", {})
        n = used.get(name, 0)
        used[name] = n + 1
        if n:
            name = "%s__%d" % (name, n)
        return self.es.enter_context(self.nc.psum_tensor(name, list(shape), dt))

    def dma(self, out, in_, r=(), w=(), eng=None, final=False):
        if eng is None:
            eng = ("sp", "act")[self.rr % 2]
            self.rr += 1
        return self.S.dma(eng, lambda e: e.dma_start(out=out, in_=in_), reads=r, writes=w, final=final)

    def dma_cast(self, out, in_, r=(), w=()):
        return self.S.dma("pool", lambda e: e.dma_start(out=out, in_=in_), reads=r, writes=w)

    def mm(self, out, lhsT, rhs, start, stop, r=(), w=()):
        return self.S.op("pe", lambda e: e.matmul(out, lhsT=lhsT, rhs=rhs, start=start, stop=stop), reads=r, writes=w)

    def tr(self, out, in_, ident, r=(), w=()):
        return self.S.op("pe", lambda e: e.transpose(out=out, in_=in_, identity=ident), reads=r, writes=w)

    def act(self, out, in_, func, r=(), w=(), scale=1.0, bias=0.0, eng="act"):
        return self.S.op(eng, lambda e: e.activation(out=out, in_=in_, func=func, bias=bias, scale=scale), reads=r, writes=w)

    def tt(self, eng, out, in0, in1, op, r=(), w=()):
        return self.S.op(eng, lambda e: e.tensor_tensor(out=out, in0=in0, in1=in1, op=op), reads=r, writes=w)

    def ts(self, eng, out, in0, s1, op0, s2=None, op1=None, r=(), w=()):
        if op1 is None:
            return self.S.op(eng, lambda e: e.tensor_scalar(out=out, in0=in0, scalar1=s1, scalar2=None, op0=op0), reads=r, writes=w)
        return self.S.op(eng, lambda e: e.tensor_scalar(out=out, in0=in0, scalar1=s1, scalar2=s2, op0=op0, op1=op1), reads=r, writes=w)

    def stt(self, out, in0, scalar, in1, op0, op1, r=(), w=()):
        return self.S.op("dve", lambda e: e.scalar_tensor_tensor(out=out, in0=in0, scalar=scalar, in1=in1, op0=op0, op1=op1), reads=r, writes=w)

    def copy(self, eng, out, in_, r=(), w=()):
        if eng == "act":
            return self.act(out, in_, AF.Copy, r=r, w=w)
        return self.S.op(eng, lambda e: e.tensor_copy(out=out, in_=in_), reads=r, writes=w)

    def memset(self, eng, ap, val, r=(), w=()):
        return self.S.op(eng, lambda e: e.memset(ap, val), reads=r, writes=w)

    def scan(self, out, d0, d1, init, r=(), w=()):
        return self.S.op("dve", lambda e: e.tensor_tensor_scan(out=out, data0=d0, data1=d1, initial=init, op0=ALU.mult, op1=ALU.add), reads=r, writes=w)

    def recip(self, out, in_, r=(), w=()):
        return self.S.op("dve", lambda e: e.reciprocal(out=out, in_=in_), reads=r, writes=w)

    def finish(self):
        self.S.emit()
        self.es.close()
        return self.nc


class Rot:
    def __init__(self, items):
        self.items = items
        self.i = 0

    def next(self):
        it = self.items[self.i % len(self.items)]
        self.i += 1
        return it


def emit_modT(kb, wmod, bmodT, cvT, nchunks, modT, pbank, pbank_name, wbufs):
    sT = kb.sb("mod_sT", [128, 8, 2])
    bm = kb.sb("mod_bm", [128, 48])
    kb.dma(sT[:], cvT.rearrange("(k p) j -> p k j", p=128), w=["mod_sT"])
    kb.dma(bm[:], bmodT[:, :], w=["mod_bm"])
    kb.act(sT[:], sT[:], AF.Silu, r=["mod_sT"], w=["mod_sT"])
    ng = nchunks // 2
    for g in range(ng):
        wt, wn = wbufs.next()
        kb.dma(wt[:], wmod[:, g * 256:(g + 1) * 256].rearrange("(k p) n -> p k n", p=128), w=[wn])
        for cc in range(2):
            c = g * 2 + cc
            for k in range(8):
                kb.mm(pbank[:, 2 * c:2 * c + 2], wt[:, k, cc * 128:(cc + 1) * 128], sT[:, k, :], k == 0, k == 7,
                      r=[wn, "mod_sT"], w=[pbank_name])
    for j in range(2):
        kb.tt("dve", modT[:, 0:nchunks, j], pbank[:, j:2 * nchunks:2], bm[:, 0:nchunks], ALU.add,
              r=[pbank_name, "mod_bm"], w=["modT"])


class LNT:
    def __init__(self, kb, ident, pbanks, light=False, xn_bufs=2):
        self.kb = kb
        self.ident = ident
        if not light:
            self.xt = Rot([(kb.sb("ln_xt%d" % i, [128, D]), "ln_xt%d" % i) for i in range(2)])
            self.xn = Rot([(kb.sb("ln_xn%d" % i, [128, D]), "ln_xn%d" % i) for i in range(xn_bufs)])
        self.st = Rot([(kb.sb("ln_st%d" % i, [128, 16]), "ln_st%d" % i) for i in range(2)])
        self.pb = Rot(pbanks)

    def stats_multi(self, xt, xres):
        return self.stats(xt, xres)

    def stats(self, xt, xtn):
        kb = self.kb
        xres = list(xtn) if isinstance(xtn, (list, tuple)) else [xtn]
        st, stn = self.st.next()
        for c in range(2):
            kb.S.op("dve", lambda e, c=c: e.bn_stats(out=st[:, c * 6:(c + 1) * 6], in_=xt[:, c * 512:(c + 1) * 512]),
                    reads=xres, writes=[stn])
        kb.S.op("dve", lambda e: e.bn_aggr(out=st[:, 12:14], in_=st[:, 0:12]), reads=[stn], writes=[stn])
        kb.act(st[:, 14:15], st[:, 13:14], AF.Sqrt, r=[stn], w=[stn], bias=EPS)
        kb.recip(st[:, 14:15], st[:, 14:15], r=[stn], w=[stn])
        return st, stn

    def run(self, x_src, src_res, uT_dst_fn, dst_res, scale_fn, shift_fn, xt_given=None, dst32_fn=None, dst32_res=None):
        kb = self.kb
        if xt_given is None:
            xt, xtn = self.xt.next()
            kb.dma(xt[:], x_src, r=src_res, w=[xtn])
        else:
            xt, xtn = xt_given
        st, stn = self.stats(xt, xtn)
        xn, xnn = self.xn.next()
        xres_ = list(xtn) if isinstance(xtn, (list, tuple)) else [xtn]
        kb.ts("dve", xn[:], xt[:], st[:, 12:13], ALU.subtract, st[:, 14:15], ALU.mult, r=xres_ + [stn], w=[xnn])
        self.last_xn = (xn, xnn)
        for half in range(2):
            pb, pbn = self.pb.next()
            for kk in range(4):
                k = half * 4 + kk
                kb.tr(pb[:, kk * 128:(kk + 1) * 128], xn[:, k * 128:(k + 1) * 128], self.ident[:], r=[xnn, "ident"], w=[pbn])
            for kk in range(4):
                k = half * 4 + kk
                if dst32_fn is not None:
                    if kk % 2 == 0:
                        kb.act(dst32_fn(k), pb[:, kk * 128:(kk + 1) * 128], AF.Identity, r=[pbn, "modT"], w=dst32_res,
                               scale=scale_fn(k), bias=shift_fn(k))
                    else:
                        kb.ts("dve", dst32_fn(k), pb[:, kk * 128:(kk + 1) * 128], scale_fn(k), ALU.mult, shift_fn(k), ALU.add,
                              r=[pbn, "modT"], w=dst32_res)
                    kb.copy("pool", uT_dst_fn(k), dst32_fn(k), r=dst32_res, w=dst_res)
                elif kk % 2 == 0:
                    kb.act(uT_dst_fn(k), pb[:, kk * 128:(kk + 1) * 128], AF.Identity, r=[pbn, "modT"], w=dst_res,
                           scale=scale_fn(k), bias=shift_fn(k))
                else:
                    kb.ts("dve", uT_dst_fn(k), pb[:, kk * 128:(kk + 1) * 128], scale_fn(k), ALU.mult, shift_fn(k), ALU.add,
                          r=[pbn, "modT"], w=dst_res)
        return xt, xtn, st, stn


BLKS = [(0, 256)] + [(256 + 512 * j, 256 + 512 * (j + 1)) for j in range(8)]


def ut_res(t0, t1):
    return ["uT.%d" % i for i in range(t0 // 128, (t1 + 127) // 128)]


class PhaseA:
    def __init__(self, with_ctx, mixers=("lru", "s5", "na", "hyena"), dbg=False, kb=None, xin_fn=None, ybuf=None,
                 modT=None, uT=None, compute_uT=True):
        self.with_ctx = with_ctx
        self.standalone = kb is None
        kb = self.kb = kb if kb is not None else KB()
        self.dbg = dbg
        if xin_fn is None:
            self.xin = kb.din("xin", [T, D])
            xin_fn = lambda t: self.xin[t * 128:(t + 1) * 128, :]
        if modT is None:
            self.cvT = kb.din("cvT", [D, 2])
            self.wmod = kb.din("wmodA", [D, 2048])
            self.bmodT = kb.din("bmodT", [128, 48])
        self.ident_d = kb.din("ident_d", [128, 128])
        self.w_in = kb.din("w_inA", [D, 1472])
        self.ybuf = ybuf if ybuf is not None else kb.dout("ybuf", [640, T], BF16)
        self.ident = kb.sb("ident", [128, 128])
        kb.dma(self.ident[:], self.ident_d[:, :], w=["ident"])
        self.identb = kb.sb("identb", [128, 128], BF16)
        kb.copy("dve", self.identb[:], self.ident[:], r=["ident"], w=["identb"])
        self.uT = uT if uT is not None else kb.sb("uT", [128, 8, T], BF16)
        self.FP = kb.sb("FP", [128, 4 * T])
        self.CP = Rot([(kb.sb("C%d" % i, [128, 512]), "C%d" % i) for i in range(8)])
        self.PS = [(kb.ps("PS%d" % i), "PS%d" % i) for i in range(8)]
        self.modT = modT if modT is not None else kb.sb("modT", [128, 48, 2])
        self.ost = Rot([(kb.sb("ost%d" % i, [128, 512], BF16), "ost%d" % i) for i in range(3)])
        with kb.scope():
            if modT is None:
                self.wmb = Rot([(kb.sb("wmb%d" % i, [128, 8, 256]), "wmb%d" % i) for i in range(2)])
                emit_modT(kb, self.wmod, self.bmodT, self.cvT, 16, self.modT, self.PS[7][0], "PS7", self.wmb)
                kb.ts("dve", self.modT[:, 8:16, :], self.modT[:, 8:16, :], 1.0, ALU.add, r=["modT"], w=["modT"])
            lnt = LNT(kb, self.ident, [self.PS[0], self.PS[1]]) if compute_uT else None
            for t in range(NT if compute_uT else 0):
                j = 1 if t < 2 else 0
                lnt.run(xin_fn(t), ["xsrc"],
                        lambda k, t=t: self.uT[:, k, t * 128:(t + 1) * 128], ["uT.%d" % t],
                        lambda k, j=j: self.modT[:, 8 + k, j:j + 1], lambda k, j=j: self.modT[:, k, j:j + 1])
        self.psr = Rot(self.PS[0:4])
        for name in mixers:
            with kb.scope():
                getattr(self, name)()
        if self.standalone:
            self.nc = kb.finish()

    def F(self, s, t0=0, t1=T):
        return self.FP[:, s * T + t0:s * T + t1]

    def fres(self, s, t0=0, t1=T):
        return ["F%d.%d" % (s, bi) for bi, (a, b) in enumerate(BLKS) if a < t1 and b > t0]

    def load_w(self, name, c0, ncols):
        wt = self.kb.sb(name, [128, 8, ncols], BF16)
        self.kb.dma_cast(wt[:], self.w_in[:, c0:c0 + ncols].rearrange("(k p) n -> p k n", p=128), w=[name])
        return wt

    def proj(self, wt, wname, c0, m, blk, pb, pbn):
        t0, t1 = blk
        for k in range(8):
            self.kb.mm(pb[0:m, 0:t1 - t0], wt[:, k, c0:c0 + m], self.uT[:, k, t0:t1], k == 0, k == 7,
                       r=[wname] + ut_res(t0, t1), w=[pbn])

    def lru(self):
        kb = self.kb
        cw_d = kb.din("lru_cw_h", [192, 5])
        W_d = kb.din("lru_W_h", [2, 2, 192, 192])
        vec_d = kb.din("lru_vec_h", [192, 6])
        wt = self.load_w("w_lru", 0, 384)
        for ct, (p0, P) in enumerate([(0, 128), (128, 64)]):
            sfx = "_%d" % ct
            cw = kb.sb("lru_cw" + sfx, [128, 5])
            vec = kb.sb("lru_vec" + sfx, [128, 6])
            nsp = kb.sb("lru_nsp" + sfx, [128, 2])
            Wt = kb.sb("lru_Wt" + sfx, [128, 4, 128])
            kb.dma(cw[0:P, :], cw_d[p0:p0 + P, :], w=["lru_cw" + sfx])
            kb.dma(vec[0:P, :], vec_d[p0:p0 + P, :], w=["lru_vec" + sfx])
            for d in range(2):
                for ri in range(2):
                    kb.dma(Wt[0:P, d * 2 + ri, 0:P], W_d[d, ri, p0:p0 + P, p0:p0 + P], w=["lru_Wt" + sfx])
            for d in range(2):
                kb.act(nsp[0:P, d:d + 1], vec[0:P, d * 3 + 2:d * 3 + 3], AF.Exp, r=["lru_vec" + sfx], w=["lru_nsp" + sfx], scale=-1.0)
            kb.act(nsp[0:P, :], nsp[0:P, :], AF.Ln, r=["lru_nsp" + sfx], w=["lru_nsp" + sfx], bias=1.0)
            kb.ts("dve", nsp[0:P, :], nsp[0:P, :], -8.0, ALU.mult, r=["lru_nsp" + sfx], w=["lru_nsp" + sfx])
            for bi, blk in enumerate(BLKS):
                t0, t1 = blk
                pb, pbn = self.psr.next()
                self.proj(wt, "w_lru", p0, P, blk, pb, pbn)
                kb.copy("act", self.F(0, t0, t1)[0:P], pb[0:P, 0:t1 - t0], r=[pbn], w=["F0.%d" % bi])
                pb, pbn = self.psr.next()
                self.proj(wt, "w_lru", 192 + p0, P, blk, pb, pbn)
                kb.copy("dve", self.F(1, t0, t1)[0:P], pb[0:P, 0:t1 - t0], r=[pbn], w=["F1.%d" % bi])
            for (s0, s1) in [(0, CTXL), (CTXL, T)]:
                rr = self.fres(1, s0, s1)
                ww = self.fres(2, s0, s1)
                kb.ts("dve", self.F(2, s0, s1)[0:P], self.F(1, s0, s1)[0:P], cw[0:P, 2:3], ALU.mult, cw[0:P, 4:5], ALU.add,
                      r=rr + ["lru_cw" + sfx], w=ww)
                for kk, sh in [(0, -2), (1, -1), (3, 1)]:
                    if sh < 0:
                        o = self.F(2, s0 - sh, s1)[0:P]
                        i0 = self.F(1, s0, s1 + sh)[0:P]
                    else:
                        o = self.F(2, s0, s1 - sh)[0:P]
                        i0 = self.F(1, s0 + sh, s1)[0:P]
                    kb.stt(o, i0, cw[0:P, kk:kk + 1], o, ALU.mult, ALU.add, r=rr + ww + ["lru_cw" + sfx], w=ww)
            for d in range(2):
                order = list(range(len(BLKS))) if d == 0 else [0] + list(range(len(BLKS) - 1, 0, -1))
                hs = 1 if d == 0 else 3
                for oi, bi in enumerate(order):
                    t0, t1 = BLKS[bi]
                    n = t1 - t0
                    xcb = self.F(2, t0, t1)[0:P]
                    pr, prn = self.psr.next()
                    kb.mm(pr[0:P, 0:n], Wt[0:P, d * 2 + 0, 0:P], xcb, True, True, r=["lru_Wt" + sfx, "F2.%d" % bi], w=[prn])
                    pi, pin = self.psr.next()
                    kb.mm(pi[0:P, 0:n], Wt[0:P, d * 2 + 1, 0:P], xcb, True, True, r=["lru_Wt" + sfx, "F2.%d" % bi], w=[pin])
                    gr, grn = self.CP.next()
                    gi, gin = self.CP.next()
                    a, an = self.CP.next()
                    om, omn = self.CP.next()
                    kb.act(gr[0:P, 0:n], pr[0:P, 0:n], AF.Sigmoid, r=[prn, "lru_vec" + sfx], w=[grn], bias=vec[0:P, d * 3:d * 3 + 1])
                    kb.act(gi[0:P, 0:n], pi[0:P, 0:n], AF.Sigmoid, r=[pin, "lru_vec" + sfx], w=[gin], bias=vec[0:P, d * 3 + 1:d * 3 + 2])
                    kb.act(a[0:P, 0:n], gr[0:P, 0:n], AF.Exp, r=[grn, "lru_nsp" + sfx], w=[an], scale=nsp[0:P, d:d + 1])
                    kb.tt("pool", om[0:P, 0:n], a[0:P, 0:n], a[0:P, 0:n], ALU.mult, r=[an], w=[omn])
                    kb.act(om[0:P, 0:n], om[0:P, 0:n], AF.Sqrt, r=[omn], w=[omn], scale=-1.0, bias=1.0)
                    kb.tt("pool", gi[0:P, 0:n], gi[0:P, 0:n], om[0:P, 0:n], ALU.mult, r=[gin, omn], w=[gin])
                    kb.tt("pool", gi[0:P, 0:n], gi[0:P, 0:n], xcb, ALU.mult, r=[gin, "F2.%d" % bi], w=[gin])
                    if d == 0:
                        init = 0.0 if oi == 0 else self.F(1, t0 - 1, t0)[0:P]
                        ir = [] if oi == 0 else self.fres(1, t0 - 1, t0)
                        kb.scan(self.F(1, t0, t1)[0:P], a[0:P, 0:n], gi[0:P, 0:n], init, r=[an, gin] + ir, w=["F1.%d" % bi])
                    else:
                        if oi == 0:
                            init, ir = 0.0, []
                        elif oi == 1:
                            init, ir = self.F(3, 0, 1)[0:P], ["F3.0"]
                        else:
                            init, ir = self.F(3, t1, t1 + 1)[0:P], self.fres(3, t1, t1 + 1)
                        kb.scan(self.F(3, t0, t1)[0:P, ::-1], a[0:P, n - 1::-1], gi[0:P, n - 1::-1], init, r=[an, gin] + ir, w=["F3.%d" % bi])
                        if bi == 0 and not self.with_ctx:
                            continue
                        kb.tt("pool", gr[0:P, 0:n], self.F(1, t0, t1)[0:P], self.F(3, t0, t1)[0:P], ALU.add, r=["F1.%d" % bi, "F3.%d" % bi, grn], w=[grn])
                        kb.act(om[0:P, 0:n], self.F(0, t0, t1)[0:P], AF.Gelu_apprx_tanh, r=["F0.%d" % bi, omn], w=[omn])
                        ob, obn = self.ost.next()
                        kb.tt("dve", ob[0:P, 0:n], gr[0:P, 0:n], om[0:P, 0:n], ALU.mult, r=[grn, omn], w=[obn])
                        kb.dma(self.ybuf[p0:p0 + P, t0:t1], ob[0:P, 0:n], r=[obn], final=True)


def _f32(a):
    return np.ascontiguousarray(a, dtype=np.float32)


def blockdiag(blocks):
    n = sum(b.shape[0] for b in blocks)
    m = sum(b.shape[1] for b in blocks)
    out = np.zeros((n, m), np.float32)
    i = j = 0
    for b in blocks:
        out[i:i + b.shape[0], j:j + b.shape[1]] = b
        i += b.shape[0]
        j += b.shape[1]
    return out


def colsel_A(h):
    a_g = OFF_A + h * 192 + np.arange(192)
    a_x = OFF_A + LRU_W + h * 192 + np.arange(192)
    b = [OFF_B + j * HY_W + h * 128 + np.arange(128) for j in range(3)]
    c = [OFF_C + j * NA_W + h * 192 + np.arange(192) for j in range(3)]
    d = OFF_D + h * 128 + np.arange(128)
    return np.concatenate([a_g, a_x] + b + c + [d])


def prep_A(inp, l, b, h, xs, ctxs):
    m = {}
    m["xin"] = _f32(np.concatenate([ctxs[b], xs[b]], axis=0))
    m["cvT"] = _f32(np.stack([inp["c"][b], inp["c_ctx"]], axis=1))
    m["wmodA"] = _f32(inp["w_mod"][l][:, 0:2048])
    m["bmodT"] = _f32(inp["b_mod"][l].reshape(48, 128).T)
    m["ident_d"] = np.eye(128, dtype=np.float32)
    m["w_inA"] = _f32(inp["w_in"][l][:, colsel_A(h)])
    ch = h * 192 + np.arange(192)
    m["lru_cw_h"] = _f32(np.concatenate([inp["lru_conv_w"][l][:, ch].T, inp["lru_conv_b"][l][ch][:, None]], axis=1))
    W = np.zeros((2, 2, 192, 192), np.float32)
    for d in range(2):
        W[d, 0] = blockdiag([inp["lru_w_r"][l][d][3 * h + g] for g in range(3)])
        W[d, 1] = blockdiag([inp["lru_w_i"][l][d][3 * h + g] for g in range(3)])
    m["lru_W_h"] = W
    m["lru_vec_h"] = _f32(np.stack([inp[k][l][d][ch] for d in range(2) for k in ("lru_b_r", "lru_b_i", "lru_lambda")], axis=1))
    prep_A_s5(inp, l, h, m)
    prep_A_na(inp, l, h, m)
    prep_A_hy(inp, l, h, m)
    return m


def prep_A_s5(inp, l, h, m):
    pa = np.zeros((128, 2, 4, 3), np.float32)
    BB = np.zeros((2, 4, 2, 128, 128), np.float32)
    CT = np.zeros((2, 4, 2, 128, 128), np.float32)
    for d in range(2):
        for st in range(4):
            for gl in range(2):
                gloc = 2 * st + gl
                g = 8 * h + gloc
                sl = slice(gl * 64, (gl + 1) * 64)
                pa[sl, d, st, 0] = inp["s5_a_re"][l][d][g]
                pa[sl, d, st, 1] = inp["s5_a_im"][l][d][g]
                pa[sl, d, st, 2] = inp["s5_log_dt"][l][d][g]
                cs = slice(gloc * 16, (gloc + 1) * 16)
                BB[d, st, 0, cs, sl] = inp["s5_b_re"][l][d][g].T
                BB[d, st, 1, cs, sl] = inp["s5_b_im"][l][d][g].T
                CT[d, st, 0, sl, cs] = inp["s5_c_re"][l][d][g].T
                CT[d, st, 1, sl, cs] = inp["s5_c_im"][l][d][g].T
    m["s5_pa_h"] = pa
    m["s5_BB_h"] = BB
    m["s5_CT_h"] = CT
    m["s5_d_h"] = _f32(inp["s5_d"][l][h * 128:(h + 1) * 128][:, None])
    m["s5_tau_h"] = np.tile(np.arange(256, dtype=np.float32)[None, :], (128, 1))


def _phasea_s5(self):
    kb = self.kb
    TC = 256
    pa_d = kb.din("s5_pa_h", [128, 2, 4, 3])
    BB_d = kb.din("s5_BB_h", [2, 4, 2, 128, 128])
    CT_d = kb.din("s5_CT_h", [2, 4, 2, 128, 128])
    d_d = kb.din("s5_d_h", [128, 1])
    tau_d = kb.din("s5_tau_h", [128, TC])
    wt = self.load_w("w_s5", 1344, 128)
    pa = kb.sb("s5_pa", [128, 8, 3])
    BB = kb.sb("s5_BB", [128, 16, 128])
    CT = kb.sb("s5_CT", [128, 16, 128])
    dv = kb.sb("s5_dv", [128, 1])
    tau = kb.sb("s5_tau", [128, TC])
    sc = kb.sb("s5_sc", [128, 16, 8])
    carry = kb.sb("s5_carry", [128, 8, 2])
    kb.dma(pa[:], pa_d.rearrange("p d s c -> p (d s) c"), w=["s5_pa"])
    kb.dma(BB[:], BB_d.rearrange("d s c k m -> k (d s c) m"), w=["s5_BB"])
    kb.dma(CT[:], CT_d.rearrange("d s c k m -> k (d s c) m"), w=["s5_CT"])
    kb.dma(dv[:], d_d[:, :], w=["s5_dv"])
    kb.dma(tau[:], tau_d[:, :], w=["s5_tau"])
    for i in range(8):
        kb.ts("pool", CT[:, 2 * i + 1, :], CT[:, 2 * i + 1, :], -1.0, ALU.mult, r=["s5_CT"], w=["s5_CT"])
    kb.memset("dve", carry[:], 0.0, w=["s5_carry%d" % i for i in range(8)])
    DT, LAM, ANG, RHO, RC, C1, S1, ABR, ABI, DEN, FRE, FIM, T0, T1, RC4 = range(15)
    CT_, ST_ = 15, 14
    R = ["s5_sc"]

    def v(i):
        return sc[:, i, :]

    are, aim, ldt = pa[:, :, 0], pa[:, :, 1], pa[:, :, 2]
    kb.act(v(DT), ldt, AF.Exp, r=["s5_pa"], w=R)
    kb.tt("dve", v(LAM), v(DT), are, ALU.mult, r=R + ["s5_pa"], w=R)
    kb.tt("dve", v(ANG), v(DT), aim, ALU.mult, r=R + ["s5_pa"], w=R)
    kb.act(v(RHO), v(LAM), AF.Exp, r=R, w=R)
    kb.ts("dve", v(RC), v(ANG), 1.0 / TWO_PI, ALU.mult, r=R, w=R)
    kb.ts("dve", v(RC4), v(RC), 0.25, ALU.add, r=R, w=R)

    def sin_cycles(out, rin, shape_tmp, rres, wres):
        kb.ts("dve", shape_tmp, rin, MAGIC, ALU.add, r=rres, w=wres)
        kb.ts("dve", shape_tmp, shape_tmp, MAGIC, ALU.subtract, r=wres, w=wres)
        kb.tt("dve", shape_tmp, rin, shape_tmp, ALU.subtract, r=rres + wres, w=wres)
        kb.act(out, shape_tmp, AF.Sin, r=wres, w=wres, scale=TWO_PI)

    sin_cycles(v(S1), v(RC), v(T0), R, R)
    sin_cycles(v(C1), v(RC4), v(T0), R, R)
    kb.ts("dve", v(T1), v(RC), float(TC), ALU.mult, r=R, w=R)
    kb.ts("dve", v(CT_), v(T1), 0.25, ALU.add, r=R, w=R)
    sin_cycles(v(ST_), v(T1), v(T0), R, R)
    sin_cycles(v(CT_), v(CT_), v(T0), R, R)
    kb.tt("dve", v(ABR), v(RHO), v(C1), ALU.mult, r=R, w=R)
    kb.tt("dve", v(ABI), v(RHO), v(S1), ALU.mult, r=R, w=R)
    kb.tt("dve", v(DEN), are, are, ALU.mult, r=R + ["s5_pa"], w=R)
    kb.tt("dve", v(T0), aim, aim, ALU.mult, r=R + ["s5_pa"], w=R)
    kb.tt("dve", v(DEN), v(DEN), v(T0), ALU.add, r=R, w=R)
    kb.recip(v(DEN), v(DEN), r=R, w=R)
    kb.ts("dve", v(T1), v(ABR), -1.0, ALU.add, r=R, w=R)
    kb.tt("dve", v(FRE), v(T1), are, ALU.mult, r=R + ["s5_pa"], w=R)
    kb.tt("dve", v(T0), v(ABI), aim, ALU.mult, r=R + ["s5_pa"], w=R)
    kb.tt("dve", v(FRE), v(FRE), v(T0), ALU.add, r=R, w=R)
    kb.tt("dve", v(FRE), v(FRE), v(DEN), ALU.mult, r=R, w=R)
    kb.tt("dve", v(FIM), v(ABI), are, ALU.mult, r=R + ["s5_pa"], w=R)
    kb.tt("dve", v(T0), v(T1), aim, ALU.mult, r=R + ["s5_pa"], w=R)
    kb.tt("dve", v(FIM), v(FIM), v(T0), ALU.subtract, r=R, w=R)
    kb.tt("dve", v(FIM), v(FIM), v(DEN), ALU.mult, r=R, w=R)
    for idx in range(8):
        Cc = self.F(2, idx * 512, idx * 512 + TC)
        Ss = self.F(2, idx * 512 + TC, idx * 512 + 2 * TC)
        Ire = self.F(3, idx * 512, idx * 512 + TC)
        Iim = self.F(3, idx * 512 + TC, idx * 512 + 2 * TC)
        tn = ["s5_tab%d" % idx]
        t0, t0n = self.CP.next()
        t1, t1n = self.CP.next()
        kb.ts("dve", t0[:, 0:TC], tau[:], sc[:, RC, idx:idx + 1], ALU.mult, r=R + ["s5_tau"], w=[t0n])
        sin_cycles(Ss, t0[:, 0:TC], t1[:, 0:TC], [t0n], [t1n] + tn)
        kb.ts("dve", t0[:, 0:TC], t0[:, 0:TC], 0.25, ALU.add, r=[t0n], w=[t0n])
        sin_cycles(Cc, t0[:, 0:TC], t1[:, 0:TC], [t0n], [t1n] + tn)
        kb.ts("dve", t0[:, 0:TC], Ss, sc[:, FIM, idx:idx + 1], ALU.mult, r=R + tn, w=[t0n])
        kb.stt(Ire, Cc, sc[:, FRE, idx:idx + 1], t0[:, 0:TC], ALU.mult, ALU.add, r=R + tn + [t0n], w=tn)
        kb.ts("dve", t1[:, 0:TC], Ss, sc[:, FRE, idx:idx + 1], ALU.mult, r=R + tn, w=[t1n])
        kb.stt(Iim, Cc, sc[:, FIM, idx:idx + 1], t1[:, 0:TC], ALU.mult, ALU.subtract, r=R + tn + [t1n], w=tn)
    for bi, blk in enumerate(BLKS):
        t0_, t1_ = blk
        pb, pbn = self.psr.next()
        self.proj(wt, "w_s5", 0, 128, blk, pb, pbn)
        kb.copy("act", self.F(0, t0_, t1_), pb[:, 0:t1_ - t0_], r=[pbn], w=["F0.%d" % bi])
    halves = []
    for (cb, cn) in self.CP.items:
        halves.append((cb[:, 0:TC], cn + "a"))
        halves.append((cb[:, TC:2 * TC], cn + "b"))
    SP = Rot(halves)
    py, pyn = self.PS[4]
    NCH = T // TC
    for d in range(2):
        order = list(range(NCH)) if d == 0 else [0] + list(range(NCH - 1, 0, -1))
        for ci in order:
            c0, c1_ = ci * TC, (ci + 1) * TC
            ub = self.F(0, c0, c1_)
            ures = self.fres(0, c0, c1_)
            for st in range(4):
                idx = d * 4 + st
                tn = ["s5_tab%d" % idx]
                Cc = self.F(2, idx * 512, idx * 512 + TC)
                Ss = self.F(2, idx * 512 + TC, idx * 512 + 2 * TC)
                Ire = self.F(3, idx * 512, idx * 512 + TC)
                Iim = self.F(3, idx * 512 + TC, idx * 512 + 2 * TC)
                pre, pren = self.psr.next()
                pim, pimn = self.psr.next()
                kb.mm(pre[:, 0:TC], BB[:, idx * 2 + 0, :], ub, True, True, r=["s5_BB"] + ures, w=[pren])
                kb.mm(pim[:, 0:TC], BB[:, idx * 2 + 1, :], ub, True, True, r=["s5_BB"] + ures, w=[pimn])
                bre = pre[:, 0:TC] if d == 0 else pre[:, TC - 1::-1]
                bim = pim[:, 0:TC] if d == 0 else pim[:, TC - 1::-1]
                (a1, a1n), (a2, a2n), (a3, a3n), (a4, a4n) = SP.next(), SP.next(), SP.next(), SP.next()
                kb.tt("dve", a1, bre, Ire, ALU.mult, r=[pren] + tn, w=[a1n])
                kb.tt("dve", a2, bim, Iim, ALU.mult, r=[pimn] + tn, w=[a2n])
                kb.tt("dve", a3, bim, Ire, ALU.mult, r=[pimn] + tn, w=[a3n])
                kb.tt("dve", a4, bre, Iim, ALU.mult, r=[pren] + tn, w=[a4n])
                kb.tt("pool", a1, a1, a2, ALU.subtract, r=[a1n, a2n], w=[a1n])
                kb.tt("pool", a3, a3, a4, ALU.add, r=[a3n, a4n], w=[a3n])
                (gre, gren), (gim, gimn) = SP.next(), SP.next()
                rho_bc = sc[:, RHO, idx:idx + 1].to_broadcast([128, TC])
                kb.scan(gre, rho_bc, a1, carry[:, idx, 0:1], r=R + [a1n, "s5_carry%d" % idx], w=[gren])
                kb.scan(gim, rho_bc, a3, carry[:, idx, 1:2], r=R + [a3n, "s5_carry%d" % idx], w=[gimn])
                (cq, cqn) = SP.next()
                gl_re, gl_im = gre[:, TC - 1:TC], gim[:, TC - 1:TC]
                kb.ts("dve", cq[:, 0:1], gl_im, sc[:, ST_, idx:idx + 1], ALU.mult, r=[gimn] + R, w=[cqn])
                kb.stt(carry[:, idx, 0:1], gl_re, sc[:, CT_, idx:idx + 1], cq[:, 0:1], ALU.mult, ALU.subtract,
                       r=[gren, cqn, "s5_carry%d" % idx] + R, w=["s5_carry%d" % idx])
                kb.ts("dve", cq[:, 1:2], gl_im, sc[:, CT_, idx:idx + 1], ALU.mult, r=[gimn, cqn] + R, w=[cqn])
                kb.stt(carry[:, idx, 1:2], gl_re, sc[:, ST_, idx:idx + 1], cq[:, 1:2], ALU.mult, ALU.add,
                       r=[gren, cqn, "s5_carry%d" % idx] + R, w=["s5_carry%d" % idx])
                (u1, u1n), (u2, u2n), (u3, u3n), (u4, u4n) = SP.next(), SP.next(), SP.next(), SP.next()
                kb.tt("pool", u1, Cc, gre, ALU.mult, r=tn + [gren], w=[u1n])
                kb.tt("pool", u2, Ss, gim, ALU.mult, r=tn + [gimn], w=[u2n])
                kb.tt("pool", u3, Ss, gre, ALU.mult, r=tn + [gren], w=[u3n])
                kb.tt("pool", u4, Cc, gim, ALU.mult, r=tn + [gimn], w=[u4n])
                (hre, hren), (him, himn) = SP.next(), SP.next()
                hre_o = hre if d == 0 else hre[:, ::-1]
                him_o = him if d == 0 else him[:, ::-1]
                kb.tt("dve", hre_o, u1, u2, ALU.subtract, r=[u1n, u2n], w=[hren])
                kb.tt("dve", him_o, u3, u4, ALU.add, r=[u3n, u4n], w=[himn])
                kb.mm(py[:, 0:TC], CT[:, idx * 2 + 0, :], hre, st == 0, False, r=["s5_CT", hren], w=[pyn])
                kb.mm(py[:, 0:TC], CT[:, idx * 2 + 1, :], him, False, st == 3, r=["s5_CT", himn], w=[pyn])
            f1res = self.fres(1, c0, c1_)
            if d == 0:
                kb.copy("act", self.F(1, c0, c1_), py[:, 0:TC], r=[pyn], w=f1res)
            else:
                if ci == 0 and not self.with_ctx:
                    continue
                (y1, y1n) = SP.next()
                kb.tt("dve", y1, py[:, 0:TC], self.F(1, c0, c1_), ALU.add, r=[pyn] + f1res, w=[y1n])
                kb.stt(y1, ub, dv[:, 0:1], y1, ALU.mult, ALU.add, r=ures + ["s5_dv", y1n], w=[y1n])
                ob, obn = self.ost.next()
                kb.act(ob[:, 0:TC], y1, AF.Gelu_apprx_tanh, r=[y1n], w=[obn])
                kb.dma(self.ybuf[512:640, c0:c1_], ob[:, 0:TC], r=[obn], final=True)


PhaseA.s5 = _phasea_s5


def prep_A_na(inp, l, h, m):
    jj = np.arange(64)[:, None]
    ww = np.arange(64)[None, :]
    cs = np.clip(ww - 8, 0, 48)
    allowed = (jj >= cs) & (jj < cs + 16)
    dc = np.clip(jj - ww + 15, 0, 30)
    rpb = inp["na_rpb"][l][3 * h:3 * h + 3]
    g = rpb[:, :, dc]
    m["na_bias_h"] = _f32(np.transpose(g, (2, 0, 1, 3)))
    mask = np.where(allowed, 0.0, -30000.0).astype(np.float32)
    m["na_mask_h"] = _f32(np.tile(mask[:, None, :], (1, 15, 1)).reshape(64, 15 * 64))


def _phasea_na(self):
    kb = self.kb
    bias_d = kb.din("na_bias_h", [64, 3, 15, 64])
    mask_d = kb.din("na_mask_h", [64, 15 * 64])
    wt = self.load_w("w_na", 768, 576)
    bias = kb.sb("na_bias", [64, 3, 15 * 64])
    mask = kb.sb("na_mask", [64, 15 * 64])
    ones = kb.sb("na_ones", [64, 64], BF16)
    PTs = Rot([(kb.sb("na_pt%d" % i, [64, 768], BF16), "na_pt%d" % i) for i in range(2)])
    recs = Rot([(kb.sb("na_rec%d" % i, [64, 64]), "na_rec%d" % i) for i in range(2)])
    kb.dma(bias[:], bias_d.rearrange("j h d w -> j h (d w)"), w=["na_bias"])
    kb.dma(mask[:], mask_d[:, :], w=["na_mask"])
    kb.memset("pool", ones[:], 1.0, w=["na_ones"])
    onesf = kb.sb("na_onesf", [64, 64])
    kb.memset("pool", onesf[:], 1.0, w=["na_onesf"])
    ptsums = Rot([(kb.sb("na_ptsum%d" % i, [64, 64]), "na_ptsum%d" % i) for i in range(2)])
    for hd in range(3):
        kb.tt("pool", bias[:, hd, :], bias[:, hd, :], mask[:], ALU.add, r=["na_bias", "na_mask"], w=["na_bias"])
    FPb = self.FP[:].bitcast(BF16)
    HB = 2 * T
    Vall = FPb[0:64, 0:68 * 192]
    qT = FPb[0:64, 2 * HB:2 * HB + T]
    kT = FPb[0:64, 2 * HB + T:3 * HB]
    yo = FPb[0:64, 3 * HB:3 * HB + T]
    sA = Rot(self.PS[0:2])
    sB = Rot(self.PS[2:4])
    sCD = Rot(self.PS[4:6])
    sP = Rot(self.PS[6:8])
    for u2 in range(34):
        pb, pbn = sP.next()
        for uu in range(2):
            u = u2 * 2 + uu
            for k in range(8):
                kb.mm(pb[0:64, uu * 192:(uu + 1) * 192], self.uT[:, k, u * 64:(u + 1) * 64], wt[:, k, 384:576], k == 0, k == 7,
                      r=["w_na", "uT.%d" % (u // 2)], w=[pbn])
        kb.copy("act" if u2 % 2 == 0 else "dve", Vall[:, u2 * 384:(u2 + 1) * 384], pb[0:64, 0:384], r=[pbn], w=["na_V.%d" % u2])
    for hd in range(3):
        for bi, blk in enumerate(BLKS):
            t0, t1 = blk
            pb, pbn = sP.next()
            self.proj(wt, "w_na", hd * 64, 64, blk, pb, pbn)
            kb.act(qT[:, t0:t1], pb[0:64, 0:t1 - t0], AF.Copy, r=[pbn], w=["na_q.%d" % bi], scale=0.125)
            pb, pbn = sP.next()
            self.proj(wt, "w_na", 192 + hd * 64, 64, blk, pb, pbn)
            kb.copy("dve", kT[:, t0:t1], pb[0:64, 0:t1 - t0], r=[pbn], w=["na_k.%d" % bi])

        def blk_of(t0, t1, pref):
            return ["%s.%d" % (pref, bi) for bi, (a, b) in enumerate(BLKS) if a < t1 and b > t0]

        def attend(q0, win_units, use_bias_dr0):
            qres = blk_of(q0, q0 + 64, "na_q")
            pt, ptn = PTs.next()
            units = []
            if win_units:
                pa_, pan = sA.next()
                for i, u in enumerate(win_units):
                    kb.mm(pa_[0:64, i * 64:(i + 1) * 64], kT[:, u * 64:(u + 1) * 64], qT[:, q0:q0 + 64], True, True,
                          r=qres + blk_of(u * 64, u * 64 + 64, "na_k"), w=[pan])
                tmp, tmpn = self.CP.next()
                dr0 = use_bias_dr0
                kb.tt("dve", tmp[0:64, :], pa_[0:64, :], bias[:, hd, dr0 * 64:(dr0 + 8) * 64], ALU.add, r=[pan, "na_bias"], w=[tmpn])
                kb.act(pt[:, 0:512], tmp[0:64, :], AF.Exp, r=[tmpn], w=[ptn])
                units += [(u, i * 64) for i, u in enumerate(win_units)]
            pb_, pbn_ = sB.next()
            for u in range(4):
                kb.mm(pb_[0:64, u * 64:(u + 1) * 64], kT[:, u * 64:(u + 1) * 64], qT[:, q0:q0 + 64], True, True,
                      r=qres + ["na_k.0"], w=[pbn_])
            kb.act(pt[:, 512:768], pb_[0:64, 0:256], AF.Exp, r=[pbn_], w=[ptn])
            units += [(u, 512 + u * 64) for u in range(4)]
            return (q0, pt, ptn, units)

        def attend2(ctx_):
            q0, pt, ptn, units = ctx_
            _sub = 9
            pcd, pcdn = sCD.next()
            nu = len(units)
            for i, (u, off) in enumerate(units):
                kb.mm(pcd[0:64, 0:64], Vall[:, u * 192 + hd * 64:u * 192 + (hd + 1) * 64], pt[:, off:off + 64], i == 0, i == nu - 1,
                      r=[ptn, "na_V.%d" % (u // 2)], w=[pcdn])
            if _sub < 2:
                return
            psm, psmn = ptsums.next()
            lo = units[0][1]
            pv = pt[:, lo:lo + nu * 64].rearrange("p (u q) -> p q u", q=64)
            kb.S.op("dve", lambda e, psm=psm, pv=pv: e.tensor_reduce(out=psm[:], in_=pv, axis=mybir.AxisListType.X, op=ALU.add),
                    reads=[ptn], writes=[psmn])
            kb.mm(pcd[0:64, 64:128], onesf[:], psm[:], True, True, r=[psmn, "na_onesf"], w=[pcdn])
            if _sub < 3:
                return
            rc, rcn = recs.next()
            kb.copy("act", rc[:], pcd[0:64, 64:128], r=[pcdn], w=[rcn])
            kb.recip(rc[:], rc[:], r=[rcn], w=[rcn])
            kb.tt("dve", rc[:], pcd[0:64, 0:64], rc[:], ALU.mult, r=[pcdn, rcn], w=[rcn])
            kb.copy("act", yo[:, q0:q0 + 64], rc[:], r=[rcn], w=["na_yo.%d" % (q0 // 512)])

        jobs = []
        if self.with_ctx:
            jobs += [(cu * 64, [], None) for cu in range(4)]
        for r in range(64):
            rs = min(max(r - 4, 0), 56)
            jobs.append((CTXL + 64 * r, [4 + rs + i for i in range(8)], rs - r + 7))
        prev = None
        for jb in jobs:
            cur = attend(*jb)
            if prev is not None:
                attend2(prev)
            prev = cur
        attend2(prev)
        t_lo = 0 if self.with_ctx else CTXL
        kb.dma(self.ybuf[320 + hd * 64:320 + (hd + 1) * 64, t_lo:T], yo[:, t_lo:T],
               r=["na_yo.%d" % i for i in range(9)], final=True)


PhaseA.na = _phasea_na


HY_MAX_DECAY = math.log(1e-2) / 0.3
HY_MIN_DECAY = math.log(1e-2) / 1.5
_HYC = {}


def hy_consts():
    if _HYC:
        return _HYC
    c = _HYC

    def feat(L):
        t = np.arange(L, dtype=np.float32)
        tn = t / np.float32(L)
        bands = np.arange(1, 17, dtype=np.float32)
        ang = np.float32(2.0 * math.pi / L) * t[:, None] * bands[None, :]
        return np.concatenate([tn[:, None], np.cos(ang), np.sin(ang)], axis=-1).astype(np.float32).T.copy()

    c["hy_feat_l_h"] = feat(SEQ)
    c["hy_feat_c_h"] = feat(CTXL)
    c["hy_t_h"] = np.tile(np.arange(SEQ, dtype=np.float32)[None, :], (128, 1))
    N = 8192
    n1 = np.arange(32)[:, None]
    k1 = np.arange(64)[None, :]
    a = 2 * np.pi * n1 * k1 / 64
    c["hy_W1f_h"] = np.concatenate([np.cos(a), -np.sin(a)], axis=1).astype(np.float32)
    n2 = np.arange(128)[:, None]
    a = 2 * np.pi * n2 * k1 / N
    T1 = np.stack([np.cos(a), -np.sin(a)], axis=0)
    c["hy_T1_h"] = np.ascontiguousarray(np.tile(T1[:, :, None, :], (1, 1, 16, 1)).transpose(1, 0, 2, 3)).astype(np.float32)
    k2 = np.arange(128)[None, :]
    a = 2 * np.pi * n2 * k2 / 128
    c["hy_W2_h"] = np.stack([np.cos(a), -np.sin(a), np.sin(a)], axis=1).astype(np.float32)
    G1 = np.concatenate([np.cos(a), np.sin(a)], axis=1)
    G2 = np.concatenate([-np.sin(a), np.cos(a)], axis=1)
    c["hy_G_h"] = np.stack([G1, G2], axis=1).astype(np.float32)
    kk1 = np.arange(64)[:, None]
    m2 = np.arange(128)[None, :]
    a = 2 * np.pi * kk1 * m2 / N
    T2 = np.stack([np.cos(a), np.sin(a)], axis=0)
    c["hy_T2_h"] = np.ascontiguousarray(np.tile(T2[:, :, None, :], (1, 1, 16, 1)).transpose(1, 0, 2, 3)).astype(np.float32)
    m1 = np.arange(32)[None, :]
    a = 2 * np.pi * kk1 * m1 / 64
    c["hy_W1i_h"] = (np.stack([np.cos(a), -np.sin(a)], axis=1) / N).astype(np.float32)
    t = np.arange(256)[:, None]
    k = np.arange(512)[None, :]
    a = 2 * np.pi * t * k / 512
    c["hy_Fc_h"] = np.stack([np.cos(a), -np.sin(a)], axis=1).astype(np.float32)
    a = 2 * np.pi * np.arange(512)[:, None] * np.arange(256)[None, :] / 512
    c["hy_Gc_h"] = (np.stack([np.cos(a), -np.sin(a)], axis=1) / 512).astype(np.float32)
    deltas = np.abs(np.linspace(HY_MIN_DECAY, HY_MAX_DECAY, HY_W, dtype=np.float32))
    c["_deltas"] = deltas
    return c


def prep_A_hy(inp, l, h, m):
    c = hy_consts()
    for k_, v_ in c.items():
        if not k_.startswith("_"):
            m[k_] = v_
    cw = []
    for j in range(3):
        idx = j * 256 + h * 128 + np.arange(128)
        cw.append(np.concatenate([inp["hy_conv_w"][l][:, idx].T, inp["hy_conv_b"][l][idx][:, None]], axis=1))
    m["hy_cw_h"] = _f32(np.concatenate(cw, axis=0))
    m["hy_w1_h"] = _f32(inp["hy_w1"][l])
    m["hy_w2_h"] = _f32(inp["hy_w2"][l])
    m["hy_vec_h"] = _f32(np.stack([inp["hy_b1"][l], inp["hy_b2"][l], inp["hy_freq"][l]], axis=1))
    cols = np.concatenate([(q * 256 + h * 128 + np.arange(128)) for q in range(4)])
    m["hy_w3_h"] = _f32(inp["hy_w3"][l][:, cols])
    m["hy_b3_h"] = _f32(inp["hy_b3"][l][cols].reshape(4, 128).T)
    m["hy_bias_h"] = _f32(inp["hy_bias"][l][:, h * 128:(h + 1) * 128].T)
    dl = c["_deltas"][h * 128:(h + 1) * 128]
    m["hy_delta_h"] = _f32(np.stack([-dl / np.float32(SEQ), -dl / np.float32(CTXL)], axis=1))


def _cmul(kb, ore, oim, are, aim, bre, bim, tmp, conj, r, wre, wim, wtmp):
    kb.tt("dve", ore, are, bre, ALU.mult, r=r, w=wre)
    kb.tt("dve", tmp, aim, bim, ALU.mult, r=r, w=wtmp)
    kb.tt("dve", ore, ore, tmp, ALU.add if conj else ALU.subtract, r=wre + wtmp, w=wre)
    kb.tt("dve", oim, are, bim, ALU.mult, r=r + wre + wtmp, w=wim)
    kb.tt("dve", tmp, aim, bre, ALU.mult, r=r + wre, w=wtmp)
    if conj:
        kb.tt("dve", oim, tmp, oim, ALU.subtract, r=wim + wtmp, w=wim)
    else:
        kb.tt("dve", oim, oim, tmp, ALU.add, r=wim + wtmp, w=wim)


def _phasea_hyena(self):
    kb = self.kb
    D_ = {n: kb.din(n, s) for n, s in [
        ("hy_feat_l_h", [33, SEQ]), ("hy_feat_c_h", [33, CTXL]), ("hy_t_h", [128, SEQ]), ("hy_W1f_h", [32, 128]),
        ("hy_T1_h", [128, 2, 16, 64]), ("hy_W2_h", [128, 3, 128]), ("hy_G_h", [128, 2, 256]), ("hy_T2_h", [64, 2, 16, 128]),
        ("hy_W1i_h", [64, 2, 32]), ("hy_Fc_h", [256, 2, 512]), ("hy_Gc_h", [512, 2, 256]),
        ("hy_cw_h", [384, 4]), ("hy_w1_h", [33, 64]), ("hy_w2_h", [64, 64]), ("hy_vec_h", [64, 3]), ("hy_w3_h", [64, 512]),
        ("hy_b3_h", [128, 4]), ("hy_bias_h", [128, 2]), ("hy_delta_h", [128, 2])]}
    hz = kb.dscratch("hy_hz", [3, 128, SEQ])
    hk = kb.dscratch("hy_hk", [4, 128, SEQ])
    wt = self.load_w("w_hy", 384, 384)

    def ld(name, shape, src):
        t_ = kb.sb(name, shape)
        kb.dma(t_[:], src, w=[name])
        return t_

    cw = ld("hy_cw", [128, 3, 4], D_["hy_cw_h"].rearrange("(j p) c -> p j c", p=128))
    w1 = ld("hy_w1", [33, 64], D_["hy_w1_h"][:, :])
    w2 = ld("hy_w2", [64, 64], D_["hy_w2_h"][:, :])
    vec = ld("hy_vec", [64, 3], D_["hy_vec_h"][:, :])
    w3 = ld("hy_w3", [64, 512], D_["hy_w3_h"][:, :])
    b3 = ld("hy_b3", [128, 4], D_["hy_b3_h"][:, :])
    hbias = ld("hy_bias", [128, 2], D_["hy_bias_h"][:, :])
    delta = ld("hy_delta", [128, 2], D_["hy_delta_h"][:, :])
    sm = kb.sb("hy_sm", [128, 24])
    kb.memset("dve", sm[:], 0.0, w=["hy_sm"])
    sm2 = kb.sb("hy_sm2", [128, 16])
    kb.ts("dve", sm[0:64, 0:1], vec[:, 2:3], 1.0 / TWO_PI, ALU.mult, r=["hy_vec"], w=["hy_sm"])
    psr = Rot(self.PS[0:4])

    zct = kc = None
    if self.with_ctx:
        zct = kb.sb("hy_zct", [128, 2, 3, 128])
        kc = kb.sb("hy_kc", [128, 4, CTXL])
    for j in range(3):
        for bi, blk in enumerate(BLKS):
            t0, t1 = blk
            pb, pbn = psr.next()
            self.proj(wt, "w_hy", j * 128, 128, blk, pb, pbn)
            kb.copy("act" if bi % 2 == 0 else "dve", self.F(3, t0, t1), pb[:, 0:t1 - t0], r=[pbn], w=["F3.%d" % bi])
        for (s0, s1) in [(0, CTXL), (CTXL, T)]:
            rr = self.fres(3, s0, s1)
            ww = self.fres(j, s0, s1)
            kb.ts("dve", self.F(j, s0, s1), self.F(3, s0, s1), cw[:, j, 1:2], ALU.mult, cw[:, j, 3:4], ALU.add, r=rr + ["hy_cw"], w=ww)
            kb.stt(self.F(j, s0 + 1, s1), self.F(3, s0, s1 - 1), cw[:, j, 0:1], self.F(j, s0 + 1, s1), ALU.mult, ALU.add, r=rr + ww + ["hy_cw"], w=ww)
            kb.stt(self.F(j, s0, s1 - 1), self.F(3, s0 + 1, s1), cw[:, j, 2:3], self.F(j, s0, s1 - 1), ALU.mult, ALU.add, r=rr + ww + ["hy_cw"], w=ww)
        kb.dma(hz[j], self.F(j, CTXL, T), r=self.fres(j, CTXL, T), w=["hz.%d" % j])
        if self.with_ctx:
            for tt_ in range(2):
                pb, pbn = psr.next()
                kb.tr(pb[:, 0:128], self.F(j, tt_ * 128, (tt_ + 1) * 128), self.ident[:], r=["F%d.0" % j, "ident"], w=[pbn])
                kb.copy("act", zct[:, tt_, j, :], pb[:, 0:128], r=[pbn], w=["hy_zct"])

    def sin_arg(dst, src_ps, P_, n, bcol, r, w):
        t0_, t0n = self.CP.next()
        t1_, t1n = self.CP.next()
        kb.ts("dve", t0_[0:P_, 0:n], src_ps, vec[0:P_, bcol:bcol + 1], ALU.add, sm[0:P_, 0:1], ALU.mult, r=r + ["hy_vec", "hy_sm"], w=[t0n])
        kb.ts("dve", t1_[0:P_, 0:n], t0_[0:P_, 0:n], MAGIC, ALU.add, r=[t0n], w=[t1n])
        kb.ts("dve", t1_[0:P_, 0:n], t1_[0:P_, 0:n], MAGIC, ALU.subtract, r=[t1n], w=[t1n])
        kb.tt("pool", t0_[0:P_, 0:n], t0_[0:P_, 0:n], t1_[0:P_, 0:n], ALU.subtract, r=[t0n, t1n], w=[t0n])
        kb.act(dst, t0_[0:P_, 0:n], AF.Sin, r=[t0n], w=w, scale=TWO_PI)

    cases = [("l", SEQ, 0)] + ([("c", CTXL, 1)] if self.with_ctx else [])
    for (cname, L, dcol) in cases:
        kb.S.barrier()
        nblk = max(1, L // 512)
        bw = min(L, 512)
        kb.dma(self.F(0, 0, L)[0:33], D_["hy_feat_%s_h" % cname][:, :], w=["hyF0"])
        for b_ in range(nblk):
            pb, pbn = psr.next()
            kb.mm(pb[0:64, 0:bw], w1[:, :], self.F(0, b_ * bw, (b_ + 1) * bw)[0:33], True, True, r=["hy_w1", "hyF0"], w=[pbn])
            sin_arg(self.F(1, b_ * bw, (b_ + 1) * bw)[0:64], pb[0:64, 0:bw], 64, bw, 0, [pbn], ["hyF1"])
        kb.S.barrier()
        for b_ in range(nblk):
            pb, pbn = psr.next()
            kb.mm(pb[0:64, 0:bw], w2[:, :], self.F(1, b_ * bw, (b_ + 1) * bw)[0:64], True, True, r=["hy_w2", "hyF1"], w=[pbn])
            sin_arg(self.F(0, b_ * bw, (b_ + 1) * bw)[0:64], pb[0:64, 0:bw], 64, bw, 1, [pbn], ["hyH2"])
        kb.S.barrier()
        kb.dma(self.F(3, 0, L), D_["hy_t_h"][:, 0:L], w=["hyF3"])
        kb.act(self.F(1, 0, L), self.F(3, 0, L), AF.Exp, r=["hyF3", "hy_delta"], w=["hyWin"], scale=delta[:, dcol:dcol + 1])
        kb.S.barrier()
        for o in range(2):
            for d in range(2):
                q = d * 2 + o
                for b_ in range(nblk):
                    pb, pbn = psr.next()
                    kb.mm(pb[:, 0:bw], w3[:, q * 128:(q + 1) * 128], self.F(0, b_ * bw, (b_ + 1) * bw)[0:64], True, True,
                          r=["hy_w3", "hyH2"], w=[pbn])
                    kb.stt(self.F(2 + d, b_ * bw, (b_ + 1) * bw), pb[:, 0:bw], b3[:, q:q + 1], self.F(1, b_ * bw, (b_ + 1) * bw),
                           ALU.add, ALU.mult, r=[pbn, "hy_b3", "hyWin"], w=["hyK%d" % d])
            kb.memset("dve", self.F(3, 0, 1), 0.0, r=["hyK1"], w=["hyK1"])
            for d in range(2):
                for b_ in range(nblk):
                    tb, tbn = self.CP.next()
                    xin_ = self.F(2 + d, b_ * bw, (b_ + 1) * bw)
                    kb.stt(tb[:, 0:bw], xin_, -1.0, xin_, ALU.mult, ALU.max, r=["hyK%d" % d], w=[tbn])
                    kb.S.op("dve", lambda e, d=d, b_=b_, tb=tb, bw=bw: e.tensor_reduce(out=sm[:, 8 + d * 8 + b_:9 + d * 8 + b_], in_=tb[:, 0:bw], axis=mybir.AxisListType.X, op=ALU.add),
                            reads=[tbn], writes=["hy_sm"])
            if nblk == 8:
                kb.tt("dve", sm2[:, 0:8], sm[:, 8:16], sm[:, 16:24], ALU.add, r=["hy_sm"], w=["hy_sm2"])
                kb.tt("dve", sm2[:, 8:12], sm2[:, 0:4], sm2[:, 4:8], ALU.add, r=["hy_sm2"], w=["hy_sm2"])
                kb.tt("dve", sm2[:, 12:14], sm2[:, 8:10], sm2[:, 10:12], ALU.add, r=["hy_sm2"], w=["hy_sm2"])
                kb.tt("dve", sm[:, 4:5], sm2[:, 12:13], sm2[:, 13:14], ALU.add, r=["hy_sm", "hy_sm2"], w=["hy_sm"])
            else:
                kb.tt("dve", sm[:, 4:5], sm[:, 8:9], sm[:, 16:17], ALU.add, r=["hy_sm"], w=["hy_sm"])
            if self.dbg and cname == "l":
                if o == 0:
                    self.dbg_sm = kb.dout("dbg_sm", [2, 128, 24])
                kb.dma(self.dbg_sm[o], sm[:, :], r=["hy_sm"], w=["dbg_sm%d" % o], final=True)
            kb.ts("dve", sm[:, 4:5], sm[:, 4:5], 1e-6, ALU.add, r=["hy_sm"], w=["hy_sm"])
            kb.recip(sm[:, 4:5], sm[:, 4:5], r=["hy_sm"], w=["hy_sm"])
            for d in range(2):
                kb.ts("dve" if d == 0 else "pool", self.F(2 + d, 0, L), self.F(2 + d, 0, L), sm[:, 4:5], ALU.mult, r=["hyK%d" % d, "hy_sm"], w=["hyK%d" % d])
            kb.tt("dve", self.F(2, 0, 1), self.F(2, 0, 1), hbias[:, o:o + 1], ALU.add, r=["hyK0", "hy_bias"], w=["hyK0"])
            for d in range(2):
                if cname == "l":
                    kb.dma(hk[o * 2 + d], self.F(2 + d, 0, L), r=["hyK%d" % d], w=["hk.%d" % (o * 2 + d)])
                else:
                    kb.copy("act", kc[:, o * 2 + d, :], self.F(2 + d, 0, L), r=["hyK%d" % d], w=["hy_kc"])
    kb.S.barrier()
    if self.dbg:
        return hz, hk, zct, kc, D_
    _hyena_conv(self, hz, hk, zct, kc, D_)


PhaseA.hyena = _phasea_hyena


def _hyena_conv(self, hz, hk, zct, kc, D_):
    kb = self.kb

    def ld(name, shape, src):
        t_ = kb.sb(name, shape)
        kb.dma(t_[:], src, w=[name])
        return t_

    W1f = ld("hy_W1f", [32, 128], D_["hy_W1f_h"][:, :])
    T1 = ld("hy_T1", [128, 2, 4, 64], D_["hy_T1_h"][:, :, 0:4, :])
    W2 = ld("hy_W2", [128, 3, 128], D_["hy_W2_h"][:, :, :])
    G = ld("hy_G", [128, 2, 256], D_["hy_G_h"][:, :, :])
    T2 = ld("hy_T2", [64, 2, 2, 128], D_["hy_T2_h"][:, :, 0:2, :])
    W1i = ld("hy_W1i", [64, 2, 32], D_["hy_W1i_h"][:, :, :])
    CONST = ["hy_W1f", "hy_T1", "hy_W2", "hy_G", "hy_T2", "hy_W1i"]
    pall = Rot(self.PS)
    off = [0]

    def carve(n):
        a = off[0]
        off[0] += n
        assert off[0] <= 4 * T
        return a

    def v3(a, P_, s, k):
        return self.FP[0:P_, a:a + s * k].rearrange("p (s k) -> p s k", k=k)

    srcs = Rot([(v3(carve(2048), 32, 16, 128), "hy_src%d" % i) for i in range(4)])
    AR = v3(carve(1024), 128, 16, 64)
    AI = v3(carve(1024), 128, 16, 64)
    KR = [v3(carve(1024), 128, 16, 64) for _ in range(2)]
    KI = [v3(carve(1024), 128, 16, 64) for _ in range(2)]
    XR = v3(carve(1024), 128, 16, 64)
    XI = v3(carve(1024), 128, 16, 64)
    tmpA = v3(carve(256), 128, 4, 64)
    tmpX = v3(carve(512), 128, 8, 64)
    Bb = Rot([(kb.sb("hy_Bb%d" % i, [64, 2, 4, 128]), "hy_Bb%d" % i) for i in range(1)])
    tmpB = kb.sb("hy_tmpB", [64, 2, 128])
    o32 = Rot([(kb.sb("hy_o32_%d" % i, [32, 4, 128]), "hy_o32_%d" % i) for i in range(1)])
    o16 = Rot([(kb.sb("hy_o16_%d" % i, [32, 16, 128], BF16), "hy_o16_%d" % i) for i in range(1)])

    def fft_fwd(src, srcn, consume):
        for sb4 in range(4):
            pA, pAn = pall.next()
            for s in range(4):
                kb.mm(pA[:, s * 128:(s + 1) * 128], src[:, sb4 * 4 + s, :], W1f[:, :], True, True, r=[srcn, "hy_W1f"], w=[pAn])
            v = pA[:, :].rearrange("p (s c k) -> p s c k", s=4, c=2)
            _cmul(kb, AR[:, sb4 * 4:(sb4 + 1) * 4, :], AI[:, sb4 * 4:(sb4 + 1) * 4, :], v[:, :, 0, :], v[:, :, 1, :],
                  T1[:, 0, :, :], T1[:, 1, :, :], tmpA, False, [pAn, "hy_T1"], ["hyAR"], ["hyAI"], ["hy_tmpA"])
        for b8 in range(2):
            pXr, pXrn = pall.next()
            pXi, pXin = pall.next()
            a_re = AR[:, b8 * 8:(b8 + 1) * 8, :].rearrange("p s k -> p (s k)")
            a_im = AI[:, b8 * 8:(b8 + 1) * 8, :].rearrange("p s k -> p (s k)")
            kb.mm(pXr[:, :], W2[:, 0, :], a_re, True, False, r=["hy_W2", "hyAR"], w=[pXrn])
            kb.mm(pXr[:, :], W2[:, 2, :], a_im, False, True, r=["hy_W2", "hyAI"], w=[pXrn])
            kb.mm(pXi[:, :], W2[:, 1, :], a_re, True, False, r=["hy_W2", "hyAR"], w=[pXin])
            kb.mm(pXi[:, :], W2[:, 0, :], a_im, False, True, r=["hy_W2", "hyAI"], w=[pXin])
            consume(b8, pXr[:, :].rearrange("p (s k) -> p s k", k=64), pXi[:, :].rearrange("p (s k) -> p s k", k=64), [pXrn, pXin])

    def ifft(consume):
        for sb4 in range(4):
            bb, bbn = Bb.next()
            for s2 in range(2):
                pB, pBn = pall.next()
                for s in range(2):
                    sig = sb4 * 4 + s2 * 2 + s
                    kb.mm(pB[0:64, s * 256:(s + 1) * 256], XR[:, sig, :], G[:, 0, :], True, False, r=["hyXR", "hy_G"], w=[pBn])
                    kb.mm(pB[0:64, s * 256:(s + 1) * 256], XI[:, sig, :], G[:, 1, :], False, True, r=["hyXI", "hy_G"], w=[pBn])
                v = pB[0:64, :].rearrange("p (s c m) -> p s c m", s=2, c=2)
                _cmul(kb, bb[:, 0, s2 * 2:(s2 + 1) * 2, :], bb[:, 1, s2 * 2:(s2 + 1) * 2, :], v[:, :, 0, :], v[:, :, 1, :],
                      T2[:, 0, :, :], T2[:, 1, :, :], tmpB[:, :, :], False, [pBn, "hy_T2"], [bbn], [bbn], ["hy_tmpB"])
            pY, pYn = pall.next()
            kb.mm(pY[0:32, :], W1i[:, 0, :], bb[:, 0, :, :].rearrange("p s m -> p (s m)"), True, False, r=["hy_W1i", bbn], w=[pYn])
            kb.mm(pY[0:32, :], W1i[:, 1, :], bb[:, 1, :, :].rearrange("p s m -> p (s m)"), False, True, r=["hy_W1i", bbn], w=[pYn])
            consume(sb4, pY[0:32, :].rearrange("p (s m) -> p s m", m=128), pYn)

    def load_src(dram_rows):
        s_, sn = srcs.next()
        kb.dma(s_, dram_rows.rearrange("c (a b) -> a c b", b=128), r=[], w=[sn])
        return s_, sn

    for g in range(8):
        c0 = g * 16
        for o in range(2):
            for d in range(2):
                s_, sn = srcs.next()
                kb.dma(s_, hk[o * 2 + d, c0:c0 + 16, :].rearrange("c (a b) -> a c b", b=128), r=["hk.%d" % (o * 2 + d)], w=[sn])

                def cons_k(b8, xr, xi, names, o=o, d=d):
                    sl = slice(b8 * 8, (b8 + 1) * 8)
                    if d == 0:
                        kb.copy("act", KR[o][:, sl, :], xr, r=names, w=["hyKR%d" % o])
                        kb.copy("act", KI[o][:, sl, :], xi, r=names, w=["hyKI%d" % o])
                    else:
                        kb.tt("dve", KR[o][:, sl, :], xr, KR[o][:, sl, :], ALU.add, r=names + ["hyKR%d" % o], w=["hyKR%d" % o])
                        kb.tt("dve", KI[o][:, sl, :], KI[o][:, sl, :], xi, ALU.subtract, r=names + ["hyKI%d" % o], w=["hyKI%d" % o])

                fft_fwd(s_, sn, cons_k)
        cur, curn = srcs.next()
        kb.dma(cur, hz[0, c0:c0 + 16, :].rearrange("c (a b) -> a c b", b=128), r=["hz.0"], w=[curn])
        for o in range(2):
            def cons_x(b8, xr, xi, names, o=o):
                sl = slice(b8 * 8, (b8 + 1) * 8)
                _cmul(kb, XR[:, sl, :], XI[:, sl, :], xr, xi, KR[o][:, sl, :], KI[o][:, sl, :], tmpX, False,
                      names + ["hyKR%d" % o, "hyKI%d" % o], ["hyXR"], ["hyXI"], ["hy_tmpX"])

            fft_fwd(cur, curn, cons_x)
            gt, gtn = srcs.next()
            kb.dma(gt, hz[1 + o, c0:c0 + 16, :].rearrange("c (a b) -> a c b", b=128), r=["hz.%d" % (1 + o)], w=[gtn])
            if o == 0:
                nxt, nxtn = srcs.next()

                def cons_y(sb4, py, pyn, gt=gt, gtn=gtn, nxt=nxt, nxtn=nxtn):
                    sl = slice(sb4 * 4, (sb4 + 1) * 4)
                    kb.tt("dve", nxt[:, sl, :], py, gt[:, sl, :], ALU.mult, r=[pyn, gtn], w=[nxtn])

                ifft(cons_y)
                cur, curn = nxt, nxtn
            else:
                ob, obn = o16.next()

                def cons_o(sb4, py, pyn, gt=gt, gtn=gtn, ob=ob, obn=obn):
                    sl = slice(sb4 * 4, (sb4 + 1) * 4)
                    t32, t32n = o32.next()
                    kb.tt("dve", t32[:, :, :], py, gt[:, sl, :], ALU.mult, r=[pyn, gtn], w=[t32n])
                    kb.copy("act", ob[:, sl, :], t32[:, :, :], r=[t32n], w=[obn])

                ifft(cons_o)
                kb.dma(self.ybuf[192 + c0:192 + c0 + 16, CTXL:T].rearrange("c (a b) -> a c b", b=128), ob[:, :, :], r=[obn], final=True)
    kb.S.barrier()
    if self.with_ctx:
        _hyena_ctx(self, zct, kc, D_)


def _hyena_ctx(self, zct, kc, D_):
    kb = self.kb
    pall = Rot(self.PS)
    Fc = self.FP[:, 0:2048].rearrange("p (t c k) -> p t c k", t=2, c=2)
    Gc = self.FP[:, 2048:4096].rearrange("p (q c t) -> p q c t", q=4, c=2)
    kb.dma(Fc, D_["hy_Fc_h"].rearrange("(t p) c k -> p t c k", p=128), w=["hyFc"])
    kb.dma(Gc, D_["hy_Gc_h"].rearrange("(q p) c t -> p q c t", p=128), w=["hyGc"])
    kct = self.FP[:, 4096:5120].rearrange("p (t q c) -> p t q c", t=2, q=4)
    KcR = [self.FP[:, 5120 + i * 512:5632 + i * 512].rearrange("p (q c) -> p q c", q=4) for i in range(2)]
    KcI = [self.FP[:, 6144 + i * 512:6656 + i * 512].rearrange("p (q c) -> p q c", q=4) for i in range(2)]
    YR = self.FP[:, 7168:7680].rearrange("p (q c) -> p q c", q=4)
    YI = self.FP[:, 7680:8192].rearrange("p (q c) -> p q c", q=4)
    tmp = self.FP[:, 8192:8704].rearrange("p (q c) -> p q c", q=4)
    y1 = self.FP[:, 8704:8960].rearrange("p (t c) -> p t c", t=2)
    yo = self.FP[:, 8960:9216].rearrange("p (t c) -> p t c", t=2)
    for q in range(4):
        for tt_ in range(2):
            pb, pbn = pall.next()
            kb.tr(pb[:, 0:128], kc[:, q, tt_ * 128:(tt_ + 1) * 128], self.ident[:], r=["hy_kc", "ident"], w=[pbn])
            kb.copy("act", kct[:, tt_, q, :], pb[:, 0:128], r=[pbn], w=["hy_kct"])

    def fwd(xfn, xres):
        pr, prn = pall.next()
        pi, pin = pall.next()
        for c_, (pp, ppn) in enumerate([(pr, prn), (pi, pin)]):
            for kq in range(4):
                for tt_ in range(2):
                    kb.mm(pp[:, kq * 128:(kq + 1) * 128], Fc[:, tt_, c_, kq * 128:(kq + 1) * 128], xfn(tt_), tt_ == 0, tt_ == 1,
                          r=["hyFc"] + xres, w=[ppn])
        return (pr[:, :].rearrange("p (q c) -> p q c", q=4), prn), (pi[:, :].rearrange("p (q c) -> p q c", q=4), pin)

    def inv(consume):
        pY, pYn = pall.next()
        for tt_ in range(2):
            for kq in range(4):
                kb.mm(pY[:, tt_ * 128:(tt_ + 1) * 128], Gc[:, kq, 0, tt_ * 128:(tt_ + 1) * 128], YR[:, kq, :], kq == 0, False, r=["hyGc", "hyYR"], w=[pYn])
                kb.mm(pY[:, tt_ * 128:(tt_ + 1) * 128], Gc[:, kq, 1, tt_ * 128:(tt_ + 1) * 128], YI[:, kq, :], False, kq == 3, r=["hyGc", "hyYI"], w=[pYn])
        consume(pY[:, 0:256].rearrange("p (t c) -> p t c", t=2), pYn)

    for o in range(2):
        (xr, xrn), (xi, xin) = fwd(lambda tt_, o=o: kct[:, tt_, o * 2 + 0, :], ["hy_kct"])
        kb.copy("act", KcR[o], xr, r=[xrn], w=["hyKcR%d" % o])
        kb.copy("act", KcI[o], xi, r=[xin], w=["hyKcI%d" % o])
        (xr, xrn), (xi, xin) = fwd(lambda tt_, o=o: kct[:, tt_, o * 2 + 1, :], ["hy_kct"])
        kb.tt("dve", KcR[o], xr, KcR[o], ALU.add, r=[xrn, "hyKcR%d" % o], w=["hyKcR%d" % o])
        kb.tt("dve", KcI[o], KcI[o], xi, ALU.subtract, r=[xin, "hyKcI%d" % o], w=["hyKcI%d" % o])
    cur = lambda tt_: zct[:, tt_, 0, :]
    cres = ["hy_zct"]
    for o in range(2):
        (xr, xrn), (xi, xin) = fwd(cur, cres)
        _cmul(kb, YR, YI, xr, xi, KcR[o], KcI[o], tmp, False, [xrn, xin, "hyKcR%d" % o, "hyKcI%d" % o], ["hyYR"], ["hyYI"], ["hyTmpc"])
        if o == 0:
            inv(lambda py, pyn: kb.tt("dve", y1, py, zct[:, :, 1, :], ALU.mult, r=[pyn, "hy_zct"], w=["hyY1"]))
            cur = lambda tt_: y1[:, tt_, :]
            cres = ["hyY1"]
        else:
            inv(lambda py, pyn: kb.tt("dve", yo, py, zct[:, :, 2, :], ALU.mult, r=[pyn, "hy_zct"], w=["hyYo"]))
    ob, obn = self.ost.next()
    for tt_ in range(2):
        pb, pbn = pall.next()
        kb.tr(pb[:, 0:128], yo[:, tt_, :], self.ident[:], r=["hyYo", "ident"], w=[pbn])
        kb.copy("act", ob[:, tt_ * 128:(tt_ + 1) * 128], pb[:, 0:128], r=[pbn], w=[obn])
    kb.dma(self.ybuf[192:320, 0:CTXL], ob[:, 0:CTXL], r=[obn], final=True)


class PhaseB:
    def __init__(self, layer, dbg=False, kb=None, xtok=None, yload=None, xout=None, xtok_fn=None, xout_fn=None, xload=None, modT=None):
        self.layer = layer
        self.with_ctx = layer == 0
        self.moe = layer % 2 == 1
        self.standalone = kb is None
        kb = self.kb = kb if kb is not None else KB()
        NTB = self.NTB = 17 if self.with_ctx else 16
        NTOK = self.NTOK = NTB * 128
        if xtok_fn is None and xload is None:
            self.xtok = xtok if xtok is not None else kb.din("xtok", [NTOK, D])
            xtok_fn = lambda i: self.xtok[i * 128:(i + 1) * 128, :]
        self.xtok_fn = xtok_fn
        self.xload = xload
        self.yload = yload
        if yload is None:
            self.yT = kb.din("yT", [1280, NTOK], BF16)
        if modT is None:
            self.cvT = kb.din("cvT", [D, 2])
            self.wmod = kb.din("wmodB", [D, 6144])
            self.bmodT = kb.din("bmodT", [128, 48])
        self.ident_d = kb.din("ident_d", [128, 128])
        self.w_g_d = kb.din("w_g_h", [D, 4096])
        self.w_br_d = kb.din("w_br_h", [14, 128, D])
        self.w_out_d = kb.din("w_out_h", [D, D])
        self.wglu_d = kb.din("s5_wglu_h", [256, 256])
        self.bglu_d = kb.din("s5_bglu_h", [128, 2])
        self.lnp_d = kb.din("lnp_h", [4, 128, D])
        if xout_fn is None:
            self.xout = xout if xout is not None else kb.dout("xout", [NTOK, D])
            xout_fn = lambda i: self.xout[i * 128:(i + 1) * 128, :]
        self.xout_fn = xout_fn
        self.x1buf = kb.dscratch("x1buf%d" % kb.__dict__.setdefault("_nb", 0), [NTOK, D])
        self.u2T_d = kb.dscratch("u2T_d%d" % kb.__dict__["_nb"], [128, 8, NTOK], BF16)
        kb.__dict__["_nb"] += 1
        self.ident = kb.sb("ident", [128, 128])
        kb.dma(self.ident[:], self.ident_d[:, :], w=["ident"])
        self.identb = kb.sb("identb", [128, 128], BF16)
        kb.copy("dve", self.identb[:], self.ident[:], r=["ident"], w=["identb"])
        self.PS = [(kb.ps("PS%d" % i), "PS%d" % i) for i in range(7)]
        self.pT = kb.ps("PSTb", [128, 1024], BF16)
        self.pT32 = self.pT[:, :].bitcast(F32)
        self.modT = modT if modT is not None else kb.sb("modT", [128, 48, 2])
        if self.moe:
            self.gates = kb.sb("gates", [128, NTB, NEXP])
        if modT is None:
            with kb.scope():
                wmb = Rot([(kb.sb("wmb%d" % i, [128, 8, 256]), "wmb%d" % i) for i in range(2)])
                emit_modT(kb, self.wmod, self.bmodT, self.cvT, 48, self.modT, self.PS[6][0], "PS6", wmb)
                for c0 in (8, 32):
                    kb.ts("dve", self.modT[:, c0:c0 + 8, :], self.modT[:, c0:c0 + 8, :], 1.0, ALU.add, r=["modT"], w=["modT"])
        with kb.scope():
            self.load_gl(0)
            self.mix()
        with kb.scope():
            self.u2T = kb.sb("u2T", [128, 8, NTOK], BF16)
            for i in range(NTB):
                kb.dma(self.u2T[:, :, i * 128:(i + 1) * 128], self.u2T_d[:, :, i * 128:(i + 1) * 128], r=["u2Td.%d" % i], w=["u2T.%d" % i])
            self.load_gl(1)
            if self.moe:
                self.ffn_moe()
            else:
                self.ffn_dense()
        if self.standalone:
            self.nc = kb.finish()

    def load_gl(self, which):
        kb = self.kb
        sfx = "_%d" % which
        self.gbc = kb.sb("gbc" + sfx, [128, 2, D])
        self.gbcn = "gbc" + sfx
        self.lnp = kb.sb("lnp" + sfx, [128, 2, D])
        self.lnpn = "lnp" + sfx
        kb.dma(self.lnp[:], self.lnp_d[which * 2:which * 2 + 2].rearrange("a p d -> p a d"), w=[self.lnpn])
        cbase = (16, 40)[which]
        with kb.scope():
            ones = kb.sb("ones_f" + sfx, [128, 128])
            kb.memset("dve", ones[:], 1.0, w=["ones_f"])
            gb = Rot([(kb.sb("gb%d" % i + sfx, [128, 128]), "gb%d" % i) for i in range(2)])
            for j in range(2):
                for half in range(2):
                    pb, pbn = self.PS[half]
                    for kk in range(4):
                        k = half * 4 + kk
                        g_, gn = gb.next()
                        kb.act(g_[:], ones[:], AF.Copy, r=["ones_f", "modT"], w=[gn], scale=self.modT[:, cbase + k, j:j + 1])
                        kb.mm(pb[:, kk * 128:(kk + 1) * 128], g_[:], self.ident[:], True, True, r=[gn, "ident"], w=[pbn])
                    kb.copy("act", self.gbc[:, j, half * 512:(half + 1) * 512], pb[:, :], r=[pbn], w=[self.gbcn])

    def jof(self, i):
        return 1 if (self.with_ctx and i == 0) else 0

    def mix(self):
        kb = self.kb
        NTB = self.NTB
        w_g = kb.sb("w_g", [128, 8, 4096], BF16)
        for nb in range(8):
            kb.dma_cast(w_g[:, :, nb * 512:(nb + 1) * 512], self.w_g_d[:, nb * 512:(nb + 1) * 512].rearrange("(k p) n -> p k n", p=128), w=["w_g.%d" % nb])
        w_br = kb.sb("w_br", [128, 14, D], BF16)
        kb.dma_cast(w_br[:], self.w_br_d.rearrange("c p n -> p c n"), w=["w_br"])
        w_out = kb.sb("w_out", [128, 8, D], BF16)
        kb.dma_cast(w_out[:], self.w_out_d.rearrange("(k p) n -> p k n", p=128), w=["w_out"])
        wglu = kb.sb("wglu", [128, 2, 256], BF16)
        kb.dma_cast(wglu[:], self.wglu_d.rearrange("(k p) n -> p k n", p=128), w=["wglu"])
        bglu = kb.sb("bglu", [128, 2])
        kb.dma(bglu[:], self.bglu_d[:, :], w=["bglu"])
        lnt = LNT(kb, self.ident, [self.PS[0], self.PS[1]], xn_bufs=1)
        if self.moe:
            _pb_router_setup(self)
        uTs = Rot([(kb.sb("uTt%d" % i, [128, 8, 128], BF16), "uTt%d" % i) for i in range(1)])
        u2ts = Rot([(kb.sb("u2t%d" % i, [128, 8, 128], BF16), "u2t%d" % i) for i in range(1)])
        Gs = Rot([(kb.sb("G%d" % i, [128, 4096], BF16), "G%d" % i) for i in range(1)])
        yts = Rot([(kb.sb("yt%d" % i, [128, 10, 128], BF16), "yt%d" % i) for i in range(1)])
        if self.xload is not None:
            self.ybl = Rot([(kb.sb("ybl%d" % i, [128, 10, 128], BF16), "ybl%d" % i) for i in range(2)])
        yds = Rot([(kb.sb("yd%d" % i, [128, 2, 128], BF16), "yd%d" % i) for i in range(1)])
        mgs = Rot([(kb.sb("mg%d" % i, [128, D]), "mg%d" % i) for i in range(1)])
        mgb = Rot([(kb.sb("mgb%d" % i, [128, D], BF16), "mgb%d" % i) for i in range(1)])
        mTs = Rot([(kb.sb("mT%d" % i, [128, 8, 128], BF16), "mT%d" % i) for i in range(1)])
        tmps = Rot([(kb.sb("tmpB%d" % i, [128, 512]), "tmpB%d" % i) for i in range(2)])
        x1s = Rot([(kb.sb("x1t%d" % i, [128, D]), "x1t%d" % i) for i in range(1)])
        pT = self.pT
        pg = Rot(self.PS[2:5])
        po = Rot(self.PS[5:7])
        BRP = [[(0, 0), (1, 1), (2, 5), (3, 6)],
               [(4, 1), (5, 2), (6, 6), (7, 7)],
               [(8, 2), (9, 3), (10, 7), (11, 8)],
               [(12, 4), (13, 9)]]
        for i in range(NTB):
            j = self.jof(i)
            uT, uTn = uTs.next()
            if self.xload is not None:
                xg_ = self.xload(self, i, lnt)
                xt, xtn, _, _ = lnt.run(None, [], lambda k, uT=uT: uT[:, k, :], [uTn],
                                        lambda k, j=j: self.modT[:, 8 + k, j:j + 1], lambda k, j=j: self.modT[:, k, j:j + 1], xt_given=xg_)
            else:
                xt, xtn, _, _ = lnt.run(self.xtok_fn(i), ["xsrc"], lambda k, uT=uT: uT[:, k, :], [uTn],
                                        lambda k, j=j: self.modT[:, 8 + k, j:j + 1], lambda k, j=j: self.modT[:, k, j:j + 1])
            xtn = list(xtn) if isinstance(xtn, (list, tuple)) else [xtn]
            G, Gn = Gs.next()
            for nb in range(8):
                pb, pbn = pg.next()
                for k in range(8):
                    kb.mm(pb[:, :], uT[:, k, :], w_g[:, k, nb * 512:(nb + 1) * 512], k == 0, k == 7, r=[uTn, "w_g.%d" % nb], w=[pbn])
                kb.act(G[:, nb * 512:(nb + 1) * 512], pb[:, :], AF.Sigmoid, r=[pbn], w=[Gn])
            yt, ytn = yts.next()
            if self.yload is not None:
                self.yload(self, i, yt, ytn)
            else:
                kb.dma(yt[:], self.yT[:, i * 128:(i + 1) * 128].rearrange("(c p) t -> p c t", p=128), w=[ytn])
            yd, ydn = yds.next()
            for oc in range(2):
                pb, pbn = pg.next()
                for k in range(2):
                    kb.mm(pb[:, 0:128], wglu[:, k, oc * 128:(oc + 1) * 128], yt[:, 4 + 5 * k, :], k == 0, k == 1, r=["wglu", ytn], w=[pbn])
                tb, tbn = tmps.next()
                kb.act(tb[:, 0:128], pb[:, 0:128], AF.Sigmoid, r=[pbn, "bglu"], w=[tbn], bias=bglu[:, oc:oc + 1])
                kb.tt("pool", yd[:, oc, :], tb[:, 0:128], yt[:, 4 + 5 * oc, :], ALU.mult, r=[tbn, ytn], w=[ydn])
            mg, mgn = mgs.next()
            for bi_, pieces in enumerate(BRP):
                for nb in range(2):
                    pb, pbn = pg.next()
                    for cc, (pc, ch) in enumerate(pieces):
                        lhs = yd[:, ch // 5, :] if bi_ == 3 else yt[:, ch, :]
                        kb.mm(pb[:, :], lhs, w_br[:, pc, nb * 512:(nb + 1) * 512], cc == 0, cc == len(pieces) - 1,
                              r=[ydn if bi_ == 3 else ytn, "w_br"], w=[pbn])
                    gsl = G[:, bi_ * 1024 + nb * 512:bi_ * 1024 + (nb + 1) * 512]
                    if bi_ == 0:
                        kb.tt("dve", mg[:, nb * 512:(nb + 1) * 512], pb[:, :], gsl, ALU.mult, r=[pbn, Gn], w=[mgn + ".%d" % nb])
                    else:
                        tb, tbn = tmps.next()
                        kb.tt("dve", tb[:, :], pb[:, :], gsl, ALU.mult, r=[pbn, Gn], w=[tbn])
                        kb.tt("dve", mg[:, nb * 512:(nb + 1) * 512], mg[:, nb * 512:(nb + 1) * 512], tb[:, :], ALU.add,
                              r=[tbn, mgn + ".%d" % nb], w=[mgn + ".%d" % nb])
            mb, mbn = mgb.next()
            kb.copy("act", mb[:], mg[:], r=[mgn + ".0", mgn + ".1"], w=[mbn])
            mT, mTn = mTs.next()
            for k in range(8):
                kb.tr(pT[:, k * 128:(k + 1) * 128], mb[:, k * 128:(k + 1) * 128], self.identb[:], r=[mbn, "identb"], w=["PSTb"])
            kb.copy("dve", mT[:].rearrange("p k t -> p (k t)"), pT[:, :], r=["PSTb"], w=[mTn])
            x1, x1n = x1s.next()
            for nb in range(2):
                pb, pbn = po.next()
                for k in range(8):
                    kb.mm(pb[:, :], mT[:, k, :], w_out[:, k, nb * 512:(nb + 1) * 512], k == 0, k == 7, r=[mTn, "w_out"], w=[pbn])
                sl = slice(nb * 512, (nb + 1) * 512)
                kb.tt("dve", x1[:, sl], pb[:, :], self.gbc[:, j, sl], ALU.mult, r=[pbn, self.gbcn], w=[x1n + ".%d" % nb])
                kb.stt(x1[:, sl], xt[:, sl], ALPHA, x1[:, sl], ALU.mult, ALU.add, r=xtn + [x1n + ".%d" % nb], w=[x1n + ".%d" % nb])
            x1res = [x1n + ".0", x1n + ".1"]
            self.ln_affine(lnt, x1, x1res, 0)
            kb.dma(self.x1buf[i * 128:(i + 1) * 128, :], x1[:], r=x1res, w=["x1buf.%d" % i])
            d32 = d32r = None
            if self.moe and not _os.environ.get("B1_NO32"):
                u32, u32n = self.u32.next()
                self.cur_u32 = (u32, u32n)
                d32, d32r = (lambda k, u32=u32: u32[:, k, :]), [u32n]
            u2t, u2tn = u2ts.next()
            lnt.run(None, [], lambda k, u2t=u2t: u2t[:, k, :], [u2tn],
                    lambda k, j=j: self.modT[:, 32 + k, j:j + 1], lambda k, j=j: self.modT[:, 24 + k, j:j + 1], xt_given=(x1, x1res),
                    dst32_fn=d32, dst32_res=d32r)
            kb.dma(self.u2T_d[:, :, i * 128:(i + 1) * 128], u2t[:], r=[u2tn], w=["u2Td.%d" % i])
            if self.moe and not _os.environ.get("B1_NOROUTER"):
                self.router(i, lnt)

    def ln_affine(self, lnt, x, xres, which):
        kb = self.kb
        st, stn = lnt.stats_multi(x, xres)
        kb.ts("dve", x[:], x[:], st[:, 12:13], ALU.subtract, st[:, 14:15], ALU.mult, r=xres + [stn], w=xres)
        kb.tt("pool", x[:], x[:], self.lnp[:, 0, :], ALU.mult, r=xres + [self.lnpn], w=xres)
        kb.tt("dve", x[:], x[:], self.lnp[:, 1, :], ALU.add, r=xres + [self.lnpn], w=xres)


def _pb_router_setup(self):
    kb = self.kb
    self.wr_d = kb.din("moe_router_h", [D, NEXP])
    self.wr = kb.sb("wr", [128, 8, NEXP])
    kb.dma(self.wr[:], self.wr_d.rearrange("(k p) e -> p k e", p=128), w=["wr"])
    self.u32 = Rot([(kb.sb("u32_%d" % i, [128, 8, 128]), "u32_%d" % i) for i in range(1)])
    self.rt = kb.sb("rt", [128, 40])


def _pb_router(self, i):
    kb = self.kb
    u32, u32n = self.cur_u32
    pb, pbn = self.PS[2]
    for k in range(8):
        kb.mm(pb[:, 0:NEXP], u32[:, k, :], self.wr[:, k, :], k == 0, k == 7, r=[u32n, "wr"], w=[pbn])
    rt = self.rt
    R_ = ["rt"]
    lg, m1, mk1, lg2, m2, mk2, e1, w1, w2 = (rt[:, 0:8], rt[:, 8:9], rt[:, 9:17], rt[:, 17:25], rt[:, 25:26], rt[:, 26:34],
                                               rt[:, 34:35], rt[:, 35:36], rt[:, 36:37])
    kb.copy("dve", lg, pb[:, 0:NEXP], r=[pbn], w=R_)
    kb.S.op("dve", lambda e: e.tensor_reduce(out=m1, in_=lg, axis=mybir.AxisListType.X, op=ALU.max), reads=R_, writes=R_)
    kb.ts("dve", mk1, lg, m1, ALU.is_equal, r=R_, w=R_)
    kb.stt(lg2, mk1, -1e30, lg, ALU.mult, ALU.add, r=R_, w=R_)
    kb.S.op("dve", lambda e: e.tensor_reduce(out=m2, in_=lg2, axis=mybir.AxisListType.X, op=ALU.max), reads=R_, writes=R_)
    kb.ts("dve", mk2, lg2, m2, ALU.is_equal, r=R_, w=R_)
    kb.tt("dve", e1, m2, m1, ALU.subtract, r=R_, w=R_)
    kb.act(e1, e1, AF.Exp, r=R_, w=R_)
    kb.ts("dve", w1, e1, 1.0, ALU.add, r=R_, w=R_)
    kb.recip(w1, w1, r=R_, w=R_)
    kb.tt("dve", w2, e1, w1, ALU.mult, r=R_, w=R_)
    g = self.gates[:, i, :]
    kb.ts("dve", g, mk1, w1, ALU.mult, r=R_, w=["gates.%d" % i])
    kb.stt(g, mk2, w2, g, ALU.mult, ALU.add, r=R_ + ["gates.%d" % i], w=["gates.%d" % i])


def _pb_final(self, lnt, i, ffn_ap, ffn_res, x1s, tmps):
    kb = self.kb
    j = self.jof(i)
    x1, x1n = x1s.next()
    kb.dma(x1[:], self.x1buf[i * 128:(i + 1) * 128, :], r=["x1buf.%d" % i], w=[x1n])
    for nb in range(2):
        sl = slice(nb * 512, (nb + 1) * 512)
        tb, tbn = tmps.next()
        kb.tt("dve", tb[:, :], ffn_ap(nb), self.gbc[:, j, sl], ALU.mult, r=ffn_res(nb) + [self.gbcn], w=[tbn])
        kb.stt(x1[:, sl], x1[:, sl], ALPHA, tb[:, :], ALU.mult, ALU.add, r=[tbn, x1n], w=[x1n])
    self.ln_affine(lnt, x1, [x1n], 1)
    kb.dma(self.xout_fn(i), x1[:], r=[x1n], w=["xout.%d" % i], final=True)


def _pb_ffn_dense(self):
    kb = self.kb
    NTB = self.NTB
    wg_d = kb.din("ff_w_gate", [D, FF_DENSE])
    wu_d = kb.din("ff_w_up", [D, FF_DENSE])
    wd_d = kb.din("ff_w_down", [FF_DENSE, D])
    NF = FF_DENSE // 128
    wg = kb.sb("ffwg", [128, 8, FF_DENSE], BF16)
    wu = kb.sb("ffwu", [128, 8, FF_DENSE], BF16)
    wd = kb.sb("ffwd", [128, NF, D], BF16)
    for c in range(0, NF, 2):
        sl = slice(c * 128, min(NF, c + 2) * 128)
        kb.dma_cast(wg[:, :, sl], wg_d[:, sl].rearrange("(k p) n -> p k n", p=128), w=["ffwg.%d" % (c // 2)])
        kb.dma_cast(wu[:, :, sl], wu_d[:, sl].rearrange("(k p) n -> p k n", p=128), w=["ffwu.%d" % (c // 2)])
        kb.dma_cast(wd[:, c:c + 2, :], wd_d[sl, :].rearrange("(c p) n -> p c n", p=128), w=["ffwd.%d" % (c // 2)])
    lnt = LNT(kb, self.ident, [self.PS[0], self.PS[1]], light=True)
    x1s = Rot([(kb.sb("x1f%d" % i, [128, D]), "x1f%d" % i) for i in range(1)])
    tmps = Rot([(kb.sb("tmpF%d" % i, [128, 512]), "tmpF%d" % i) for i in range(2)])
    hTs = Rot([(kb.sb("hT%d" % i, [128, 256], BF16), "hT%d" % i) for i in range(1)])
    pgu = Rot(self.PS[4:7])
    groups = [(i0, min(2, NTB - i0)) for i0 in range(0, NTB, 2)]
    for (i0, n) in groups:
        t0, nt = i0 * 128, n * 128
        ures = ["u2T.%d" % (i0 + q) for q in range(n)]
        for f in range(NF):
            pgt, pgtn = pgu.next()
            put, putn = pgu.next()
            for k in range(8):
                kb.mm(pgt[:, 0:nt], wg[:, k, f * 128:(f + 1) * 128], self.u2T[:, k, t0:t0 + nt], k == 0, k == 7, r=["ffwg.%d" % (f // 2)] + ures, w=[pgtn])
            for k in range(8):
                kb.mm(put[:, 0:nt], wu[:, k, f * 128:(f + 1) * 128], self.u2T[:, k, t0:t0 + nt], k == 0, k == 7, r=["ffwu.%d" % (f // 2)] + ures, w=[putn])
            s_, sn = tmps.next()
            u_, un = tmps.next()
            kb.act(s_[:, 0:nt], pgt[:, 0:nt], AF.Silu, r=[pgtn], w=[sn])
            kb.copy("act", u_[:, 0:nt], put[:, 0:nt], r=[putn], w=[un])
            hT, hTn = hTs.next()
            kb.tt("pool", hT[:, 0:nt], s_[:, 0:nt], u_[:, 0:nt], ALU.mult, r=[sn, un], w=[hTn])
            for q in range(n):
                for nb in range(2):
                    pa_, pan = self.PS[q * 2 + nb]
                    kb.mm(pa_[:, :], hT[:, q * 128:(q + 1) * 128], wd[:, f, nb * 512:(nb + 1) * 512], f == 0, f == NF - 1,
                          r=[hTn, "ffwd.%d" % (f // 2)], w=[pan])
        for q in range(n):
            _pb_final(self, lnt, i0 + q, lambda nb, q=q: self.PS[q * 2 + nb][0][:, :], lambda nb, q=q: [self.PS[q * 2 + nb][1]], x1s, tmps)


def _pb_ffn_moe(self):
    kb = self.kb
    NTB = self.NTB
    if int(_os.environ.get("B1_NEXP", NEXP)) > 0:
        wg_d = kb.din("moe_w_gate", [NEXP, D, FF_EXP])
        wu_d = kb.din("moe_w_up", [NEXP, D, FF_EXP])
        wd_d = kb.din("moe_w_down", [NEXP, FF_EXP, D])
    FG = 4
    NG = FF_EXP // (FG * 128)
    acc = kb.sb("moe_acc", [128, NTB, D])
    for i in range(NTB):
        kb.memset("pool" if i % 2 else "dve", acc[:, i, :], 0.0, w=["acc.%d" % i])
    wgs = Rot([(kb.sb("mwg%d" % i, [128, 8, FG * 128], BF16), "mwg%d" % i) for i in range(2)])
    wus = Rot([(kb.sb("mwu%d" % i, [128, 8, FG * 128], BF16), "mwu%d" % i) for i in range(2)])
    wds = Rot([(kb.sb("mwd%d" % i, [128, FG, D], BF16), "mwd%d" % i) for i in range(2)])
    tmps = Rot([(kb.sb("tmpF%d" % i, [128, 512]), "tmpF%d" % i) for i in range(4)])
    hTs = Rot([(kb.sb("hT%d" % i, [128, FG, 512], BF16), "hT%d" % i) for i in range(2)])
    pgu = Rot(self.PS[0:3])
    pac = Rot([(self.PS[3], self.PS[4]), (self.PS[5], self.PS[6])])
    nblk = NTB // 4
    for e in range(int(_os.environ.get("B1_NEXP", NEXP))):
        for fg in range(NG):
            c0 = fg * FG * 128
            wg, wgn = wgs.next()
            wu, wun = wus.next()
            wd, wdn = wds.next()
            kb.dma_cast(wg[:], wg_d[e, :, c0:c0 + FG * 128].rearrange("(k p) n -> p k n", p=128), w=[wgn])
            kb.dma_cast(wu[:], wu_d[e, :, c0:c0 + FG * 128].rearrange("(k p) n -> p k n", p=128), w=[wun])
            kb.dma_cast(wd[:], wd_d[e, c0:c0 + FG * 128, :].rearrange("(c p) n -> p c n", p=128), w=[wdn])
            for blk in range(nblk):
                t0 = blk * 512
                ures = ["u2T.%d" % (blk * 4 + q) for q in range(4)]
                hT, hTn = hTs.next()
                for c in range(FG):
                    pgt, pgtn = pgu.next()
                    put, putn = pgu.next()
                    for k in range(8):
                        kb.mm(pgt[:, :], wg[:, k, c * 128:(c + 1) * 128], self.u2T[:, k, t0:t0 + 512], k == 0, k == 7, r=[wgn] + ures, w=[pgtn])
                    for k in range(8):
                        kb.mm(put[:, :], wu[:, k, c * 128:(c + 1) * 128], self.u2T[:, k, t0:t0 + 512], k == 0, k == 7, r=[wun] + ures, w=[putn])
                    s_, sn = tmps.next()
                    u_, un = tmps.next()
                    kb.act(s_[:, :], pgt[:, :], AF.Silu, r=[pgtn], w=[sn])
                    kb.copy("act", u_[:, :], put[:, :], r=[putn], w=[un])
                    kb.tt("pool", hT[:, c, :], s_[:, :], u_[:, :], ALU.mult, r=[sn, un], w=[hTn + ".%d" % c])
                for q in range(4):
                    i = blk * 4 + q
                    banks = pac.next()
                    for nb in range(2):
                        pa_, pan = banks[nb]
                        for c in range(FG):
                            kb.mm(pa_[:, 0:512], hT[:, c, q * 128:(q + 1) * 128], wd[:, c, nb * 512:(nb + 1) * 512], c == 0, c == FG - 1,
                                  r=[hTn + ".%d" % c, wdn], w=[pan])
                        sl = slice(nb * 512, (nb + 1) * 512)
                        kb.stt(acc[:, i, sl], pa_[:, 0:512], self.gates[:, i, e:e + 1], acc[:, i, sl], ALU.mult, ALU.add,
                               r=[pan, "gates.%d" % i, "acc.%d" % i], w=["acc.%d" % i])
    lnt = LNT(kb, self.ident, [self.PS[0], self.PS[1]], light=True)
    x1s = Rot([(kb.sb("x1f%d" % i, [128, D]), "x1f%d" % i) for i in range(2)])
    for i in range(NTB):
        _pb_final(self, lnt, i, lambda nb, i=i: acc[:, i, nb * 512:(nb + 1) * 512], lambda nb, i=i: ["acc.%d" % i], x1s, tmps)


PhaseB.router = lambda self, i, lnt: _pb_router(self, i)
PhaseB.ffn_dense = _pb_ffn_dense
PhaseB.ffn_moe = _pb_ffn_moe


def prep_B(inp, l, b, h, xs, ctxs, yfull):
    with_ctx = l == 0
    m = {}
    lat = slice(h * 2048, (h + 1) * 2048)
    if with_ctx:
        cs = slice(h * 128, (h + 1) * 128)
        m["xtok"] = _f32(np.concatenate([ctxs[b][cs], xs[b][lat]], axis=0))
        cols = np.concatenate([np.arange(h * 128, (h + 1) * 128), CTXL + np.arange(h * 2048, (h + 1) * 2048)])
    else:
        m["xtok"] = _f32(xs[b][lat])
        cols = CTXL + np.arange(h * 2048, (h + 1) * 2048)
    if yfull is not None:
        m["yT"] = np.ascontiguousarray(yfull[:, cols])
    m["cvT"] = _f32(np.stack([inp["c"][b], inp["c_ctx"]], axis=1))
    m["wmodB"] = _f32(inp["w_mod"][l])
    m["bmodT"] = _f32(inp["b_mod"][l].reshape(48, 128).T)
    m["ident_d"] = np.eye(128, dtype=np.float32)
    m["w_g_h"] = _f32(inp["w_in"][l][:, OFF_G:])
    wbr = {"a": inp["w_br_a"][l], "b": inp["w_br_b"][l], "c": inp["w_br_c"][l], "d": inp["w_br_d"][l]}
    bounds = {"a": (0, 192), "b": (192, 320), "c": (320, 512), "d": (512, 640)}
    width = {"a": 192, "b": 128, "c": 192, "d": 128}
    pieces = []
    for br in "abcd":
        lo, hi = bounds[br]
        for hh in range(2):
            for ch in range(5):
                r0, r1 = max(lo, ch * 128), min(hi, (ch + 1) * 128)
                if r0 >= r1:
                    continue
                pc = np.zeros((128, D), np.float32)
                pc[r0 - ch * 128:r1 - ch * 128] = wbr[br][hh * width[br] + (r0 - lo):hh * width[br] + (r1 - lo)]
                pieces.append(pc)
    m["w_br_h"] = _f32(np.stack(pieces, axis=0))
    m["w_out_h"] = _f32(inp["w_out"][l])
    m["s5_wglu_h"] = _f32(inp["s5_w_glu"][l])
    m["s5_bglu_h"] = _f32(inp["s5_b_glu"][l].reshape(2, 128).T)
    m["lnp_h"] = _f32(np.stack([np.tile(inp[k][l][None, :], (128, 1)) for k in ("ln1_g", "ln1_b", "ln2_g", "ln2_b")], axis=0))
    if l % 2 == 0:
        m["ff_w_gate"] = _f32(inp["ff_w_gate"][l // 2])
        m["ff_w_up"] = _f32(inp["ff_w_up"][l // 2])
        m["ff_w_down"] = _f32(inp["ff_w_down"][l // 2])
    else:
        m["moe_router_h"] = _f32(inp["moe_router"][l // 2])
        m["moe_w_gate"] = _f32(inp["moe_w_gate"][l // 2])
        m["moe_w_up"] = _f32(inp["moe_w_up"][l // 2])
        m["moe_w_down"] = _f32(inp["moe_w_down"][l // 2])
    return m


def assemble_y(yb0, yb1):
    return np.concatenate([yb0, yb1], axis=0)


PAIRS = [[0, 1], [2, 3], [4, 5], [6, 7]]


class Fused:
    def __init__(self):
        kb = self.kb = KB()
        S = kb.S
        kb.pfx = ""
        hsel_d = kb.din("hsel_h", [128, 2])
        out = kb.dout("out", [2048, D])
        hsel = kb.sb("hsel", [128, 2])
        kb.dma(hsel[:], hsel_d[:, :], w=["hsel"])
        ybuf = [kb.dscratch("ybuf%d" % l, [640, T], BF16) for l in range(2)]
        ygath = [kb.dscratch("ygath%d" % l, [1280, T], BF16) for l in range(2)]
        xo0 = kb.dscratch("xo0", [2176, D])
        xg = kb.dscratch("xg", [4352, D])

        def allgather(src, dst, wres):
            S.barrier()
            S.coll(kb.es, lambda e: e.collective_compute("AllGather", ALU.bypass, replica_groups=PAIRS, ins=[src], outs=[dst]),
                   reads=[], writes=wres)

        def make_yload(l, with_ctx):
            def yload(pb, i, yt, ytn):
                if with_ctx:
                    c0, c1 = (0, 128) if i == 0 else (CTXL + (i - 1) * 128, CTXL + 2048 + (i - 1) * 128)
                else:
                    c0, c1 = CTXL + i * 128, CTXL + 2048 + i * 128
                (ya, yan), (yb, ybn) = pb.ybl.next(), pb.ybl.next()
                kb.dma(ya[:], ygath[l][:, c0:c0 + 128].rearrange("(c p) t -> p c t", p=128), r=["ygath%d" % l], w=[yan])
                kb.dma(yb[:], ygath[l][:, c1:c1 + 128].rearrange("(c p) t -> p c t", p=128), r=["ygath%d" % l], w=[ybn])
                kb.ts("pool", yt[:], ya[:], hsel[:, 0:1], ALU.mult, r=[yan, "hsel"], w=[ytn])
                kb.stt(yt[:], yb[:], hsel[:, 1:2], yt[:], ALU.mult, ALU.add, r=[ybn, "hsel", ytn], w=[ytn])
            return yload

        kb.pfx = "L0_"
        with kb.scope():
            PhaseA(True, kb=kb, ybuf=ybuf[0])
        allgather(ybuf[0][:, :], ygath[0][:, :], ["ygath0"])
        with kb.scope():
            PhaseB(0, kb=kb, yload=make_yload(0, True), xout=xo0)
        allgather(xo0[:, :], xg[:, :], ["xsrc"])
        kb.pfx = "L1_"

        def xin1(t):
            if t < 2:
                r0 = t * 2176
            else:
                n = t - 2
                r0 = 128 + n * 128 if n < 16 else 2176 + 128 + (n - 16) * 128
            return xg[r0:r0 + 128, :]

        with kb.scope():
            PhaseA(False, kb=kb, xin_fn=xin1, ybuf=ybuf[1])
        allgather(ybuf[1][:, :], ygath[1][:, :], ["ygath1"])
        with kb.scope():
            PhaseB(1, kb=kb, xtok=xo0[128:2176, :], yload=make_yload(1, False), xout=out)
        self.nc = kb.finish()


_PROGS = {}


def _prog(kind, layer):
    key = (kind, layer)
    if key not in _PROGS:
        _PROGS[key] = PhaseA(with_ctx=(layer == 0)) if kind == "A" else PhaseB(layer)
    return _PROGS[key]


def kernel_unfused(**inputs):
    inp = {k: np.asarray(v) for k, v in inputs.items()}
    xs = [_f32(inp["x"][b]) for b in range(NB)]
    ctxs = [_f32(inp["ctx"][b]) for b in range(NB)]
    cores = [(b, h) for b in range(NB) for h in range(2)]
    for l in range(2):
        pa = _prog("A", l)
        maps = []
        for (b, h) in cores:
            m = prep_A(inp, l, b, h, xs, ctxs)
            maps.append({k: m[k] for k in pa.kb.in_names})
        res = run_bass_kernel_spmd(pa.nc, maps, core_ids=list(range(8))).results
        del maps
        yfull = [assemble_y(np.asarray(res[2 * b]["ybuf"]), np.asarray(res[2 * b + 1]["ybuf"])) for b in range(NB)]
        pb = _prog("B", l)
        maps = []
        for (b, h) in cores:
            m = prep_B(inp, l, b, h, xs, ctxs, yfull[b])
            maps.append({k: m[k] for k in pb.kb.in_names})
        res = run_bass_kernel_spmd(pb.nc, maps, core_ids=list(range(8))).results
        del maps
        if l == 0:
            ctxs = [np.concatenate([np.asarray(res[2 * b + h]["xout"])[0:128] for h in range(2)], axis=0) for b in range(NB)]
            xs = [np.concatenate([np.asarray(res[2 * b + h]["xout"])[128:] for h in range(2)], axis=0) for b in range(NB)]
        else:
            xs = [np.concatenate([np.asarray(res[2 * b + h]["xout"]) for h in range(2)], axis=0) for b in range(NB)]
    return np.stack(xs, axis=0).astype(np.float32)


class Fused2:
    def __init__(self):
        kb = self.kb = KB()
        S = kb.S
        kb.pfx = ""
        hsel_d = kb.din("hsel_h", [128, 2])
        out = kb.dout("out", [2048, D])
        hsel = kb.sb("hsel", [128, 2])
        kb.dma(hsel[:], hsel_d[:, :], w=["hsel"])
        yfull = [kb.dscratch("yfull%d" % l, [1280, T], BF16) for l in range(2)]
        xfull = kb.dscratch("xfull", [T, D])
        kb.pfx = "L0_"
        xin0 = kb.din("xin", [T, D])

        def urow(hh, i, with_ctx):
            if with_ctx:
                return hh * 128 if i == 0 else CTXL + hh * 2048 + (i - 1) * 128
            return CTXL + hh * 2048 + i * 128

        def static_yload(l, hh, with_ctx):
            def yload(pb, i, yt, ytn):
                c0 = urow(hh, i, with_ctx)
                kb.dma(yt[:], yfull[l][:, c0:c0 + 128].rearrange("(c p) t -> p c t", p=128), r=["yfull%d" % l], w=[ytn])
            return yload

        def layer_mod(l):
            kb.pfx = "L%d_" % l
            modT = kb.sb("modT_L%d" % l, [128, 48, 2])
            cvT = kb.din("cvT", [D, 2])
            wmod = kb.din("wmodB", [D, 6144])
            bmodT = kb.din("bmodT", [128, 48])
            with kb.scope():
                pm = kb.ps("PSmod")
                wmb = Rot([(kb.sb("wmb%d" % i, [128, 8, 256]), "wmb%d" % i) for i in range(2)])
                emit_modT(kb, wmod, bmodT, cvT, 48, modT, pm, "PSmod", wmb)
                for c0 in (8, 32):
                    kb.ts("dve", modT[:, c0:c0 + 8, :], modT[:, c0:c0 + 8, :], 1.0, ALU.add, r=["modT"], w=["modT"])
            return modT

        modT0 = layer_mod(0)
        with kb.scope():
            uT = kb.sb("uT_sh", [128, 8, T], BF16)
            for h in range(2):
                kb.pfx = "L0h%d_" % h
                with kb.scope():
                    PhaseA(True, kb=kb, xin_fn=lambda t: xin0[t * 128:(t + 1) * 128, :], ybuf=yfull[0][h * 640:(h + 1) * 640, :],
                           modT=modT0, uT=uT, compute_uT=(h == 0))
        S.barrier()
        _stop = int(_os.environ.get("FZ_STOP", "9"))
        kb.pfx = "L0_"
        for hh in range(2 if _stop >= 2 else 0):
            with kb.scope():
                PhaseB(0, kb=kb, yload=static_yload(0, hh, True), modT=modT0,
                       xtok_fn=lambda i, hh=hh: xin0[urow(hh, i, True):urow(hh, i, True) + 128, :],
                       xout_fn=lambda i, hh=hh: xfull[urow(hh, i, True):urow(hh, i, True) + 128, :])
        S.barrier()
        modT1 = layer_mod(1)
        with kb.scope():
            uT = kb.sb("uT_sh", [128, 8, T], BF16)
            for h in range(2 if _stop >= 3 else 0):
                kb.pfx = "L1h%d_" % h
                with kb.scope():
                    PhaseA(False, kb=kb, xin_fn=lambda t: xfull[t * 128:(t + 1) * 128, :], ybuf=yfull[1][h * 640:(h + 1) * 640, :],
                           modT=modT1, uT=uT, compute_uT=(h == 0))
        S.barrier()
        kb.pfx = "L1_"

        def yload1(pb, i, yt, ytn):
            (ya, yan), (yb, ybn) = pb.ybl.next(), pb.ybl.next()
            c0, c1 = urow(0, i, False), urow(1, i, False)
            kb.dma(ya[:], yfull[1][:, c0:c0 + 128].rearrange("(c p) t -> p c t", p=128), w=[yan])
            kb.dma(yb[:], yfull[1][:, c1:c1 + 128].rearrange("(c p) t -> p c t", p=128), w=[ybn])
            kb.ts("pool", yt[:], ya[:], hsel[:, 0:1], ALU.mult, r=[yan, "hsel"], w=[ytn])
            kb.stt(yt[:], yb[:], hsel[:, 1:2], yt[:], ALU.mult, ALU.add, r=[ybn, "hsel", ytn], w=[ytn])

        def xload1(pb, i, lnt):
            (xa, xan), (xb, xbn) = lnt.xt.next(), lnt.xt.next()
            r0, r1 = urow(0, i, False), urow(1, i, False)
            kb.dma(xa[:], xfull[r0:r0 + 128, :], w=[xan])
            kb.dma(xb[:], xfull[r1:r1 + 128, :], w=[xbn])
            kb.ts("pool", xa[:], xa[:], hsel[:, 0:1], ALU.mult, r=[xan, "hsel"], w=[xan])
            kb.stt(xa[:], xb[:], hsel[:, 1:2], xa[:], ALU.mult, ALU.add, r=[xbn, "hsel", xan], w=[xan])
            return xa, [xan]

        if _stop >= 4:
            with kb.scope():
                PhaseB(1, kb=kb, yload=yload1, xload=xload1, xout=out, modT=modT1)
        self.nc = kb.finish()


_FUSED = []


def kernel(**inputs):
    inp = {k: np.asarray(v) for k, v in inputs.items()}
    xs = [_f32(inp["x"][b]) for b in range(NB)]
    ctxs = [_f32(inp["ctx"][b]) for b in range(NB)]
    if not _FUSED:
        _FUSED.append(Fused2())
    fz = _FUSED[0]
    names = set(fz.kb.in_names)
    maps = []
    for b in range(NB):
        shared = {}
        for l in range(2):
            for hp in range(2):
                for k, v in prep_A(inp, l, b, hp, xs, ctxs).items():
                    kk = "L%dh%d_%s" % (l, hp, k)
                    if kk in names:
                        shared[kk] = v
                    kk = "L%d_%s" % (l, k)
                    if kk in names and kk not in shared:
                        shared[kk] = v
            for k, v in prep_B(inp, l, b, 0, xs, ctxs, None).items():
                kk = "L%d_%s" % (l, k)
                if kk in names and kk not in shared:
                    shared[kk] = v
        for h in range(2):
            m = dict(shared)
            m["hsel_h"] = np.tile(np.array([[1.0 - h, float(h)]], np.float32), (128, 1))
            missing = names - set(m)
            assert not missing, missing
            maps.append(m)
    res = run_bass_kernel_spmd(fz.nc, maps, core_ids=list(range(8))).results
    outs = [np.concatenate([np.asarray(res[2 * b + h]["out"]) for h in range(2)], axis=0) for b in range(NB)]
    return np.stack(outs, axis=0).astype(np.float32)
```

```python
import math
import os as _os
from contextlib import ExitStack

import numpy as np
import ml_dtypes

import concourse.bass as bass
import concourse.mybir as mybir
from concourse.bass_utils import run_bass_kernel_spmd

F32 = mybir.dt.float32
BF16 = mybir.dt.bfloat16
ALU = mybir.AluOpType
AF = mybir.ActivationFunctionType

D = 1024
NB = 4
SEQ = 4096
CTXL = 256
T = SEQ + CTXL
NT = T // 128
LRU_W = 384
HY_W = 256
NA_W = 384
S5_W = 256
OFF_A = 0
OFF_B = 768
OFF_C = 1536
OFF_D = 2688
OFF_G = 2944
FF_DENSE = 2816
FF_EXP = 3584
NEXP = 8
ALPHA = 4.0 ** 0.25
EPS = 1e-5
MAGIC = 12582912.0
TWO_PI = 2.0 * math.pi

ENGS = ["pe", "act", "dve", "pool", "sp"]
N_DMA_SEMS = 32
SBUF_RESERVE = 16384
N_HW_SEMS = 24


class Sched:
    def __init__(self, nc, es):
        self.nc = nc
        self.ops = {e: [] for e in ENGS}
        self.cnt = {e: 0 for e in ENGS}
        self.res = {}
        self.esem = {e: es.enter_context(nc.semaphore("s_" + e)) for e in ENGS}
        self.dsem = [es.enter_context(nc.semaphore("d_%d" % i)) for i in range(N_DMA_SEMS)]
        self.dval = [0] * N_DMA_SEMS
        self.dnext = 0
        self.gnext = 0
        self.waited = {e: {} for e in ENGS}
        self.final_tokens = []
        self.pending = {e: [] for e in ENGS}

    def barrier(self):
        toks = [("e", e, self.cnt[e]) for e in ENGS if self.cnt[e] > 0]
        toks += [("d", k, self.dval[k]) for k in range(N_DMA_SEMS) if self.dval[k] > 0]
        toks += [("c", k, 16) for k in range(len(getattr(self, "csem", [])))]
        for e in ENGS:
            self.pending[e].extend(toks)

    def _sem(self, tok):
        if tok[0] == "c":
            return self.csem[tok[1]]
        return self.esem[tok[1]] if tok[0] == "e" else self.dsem[tok[1]]

    def coll(self, es, fn, reads=(), writes=()):
        if not hasattr(self, "csem"):
            self.csem = []
        deps = self._deps(reads, writes) + self.pending["pool"]
        self.pending["pool"] = []
        waits = self._waits_for("pool", deps)
        self.csem.append(es.enter_context(self.nc.semaphore("c_%d" % len(self.csem))))
        tok = ("c", len(self.csem) - 1, 16)
        self.ops["pool"].append((fn, waits, tok))
        self._commit(tok, reads, writes)
        return tok

    def _deps(self, reads, writes):
        deps = []
        for r in reads:
            st = self.res.get(r)
            if st and st["w"] is not None:
                deps.append(st["w"])
        for w in writes:
            st = self.res.get(w)
            if st:
                if st["w"] is not None:
                    deps.append(st["w"])
                deps.extend(st["r"])
        return deps

    def _commit(self, tok, reads, writes):
        for r in reads:
            st = self.res.setdefault(r, {"w": None, "r": []})
            st["r"].append(tok)
        for w in writes:
            self.res[w] = {"w": tok, "r": []}

    def _waits_for(self, eng, deps):
        need = {}
        for t in deps:
            if t[0] == "e" and t[1] == eng and eng == "pe":
                continue
            k = (t[0], t[1])
            if need.get(k, -1) < t[2]:
                need[k] = t[2]
        out = []
        for k, v in need.items():
            if self.waited[eng].get(k, -1) >= v:
                continue
            self.waited[eng][k] = v
            out.append((k[0], k[1], v))
        return out

    def op(self, eng, fn, reads=(), writes=()):
        deps = self._deps(reads, writes) + self.pending[eng]
        self.pending[eng] = []
        waits = self._waits_for(eng, deps)
        self.cnt[eng] += 1
        tok = ("e", eng, self.cnt[eng])
        self.ops[eng].append((fn, waits, tok))
        self._commit(tok, reads, writes)
        return tok

    def dma(self, eng, fn, reads=(), writes=(), final=False):
        deps = self._deps(reads, writes) + self.pending[eng]
        self.pending[eng] = []
        if eng == "pool":
            k = N_HW_SEMS + self.gnext
            self.gnext = (self.gnext + 1) % (N_DMA_SEMS - N_HW_SEMS)
        else:
            k = self.dnext
            self.dnext = (self.dnext + 1) % N_HW_SEMS
        if self.dval[k] > 0:
            deps.append(("d", k, self.dval[k]))
        waits = self._waits_for(eng, deps)
        self.dval[k] += 16
        tok = ("d", k, self.dval[k])
        self.ops[eng].append((fn, waits, tok))
        self._commit(tok, reads, writes)
        if final:
            self.final_tokens.append(tok)
        return tok

    def emit(self):
        nc = self.nc
        fin_waits = self._waits_for("sp", self.final_tokens)
        engmap = {"pe": "tensor", "act": "scalar", "dve": "vector", "pool": "gpsimd", "sp": "sync"}
        with nc.Block() as block:
            for e in ENGS:
                ops = self.ops[e]
                extra = fin_waits if e == "sp" else []
                if not ops and not extra:
                    continue

                def body(engine, ops=ops, extra=extra):
                    for fn, waits, tok in ops:
                        for w in waits:
                            engine.wait_ge(self._sem(w), w[2])
                        ins = fn(engine)
                        ins.then_inc(self._sem(tok), 1 if tok[0] == "e" else 16)
                    for w in extra:
                        engine.wait_ge(self._sem(w), w[2])

                getattr(block, engmap[e])(body)


class KB:
    def __init__(self, name="k"):
        self.nc = bass.Bass("TRN2", target_bir_lowering=False)
        self.es = ExitStack()
        self.S = Sched(self.nc, self.es)
        self.rr = 0
        self.in_names = []
        self.out_names = []

    def din(self, name, shape, dt=F32):
        name = self.__dict__.get("pfx", "") + name
        cache = self.__dict__.setdefault("_dins", {})
        if name in cache:
            return cache[name]
        self.in_names.append(name)
        cache[name] = self.nc.dram_tensor(name, list(shape), dt, kind="ExternalInput").ap()
        return cache[name]

    def dout(self, name, shape, dt=F32):
        name = self.__dict__.get("pfx", "") + name
        self.out_names.append(name)
        return self.nc.dram_tensor(name, list(shape), dt, kind="ExternalOutput").ap()

    def dscratch(self, name, shape, dt=F32):
        name = self.__dict__.get("pfx", "") + name
        return self.nc.dram_tensor(name, list(shape), dt).ap()

    def sb(self, name, shape, dt=F32):
        used = self.__dict__.setdefault("_used", {})
        n = used.get(name, 0)
        used[name] = n + 1
        if n:
            name = "%s__%d" % (name, n)
        t_ = self.es.enter_context(self.nc.sbuf_tensor(name, list(shape), dt))
        rem = self.nc.sbuf_bytes_remaining
        self.min_rem = min(self.__dict__.get("min_rem", 1 << 30), rem)
        assert rem >= SBUF_RESERVE, "SBUF budget exceeded at %s: remaining %d" % (name, rem)
        return t_

    def scope(self):
        kb = self

        class _Scope:
            def __enter__(self_):
                self_.old = kb.es
                kb.es = ExitStack()
                return self_

            def __exit__(self_, *a):
                kb.S.barrier()
                kb.es.close()
                kb.es = self_.old
                return False

        return _Scope()

    def ps(self, name, shape=(128, 512), dt=F32):
        used = self.__dict__.setdefault("_usedp", {})
        n = used.get(name, 0)
        used[name] = n + 1
        if n:
            name = "%s__%d" % (name, n)
        return self.es.enter_context(self.nc.psum_tensor(name, list(shape), dt))

    def dma(self, out, in_, r=(), w=(), eng=None, final=False):
        if eng is None:
            eng = ("sp", "act")[self.rr % 2]
            self.rr += 1
        return self.S.dma(eng, lambda e: e.dma_start(out=out, in_=in_), reads=r, writes=w, final=final)

    def dma_cast(self, out, in_, r=(), w=()):
        return self.S.dma("pool", lambda e: e.dma_start(out=out, in_=in_), reads=r, writes=w)

    def mm(self, out, lhsT, rhs, start, stop, r=(), w=()):
        return self.S.op("pe", lambda e: e.matmul(out, lhsT=lhsT, rhs=rhs, start=start, stop=stop), reads=r, writes=w)

    def tr(self, out, in_, ident, r=(), w=()):
        return self.S.op("pe", lambda e: e.transpose(out=out, in_=in_, identity=ident), reads=r, writes=w)

    def act(self, out, in_, func, r=(), w=(), scale=1.0, bias=0.0, eng="act"):
        return self.S.op(eng, lambda e: e.activation(out=out, in_=in_, func=func, bias=bias, scale=scale), reads=r, writes=w)

    def tt(self, eng, out, in0, in1, op, r=(), w=()):
        return self.S.op(eng, lambda e: e.tensor_tensor(out=out, in0=in0, in1=in1, op=op), reads=r, writes=w)

    def ts(self, eng, out, in0, s1, op0, s2=None, op1=None, r=(), w=()):
        if op1 is None:
            return self.S.op(eng, lambda e: e.tensor_scalar(out=out, in0=in0, scalar1=s1, scalar2=None, op0=op0), reads=r, writes=w)
        return self.S.op(eng, lambda e: e.tensor_scalar(out=out, in0=in0, scalar1=s1, scalar2=s2, op0=op0, op1=op1), reads=r, writes=w)

    def stt(self, out, in0, scalar, in1, op0, op1, r=(), w=()):
        return self.S.op("dve", lambda e: e.scalar_tensor_tensor(out=out, in0=in0, scalar=scalar, in1=in1, op0=op0, op1=op1), reads=r, writes=w)

    def copy(self, eng, out, in_, r=(), w=()):
        if eng == "act":
            return self.act(out, in_, AF.Copy, r=r, w=w)
        return self.S.op(eng, lambda e: e.tensor_copy(out=out, in_=in_), reads=r, writes=w)

    def memset(self, eng, ap, val, r=(), w=()):
        return self.S.op(eng, lambda e: e.memset(ap, val), reads=r, writes=w)

    def scan(self, out, d0, d1, init, r=(), w=()):
        return self.S.op("dve", lambda e: e.tensor_tensor_scan(out=out, data0=d0, data1=d1, initial=init, op0=ALU.mult, op1=ALU.add), reads=r, writes=w)

    def recip(self, out, in_, r=(), w=()):
        return self.S.op("dve", lambda e: e.reciprocal(out=out, in_=in_), reads=r, writes=w)

    def finish(self):
        self.S.emit()
        self.es.close()
        return self.nc


class Rot:
    def __init__(self, items):
        self.items = items
        self.i = 0

    def next(self):
        it = self.items[self.i % len(self.items)]
        self.i += 1
        return it


def emit_modT(kb, wmod, bmodT, cvT, nchunks, modT, pbank, pbank_name, wbufs):
    sT = kb.sb("mod_sT", [128, 8, 2])
    bm = kb.sb("mod_bm", [128, 48])
    kb.dma(sT[:], cvT.rearrange("(k p) j -> p k j", p=128), w=["mod_sT"])
    kb.dma(bm[:], bmodT[:, :], w=["mod_bm"])
    kb.act(sT[:], sT[:], AF.Silu, r=["mod_sT"], w=["mod_sT"])
    ng = nchunks // 2
    for g in range(ng):
        wt, wn = wbufs.next()
        kb.dma(wt[:], wmod[:, g * 256:(g + 1) * 256].rearrange("(k p) n -> p k n", p=128), w=[wn])
        for cc in range(2):
            c = g * 2 + cc
            for k in range(8):
                kb.mm(pbank[:, 2 * c:2 * c + 2], wt[:, k, cc * 128:(cc + 1) * 128], sT[:, k, :], k == 0, k == 7,
                      r=[wn, "mod_sT"], w=[pbank_name])
    for j in range(2):
        kb.tt("dve", modT[:, 0:nchunks, j], pbank[:, j:2 * nchunks:2], bm[:, 0:nchunks], ALU.add,
              r=[pbank_name, "mod_bm"], w=["modT"])


class LNT:
    def __init__(self, kb, ident, pbanks, light=False, xn_bufs=2):
        self.kb = kb
        self.ident = ident
        if not light:
            self.xt = Rot([(kb.sb("ln_xt%d" % i, [128, D]), "ln_xt%d" % i) for i in range(2)])
            self.xn = Rot([(kb.sb("ln_xn%d" % i, [128, D]), "ln_xn%d" % i) for i in range(xn_bufs)])
        self.st = Rot([(kb.sb("ln_st%d" % i, [128, 16]), "ln_st%d" % i) for i in range(2)])
        self.pb = Rot(pbanks)

    def stats_multi(self, xt, xres):
        return self.stats(xt, xres)

    def stats(self, xt, xtn):
        kb = self.kb
        xres = list(xtn) if isinstance(xtn, (list, tuple)) else [xtn]
        st, stn = self.st.next()
        for c in range(2):
            kb.S.op("dve", lambda e, c=c: e.bn_stats(out=st[:, c * 6:(c + 1) * 6], in_=xt[:, c * 512:(c + 1) * 512]),
                    reads=xres, writes=[stn])
        kb.S.op("dve", lambda e: e.bn_aggr(out=st[:, 12:14], in_=st[:, 0:12]), reads=[stn], writes=[stn])
        kb.act(st[:, 14:15], st[:, 13:14], AF.Sqrt, r=[stn], w=[stn], bias=EPS)
        kb.recip(st[:, 14:15], st[:, 14:15], r=[stn], w=[stn])
        return st, stn

    def run(self, x_src, src_res, uT_dst_fn, dst_res, scale_fn, shift_fn, xt_given=None, dst32_fn=None, dst32_res=None):
        kb = self.kb
        if xt_given is None:
            xt, xtn = self.xt.next()
            kb.dma(xt[:], x_src, r=src_res, w=[xtn])
        else:
            xt, xtn = xt_given
        st, stn = self.stats(xt, xtn)
        xn, xnn = self.xn.next()
        xres_ = list(xtn) if isinstance(xtn, (list, tuple)) else [xtn]
        kb.ts("dve", xn[:], xt[:], st[:, 12:13], ALU.subtract, st[:, 14:15], ALU.mult, r=xres_ + [stn], w=[xnn])
        self.last_xn = (xn, xnn)
        for half in range(2):
            pb, pbn = self.pb.next()
            for kk in range(4):
                k = half * 4 + kk
                kb.tr(pb[:, kk * 128:(kk + 1) * 128], xn[:, k * 128:(k + 1) * 128], self.ident[:], r=[xnn, "ident"], w=[pbn])
            for kk in range(4):
                k = half * 4 + kk
                if dst32_fn is not None:
                    if kk % 2 == 0:
                        kb.act(dst32_fn(k), pb[:, kk * 128:(kk + 1) * 128], AF.Identity, r=[pbn, "modT"], w=dst32_res,
                               scale=scale_fn(k), bias=shift_fn(k))
                    else:
                        kb.ts("dve", dst32_fn(k), pb[:, kk * 128:(kk + 1) * 128], scale_fn(k), ALU.mult, shift_fn(k), ALU.add,
                              r=[pbn, "modT"], w=dst32_res)
                    kb.copy("pool", uT_dst_fn(k), dst32_fn(k), r=dst32_res, w=dst_res)
                elif kk % 2 == 0:
                    kb.act(uT_dst_fn(k), pb[:, kk * 128:(kk + 1) * 128], AF.Identity, r=[pbn, "modT"], w=dst_res,
                           scale=scale_fn(k), bias=shift_fn(k))
                else:
                    kb.ts("dve", uT_dst_fn(k), pb[:, kk * 128:(kk + 1) * 128], scale_fn(k), ALU.mult, shift_fn(k), ALU.add,
                          r=[pbn, "modT"], w=dst_res)
        return xt, xtn, st, stn


BLKS = [(0, 256)] + [(256 + 512 * j, 256 + 512 * (j + 1)) for j in range(8)]


def ut_res(t0, t1):
    return ["uT.%d" % i for i in range(t0 // 128, (t1 + 127) // 128)]


class PhaseA:
    def __init__(self, with_ctx, mixers=("lru", "s5", "na", "hyena"), dbg=False, kb=None, xin_fn=None, ybuf=None,
                 modT=None, uT=None, compute_uT=True):
        self.with_ctx = with_ctx
        self.standalone = kb is None
        kb = self.kb = kb if kb is not None else KB()
        self.dbg = dbg
        if xin_fn is None:
            self.xin = kb.din("xin", [T, D])
            xin_fn = lambda t: self.xin[t * 128:(t + 1) * 128, :]
        if modT is None:
            self.cvT = kb.din("cvT", [D, 2])
            self.wmod = kb.din("wmodA", [D, 2048])
            self.bmodT = kb.din("bmodT", [128, 48])
        self.ident_d = kb.din("ident_d", [128, 128])
        self.w_in = kb.din("w_inA", [D, 1472])
        self.ybuf = ybuf if ybuf is not None else kb.dout("ybuf", [640, T], BF16)
        self.ident = kb.sb("ident", [128, 128])
        kb.dma(self.ident[:], self.ident_d[:, :], w=["ident"])
        self.identb = kb.sb("identb", [128, 128], BF16)
        kb.copy("dve", self.identb[:], self.ident[:], r=["ident"], w=["identb"])
        self.uT = uT if uT is not None else kb.sb("uT", [128, 8, T], BF16)
        self.FP = kb.sb("FP", [128, 4 * T])
        self.CP = Rot([(kb.sb("C%d" % i, [128, 512]), "C%d" % i) for i in range(8)])
        self.PS = [(kb.ps("PS%d" % i), "PS%d" % i) for i in range(8)]
        self.modT = modT if modT is not None else kb.sb("modT", [128, 48, 2])
        self.ost = Rot([(kb.sb("ost%d" % i, [128, 512], BF16), "ost%d" % i) for i in range(3)])
        with kb.scope():
            if modT is None:
                self.wmb = Rot([(kb.sb("wmb%d" % i, [128, 8, 256]), "wmb%d" % i) for i in range(2)])
                emit_modT(kb, self.wmod, self.bmodT, self.cvT, 16, self.modT, self.PS[7][0], "PS7", self.wmb)
                kb.ts("dve", self.modT[:, 8:16, :], self.modT[:, 8:16, :], 1.0, ALU.add, r=["modT"], w=["modT"])
            lnt = LNT(kb, self.ident, [self.PS[0], self.PS[1]]) if compute_uT else None
            for t in range(NT if compute_uT else 0):
                j = 1 if t < 2 else 0
                lnt.run(xin_fn(t), ["xsrc"],
                        lambda k, t=t: self.uT[:, k, t * 128:(t + 1) * 128], ["uT.%d" % t],
                        lambda k, j=j: self.modT[:, 8 + k, j:j + 1], lambda k, j=j: self.modT[:, k, j:j + 1])
        self.psr = Rot(self.PS[0:4])
        for name in mixers:
            with kb.scope():
                getattr(self, name)()
        if self.standalone:
            self.nc = kb.finish()

    def F(self, s, t0=0, t1=T):
        return self.FP[:, s * T + t0:s * T + t1]

    def fres(self, s, t0=0, t1=T):
        return ["F%d.%d" % (s, bi) for bi, (a, b) in enumerate(BLKS) if a < t1 and b > t0]

    def load_w(self, name, c0, ncols):
        wt = self.kb.sb(name, [128, 8, ncols], BF16)
        self.kb.dma_cast(wt[:], self.w_in[:, c0:c0 + ncols].rearrange("(k p) n -> p k n", p=128), w=[name])
        return wt

    def proj(self, wt, wname, c0, m, blk, pb, pbn):
        t0, t1 = blk
        for k in range(8):
            self.kb.mm(pb[0:m, 0:t1 - t0], wt[:, k, c0:c0 + m], self.uT[:, k, t0:t1], k == 0, k == 7,
                       r=[wname] + ut_res(t0, t1), w=[pbn])

    def lru(self):
        kb = self.kb
        cw_d = kb.din("lru_cw_h", [192, 5])
        W_d = kb.din("lru_W_h", [2, 2, 192, 192])
        vec_d = kb.din("lru_vec_h", [192, 6])
        wt = self.load_w("w_lru", 0, 384)
        for ct, (p0, P) in enumerate([(0, 128), (128, 64)]):
            sfx = "_%d" % ct
            cw = kb.sb("lru_cw" + sfx, [128, 5])
            vec = kb.sb("lru_vec" + sfx, [128, 6])
            nsp = kb.sb("lru_nsp" + sfx, [128, 2])
            Wt = kb.sb("lru_Wt" + sfx, [128, 4, 128])
            kb.dma(cw[0:P, :], cw_d[p0:p0 + P, :], w=["lru_cw" + sfx])
            kb.dma(vec[0:P, :], vec_d[p0:p0 + P, :], w=["lru_vec" + sfx])
            for d in range(2):
                for ri in range(2):
                    kb.dma(Wt[0:P, d * 2 + ri, 0:P], W_d[d, ri, p0:p0 + P, p0:p0 + P], w=["lru_Wt" + sfx])
            for d in range(2):
                kb.act(nsp[0:P, d:d + 1], vec[0:P, d * 3 + 2:d * 3 + 3], AF.Exp, r=["lru_vec" + sfx], w=["lru_nsp" + sfx], scale=-1.0)
            kb.act(nsp[0:P, :], nsp[0:P, :], AF.Ln, r=["lru_nsp" + sfx], w=["lru_nsp" + sfx], bias=1.0)
            kb.ts("dve", nsp[0:P, :], nsp[0:P, :], -8.0, ALU.mult, r=["lru_nsp" + sfx], w=["lru_nsp" + sfx])
            for bi, blk in enumerate(BLKS):
                t0, t1 = blk
                pb, pbn = self.psr.next()
                self.proj(wt, "w_lru", p0, P, blk, pb, pbn)
                kb.copy("act", self.F(0, t0, t1)[0:P], pb[0:P, 0:t1 - t0], r=[pbn], w=["F0.%d" % bi])
                pb, pbn = self.psr.next()
                self.proj(wt, "w_lru", 192 + p0, P, blk, pb, pbn)
                kb.copy("dve", self.F(1, t0, t1)[0:P], pb[0:P, 0:t1 - t0], r=[pbn], w=["F1.%d" % bi])
            for (s0, s1) in [(0, CTXL), (CTXL, T)]:
                rr = self.fres(1, s0, s1)
                ww = self.fres(2, s0, s1)
                kb.ts("dve", self.F(2, s0, s1)[0:P], self.F(1, s0, s1)[0:P], cw[0:P, 2:3], ALU.mult, cw[0:P, 4:5], ALU.add,
                      r=rr + ["lru_cw" + sfx], w=ww)
                for kk, sh in [(0, -2), (1, -1), (3, 1)]:
                    if sh < 0:
                        o = self.F(2, s0 - sh, s1)[0:P]
                        i0 = self.F(1, s0, s1 + sh)[0:P]
                    else:
                        o = self.F(2, s0, s1 - sh)[0:P]
                        i0 = self.F(1, s0 + sh, s1)[0:P]
                    kb.stt(o, i0, cw[0:P, kk:kk + 1], o, ALU.mult, ALU.add, r=rr + ww + ["lru_cw" + sfx], w=ww)
            for d in range(2):
                order = list(range(len(BLKS))) if d == 0 else [0] + list(range(len(BLKS) - 1, 0, -1))
                hs = 1 if d == 0 else 3
                for oi, bi in enumerate(order):
                    t0, t1 = BLKS[bi]
                    n = t1 - t0
                    xcb = self.F(2, t0, t1)[0:P]
                    pr, prn = self.psr.next()
                    kb.mm(pr[0:P, 0:n], Wt[0:P, d * 2 + 0, 0:P], xcb, True, True, r=["lru_Wt" + sfx, "F2.%d" % bi], w=[prn])
                    pi, pin = self.psr.next()
                    kb.mm(pi[0:P, 0:n], Wt[0:P, d * 2 + 1, 0:P], xcb, True, True, r=["lru_Wt" + sfx, "F2.%d" % bi], w=[pin])
                    gr, grn = self.CP.next()
                    gi, gin = self.CP.next()
                    a, an = self.CP.next()
                    om, omn = self.CP.next()
                    kb.act(gr[0:P, 0:n], pr[0:P, 0:n], AF.Sigmoid, r=[prn, "lru_vec" + sfx], w=[grn], bias=vec[0:P, d * 3:d * 3 + 1])
                    kb.act(gi[0:P, 0:n], pi[0:P, 0:n], AF.Sigmoid, r=[pin, "lru_vec" + sfx], w=[gin], bias=vec[0:P, d * 3 + 1:d * 3 + 2])
                    kb.act(a[0:P, 0:n], gr[0:P, 0:n], AF.Exp, r=[grn, "lru_nsp" + sfx], w=[an], scale=nsp[0:P, d:d + 1])
                    kb.tt("pool", om[0:P, 0:n], a[0:P, 0:n], a[0:P, 0:n], ALU.mult, r=[an], w=[omn])
                    kb.act(om[0:P, 0:n], om[0:P, 0:n], AF.Sqrt, r=[omn], w=[omn], scale=-1.0, bias=1.0)
                    kb.tt("pool", gi[0:P, 0:n], gi[0:P, 0:n], om[0:P, 0:n], ALU.mult, r=[gin, omn], w=[gin])
                    kb.tt("pool", gi[0:P, 0:n], gi[0:P, 0:n], xcb, ALU.mult, r=[gin, "F2.%d" % bi], w=[gin])
                    if d == 0:
                        init = 0.0 if oi == 0 else self.F(1, t0 - 1, t0)[0:P]
                        ir = [] if oi == 0 else self.fres(1, t0 - 1, t0)
                        kb.scan(self.F(1, t0, t1)[0:P], a[0:P, 0:n], gi[0:P, 0:n], init, r=[an, gin] + ir, w=["F1.%d" % bi])
                    else:
                        if oi == 0:
                            init, ir = 0.0, []
                        elif oi == 1:
                            init, ir = self.F(3, 0, 1)[0:P], ["F3.0"]
                        else:
                            init, ir = self.F(3, t1, t1 + 1)[0:P], self.fres(3, t1, t1 + 1)
                        kb.scan(self.F(3, t0, t1)[0:P, ::-1], a[0:P, n - 1::-1], gi[0:P, n - 1::-1], init, r=[an, gin] + ir, w=["F3.%d" % bi])
                        if bi == 0 and not self.with_ctx:
                            continue
                        kb.tt("pool", gr[0:P, 0:n], self.F(1, t0, t1)[0:P], self.F(3, t0, t1)[0:P], ALU.add, r=["F1.%d" % bi, "F3.%d" % bi, grn], w=[grn])
                        kb.act(om[0:P, 0:n], self.F(0, t0, t1)[0:P], AF.Gelu_apprx_tanh, r=["F0.%d" % bi, omn], w=[omn])
                        ob, obn = self.ost.next()
                        kb.tt("dve", ob[0:P, 0:n], gr[0:P, 0:n], om[0:P, 0:n], ALU.mult, r=[grn, omn], w=[obn])
                        kb.dma(self.ybuf[p0:p0 + P, t0:t1], ob[0:P, 0:n], r=[obn], final=True)


def _f32(a):
    return np.ascontiguousarray(a, dtype=np.float32)


def blockdiag(blocks):
    n = sum(b.shape[0] for b in blocks)
    m = sum(b.shape[1] for b in blocks)
    out = np.zeros((n, m), np.float32)
    i = j = 0
    for b in blocks:
        out[i:i + b.shape[0], j:j + b.shape[1]] = b
        i += b.shape[0]
        j += b.shape[1]
    return out


def colsel_A(h):
    a_g = OFF_A + h * 192 + np.arange(192)
    a_x = OFF_A + LRU_W + h * 192 + np.arange(192)
    b = [OFF_B + j * HY_W + h * 128 + np.arange(128) for j in range(3)]
    c = [OFF_C + j * NA_W + h * 192 + np.arange(192) for j in range(3)]
    d = OFF_D + h * 128 + np.arange(128)
    return np.concatenate([a_g, a_x] + b + c + [d])


def prep_A(inp, l, b, h, xs, ctxs):
    m = {}
    m["xin"] = _f32(np.concatenate([ctxs[b], xs[b]], axis=0))
    m["cvT"] = _f32(np.stack([inp["c"][b], inp["c_ctx"]], axis=1))
    m["wmodA"] = _f32(inp["w_mod"][l][:, 0:2048])
    m["bmodT"] = _f32(inp["b_mod"][l].reshape(48, 128).T)
    m["ident_d"] = np.eye(128, dtype=np.float32)
    m["w_inA"] = _f32(inp["w_in"][l][:, colsel_A(h)])
    ch = h * 192 + np.arange(192)
    m["lru_cw_h"] = _f32(np.concatenate([inp["lru_conv_w"][l][:, ch].T, inp["lru_conv_b"][l][ch][:, None]], axis=1))
    W = np.zeros((2, 2, 192, 192), np.float32)
    for d in range(2):
        W[d, 0] = blockdiag([inp["lru_w_r"][l][d][3 * h + g] for g in range(3)])
        W[d, 1] = blockdiag([inp["lru_w_i"][l][d][3 * h + g] for g in range(3)])
    m["lru_W_h"] = W
    m["lru_vec_h"] = _f32(np.stack([inp[k][l][d][ch] for d in range(2) for k in ("lru_b_r", "lru_b_i", "lru_lambda")], axis=1))
    prep_A_s5(inp, l, h, m)
    prep_A_na(inp, l, h, m)
    prep_A_hy(inp, l, h, m)
    return m


def prep_A_s5(inp, l, h, m):
    pa = np.zeros((128, 2, 4, 3), np.float32)
    BB = np.zeros((2, 4, 2, 128, 128), np.float32)
    CT = np.zeros((2, 4, 2, 128, 128), np.float32)
    for d in range(2):
        for st in range(4):
            for gl in range(2):
                gloc = 2 * st + gl
                g = 8 * h + gloc
                sl = slice(gl * 64, (gl + 1) * 64)
                pa[sl, d, st, 0] = inp["s5_a_re"][l][d][g]
                pa[sl, d, st, 1] = inp["s5_a_im"][l][d][g]
                pa[sl, d, st, 2] = inp["s5_log_dt"][l][d][g]
                cs = slice(gloc * 16, (gloc + 1) * 16)
                BB[d, st, 0, cs, sl] = inp["s5_b_re"][l][d][g].T
                BB[d, st, 1, cs, sl] = inp["s5_b_im"][l][d][g].T
                CT[d, st, 0, sl, cs] = inp["s5_c_re"][l][d][g].T
                CT[d, st, 1, sl, cs] = inp["s5_c_im"][l][d][g].T
    m["s5_pa_h"] = pa
    m["s5_BB_h"] = BB
    m["s5_CT_h"] = CT
    m["s5_d_h"] = _f32(inp["s5_d"][l][h * 128:(h + 1) * 128][:, None])
    m["s5_tau_h"] = np.tile(np.arange(256, dtype=np.float32)[None, :], (128, 1))


def _phasea_s5(self):
    kb = self.kb
    TC = 256
    pa_d = kb.din("s5_pa_h", [128, 2, 4, 3])
    BB_d = kb.din("s5_BB_h", [2, 4, 2, 128, 128])
    CT_d = kb.din("s5_CT_h", [2, 4, 2, 128, 128])
    d_d = kb.din("s5_d_h", [128, 1])
    tau_d = kb.din("s5_tau_h", [128, TC])
    wt = self.load_w("w_s5", 1344, 128)
    pa = kb.sb("s5_pa", [128, 8, 3])
    BB = kb.sb("s5_BB", [128, 16, 128])
    CT = kb.sb("s5_CT", [128, 16, 128])
    dv = kb.sb("s5_dv", [128, 1])
    tau = kb.sb("s5_tau", [128, TC])
    sc = kb.sb("s5_sc", [128, 16, 8])
    carry = kb.sb("s5_carry", [128, 8, 2])
    kb.dma(pa[:], pa_d.rearrange("p d s c -> p (d s) c"), w=["s5_pa"])
    kb.dma(BB[:], BB_d.rearrange("d s c k m -> k (d s c) m"), w=["s5_BB"])
    kb.dma(CT[:], CT_d.rearrange("d s c k m -> k (d s c) m"), w=["s5_CT"])
    kb.dma(dv[:], d_d[:, :], w=["s5_dv"])
    kb.dma(tau[:], tau_d[:, :], w=["s5_tau"])
    for i in range(8):
        kb.ts("pool", CT[:, 2 * i + 1, :], CT[:, 2 * i + 1, :], -1.0, ALU.mult, r=["s5_CT"], w=["s5_CT"])
    kb.memset("dve", carry[:], 0.0, w=["s5_carry%d" % i for i in range(8)])
    DT, LAM, ANG, RHO, RC, C1, S1, ABR, ABI, DEN, FRE, FIM, T0, T1, RC4 = range(15)
    CT_, ST_ = 15, 14
    R = ["s5_sc"]

    def v(i):
        return sc[:, i, :]

    are, aim, ldt = pa[:, :, 0], pa[:, :, 1], pa[:, :, 2]
    kb.act(v(DT), ldt, AF.Exp, r=["s5_pa"], w=R)
    kb.tt("dve", v(LAM), v(DT), are, ALU.mult, r=R + ["s5_pa"], w=R)
    kb.tt("dve", v(ANG), v(DT), aim, ALU.mult, r=R + ["s5_pa"], w=R)
    kb.act(v(RHO), v(LAM), AF.Exp, r=R, w=R)
    kb.ts("dve", v(RC), v(ANG), 1.0 / TWO_PI, ALU.mult, r=R, w=R)
    kb.ts("dve", v(RC4), v(RC), 0.25, ALU.add, r=R, w=R)

    def sin_cycles(out, rin, shape_tmp, rres, wres):
        kb.ts("dve", shape_tmp, rin, MAGIC, ALU.add, r=rres, w=wres)
        kb.ts("dve", shape_tmp, shape_tmp, MAGIC, ALU.subtract, r=wres, w=wres)
        kb.tt("dve", shape_tmp, rin, shape_tmp, ALU.subtract, r=rres + wres, w=wres)
        kb.act(out, shape_tmp, AF.Sin, r=wres, w=wres, scale=TWO_PI)

    sin_cycles(v(S1), v(RC), v(T0), R, R)
    sin_cycles(v(C1), v(RC4), v(T0), R, R)
    kb.ts("dve", v(T1), v(RC), float(TC), ALU.mult, r=R, w=R)
    kb.ts("dve", v(CT_), v(T1), 0.25, ALU.add, r=R, w=R)
    sin_cycles(v(ST_), v(T1), v(T0), R, R)
    sin_cycles(v(CT_), v(CT_), v(T0), R, R)
    kb.tt("dve", v(ABR), v(RHO), v(C1), ALU.mult, r=R, w=R)
    kb.tt("dve", v(ABI), v(RHO), v(S1), ALU.mult, r=R, w=R)
    kb.tt("dve", v(DEN), are, are, ALU.mult, r=R + ["s5_pa"], w=R)
    kb.tt("dve", v(T0), aim, aim, ALU.mult, r=R + ["s5_pa"], w=R)
    kb.tt("dve", v(DEN), v(DEN), v(T0), ALU.add, r=R, w=R)
    kb.recip(v(DEN), v(DEN), r=R, w=R)
    kb.ts("dve", v(T1), v(ABR), -1.0, ALU.add, r=R, w=R)
    kb.tt("dve", v(FRE), v(T1), are, ALU.mult, r=R + ["s5_pa"], w=R)
    kb.tt("dve", v(T0), v(ABI), aim, ALU.mult, r=R + ["s5_pa"], w=R)
    kb.tt("dve", v(FRE), v(FRE), v(T0), ALU.add, r=R, w=R)
    kb.tt("dve", v(FRE), v(FRE), v(DEN), ALU.mult, r=R, w=R)
    kb.tt("dve", v(FIM), v(ABI), are, ALU.mult, r=R + ["s5_pa"], w=R)
    kb.tt("dve", v(T0), v(T1), aim, ALU.mult, r=R + ["s5_pa"], w=R)
    kb.tt("dve", v(FIM), v(FIM), v(T0), ALU.subtract, r=R, w=R)
    kb.tt("dve", v(FIM), v(FIM), v(DEN), ALU.mult, r=R, w=R)
    for idx in range(8):
        Cc = self.F(2, idx * 512, idx * 512 + TC)
        Ss = self.F(2, idx * 512 + TC, idx * 512 + 2 * TC)
        Ire = self.F(3, idx * 512, idx * 512 + TC)
        Iim = self.F(3, idx * 512 + TC, idx * 512 + 2 * TC)
        tn = ["s5_tab%d" % idx]
        t0, t0n = self.CP.next()
        t1, t1n = self.CP.next()
        kb.ts("dve", t0[:, 0:TC], tau[:], sc[:, RC, idx:idx + 1], ALU.mult, r=R + ["s5_tau"], w=[t0n])
        sin_cycles(Ss, t0[:, 0:TC], t1[:, 0:TC], [t0n], [t1n] + tn)
        kb.ts("dve", t0[:, 0:TC], t0[:, 0:TC], 0.25, ALU.add, r=[t0n], w=[t0n])
        sin_cycles(Cc, t0[:, 0:TC], t1[:, 0:TC], [t0n], [t1n] + tn)
        kb.ts("dve", t0[:, 0:TC], Ss, sc[:, FIM, idx:idx + 1], ALU.mult, r=R + tn, w=[t0n])
        kb.stt(Ire, Cc, sc[:, FRE, idx:idx + 1], t0[:, 0:TC], ALU.mult, ALU.add, r=R + tn + [t0n], w=tn)
        kb.ts("dve", t1[:, 0:TC], Ss, sc[:, FRE, idx:idx + 1], ALU.mult, r=R + tn, w=[t1n])
        kb.stt(Iim, Cc, sc[:, FIM, idx:idx + 1], t1[:, 0:TC], ALU.mult, ALU.subtract, r=R + tn + [t1n], w=tn)
    for bi, blk in enumerate(BLKS):
        t0_, t1_ = blk
        pb, pbn = self.psr.next()
        self.proj(wt, "w_s5", 0, 128, blk, pb, pbn)
        kb.copy("act", self.F(0, t0_, t1_), pb[:, 0:t1_ - t0_], r=[pbn], w=["F0.%d" % bi])
    halves = []
    for (cb, cn) in self.CP.items:
        halves.append((cb[:, 0:TC], cn + "a"))
        halves.append((cb[:, TC:2 * TC], cn + "b"))
    SP = Rot(halves)
    py, pyn = self.PS[4]
    NCH = T // TC
    for d in range(2):
        order = list(range(NCH)) if d == 0 else [0] + list(range(NCH - 1, 0, -1))
        for ci in order:
            c0, c1_ = ci * TC, (ci + 1) * TC
            ub = self.F(0, c0, c1_)
            ures = self.fres(0, c0, c1_)
            for st in range(4):
                idx = d * 4 + st
                tn = ["s5_tab%d" % idx]
                Cc = self.F(2, idx * 512, idx * 512 + TC)
                Ss = self.F(2, idx * 512 + TC, idx * 512 + 2 * TC)
                Ire = self.F(3, idx * 512, idx * 512 + TC)
                Iim = self.F(3, idx * 512 + TC, idx * 512 + 2 * TC)
                pre, pren = self.psr.next()
                pim, pimn = self.psr.next()
                kb.mm(pre[:, 0:TC], BB[:, idx * 2 + 0, :], ub, True, True, r=["s5_BB"] + ures, w=[pren])
                kb.mm(pim[:, 0:TC], BB[:, idx * 2 + 1, :], ub, True, True, r=["s5_BB"] + ures, w=[pimn])
                bre = pre[:, 0:TC] if d == 0 else pre[:, TC - 1::-1]
                bim = pim[:, 0:TC] if d == 0 else pim[:, TC - 1::-1]
                (a1, a1n), (a2, a2n), (a3, a3n), (a4, a4n) = SP.next(), SP.next(), SP.next(), SP.next()
                kb.tt("dve", a1, bre, Ire, ALU.mult, r=[pren] + tn, w=[a1n])
                kb.tt("dve", a2, bim, Iim, ALU.mult, r=[pimn] + tn, w=[a2n])
                kb.tt("dve", a3, bim, Ire, ALU.mult, r=[pimn] + tn, w=[a3n])
                kb.tt("dve", a4, bre, Iim, ALU.mult, r=[pren] + tn, w=[a4n])
                kb.tt("pool", a1, a1, a2, ALU.subtract, r=[a1n, a2n], w=[a1n])
                kb.tt("pool", a3, a3, a4, ALU.add, r=[a3n, a4n], w=[a3n])
                (gre, gren), (gim, gimn) = SP.next(), SP.next()
                rho_bc = sc[:, RHO, idx:idx + 1].to_broadcast([128, TC])
                kb.scan(gre, rho_bc, a1, carry[:, idx, 0:1], r=R + [a1n, "s5_carry%d" % idx], w=[gren])
                kb.scan(gim, rho_bc, a3, carry[:, idx, 1:2], r=R + [a3n, "s5_carry%d" % idx], w=[gimn])
                (cq, cqn) = SP.next()
                gl_re, gl_im = gre[:, TC - 1:TC], gim[:, TC - 1:TC]
                kb.ts("dve", cq[:, 0:1], gl_im, sc[:, ST_, idx:idx + 1], ALU.mult, r=[gimn] + R, w=[cqn])
                kb.stt(carry[:, idx, 0:1], gl_re, sc[:, CT_, idx:idx + 1], cq[:, 0:1], ALU.mult, ALU.subtract,
                       r=[gren, cqn, "s5_carry%d" % idx] + R, w=["s5_carry%d" % idx])
                kb.ts("dve", cq[:, 1:2], gl_im, sc[:, CT_, idx:idx + 1], ALU.mult, r=[gimn, cqn] + R, w=[cqn])
                kb.stt(carry[:, idx, 1:2], gl_re, sc[:, ST_, idx:idx + 1], cq[:, 1:2], ALU.mult, ALU.add,
                       r=[gren, cqn, "s5_carry%d" % idx] + R, w=["s5_carry%d" % idx])
                (u1, u1n), (u2, u2n), (u3, u3n), (u4, u4n) = SP.next(), SP.next(), SP.next(), SP.next()
                kb.tt("pool", u1, Cc, gre, ALU.mult, r=tn + [gren], w=[u1n])
                kb.tt("pool", u2, Ss, gim, ALU.mult, r=tn + [gimn], w=[u2n])
                kb.tt("pool", u3, Ss, gre, ALU.mult, r=tn + [gren], w=[u3n])
                kb.tt("pool", u4, Cc, gim, ALU.mult, r=tn + [gimn], w=[u4n])
                (hre, hren), (him, himn) = SP.next(), SP.next()
                hre_o = hre if d == 0 else hre[:, ::-1]
                him_o = him if d == 0 else him[:, ::-1]
                kb.tt("dve", hre_o, u1, u2, ALU.subtract, r=[u1n, u2n], w=[hren])
                kb.tt("dve", him_o, u3, u4, ALU.add, r=[u3n, u4n], w=[himn])
                kb.mm(py[:, 0:TC], CT[:, idx * 2 + 0, :], hre, st == 0, False, r=["s5_CT", hren], w=[pyn])
                kb.mm(py[:, 0:TC], CT[:, idx * 2 + 1, :], him, False, st == 3, r=["s5_CT", himn], w=[pyn])
            f1res = self.fres(1, c0, c1_)
            if d == 0:
                kb.copy("act", self.F(1, c0, c1_), py[:, 0:TC], r=[pyn], w=f1res)
            else:
                if ci == 0 and not self.with_ctx:
                    continue
                (y1, y1n) = SP.next()
                kb.tt("dve", y1, py[:, 0:TC], self.F(1, c0, c1_), ALU.add, r=[pyn] + f1res, w=[y1n])
                kb.stt(y1, ub, dv[:, 0:1], y1, ALU.mult, ALU.add, r=ures + ["s5_dv", y1n], w=[y1n])
                ob, obn = self.ost.next()
                kb.act(ob[:, 0:TC], y1, AF.Gelu_apprx_tanh, r=[y1n], w=[obn])
                kb.dma(self.ybuf[512:640, c0:c1_], ob[:, 0:TC], r=[obn], final=True)


PhaseA.s5 = _phasea_s5


def prep_A_na(inp, l, h, m):
    jj = np.arange(64)[:, None]
    ww = np.arange(64)[None, :]
    cs = np.clip(ww - 8, 0, 48)
    allowed = (jj >= cs) & (jj < cs + 16)
    dc = np.clip(jj - ww + 15, 0, 30)
    rpb = inp["na_rpb"][l][3 * h:3 * h + 3]
    g = rpb[:, :, dc]
    m["na_bias_h"] = _f32(np.transpose(g, (2, 0, 1, 3)))
    mask = np.where(allowed, 0.0, -30000.0).astype(np.float32)
    m["na_mask_h"] = _f32(np.tile(mask[:, None, :], (1, 15, 1)).reshape(64, 15 * 64))


def _phasea_na(self):
    kb = self.kb
    bias_d = kb.din("na_bias_h", [64, 3, 15, 64])
    mask_d = kb.din("na_mask_h", [64, 15 * 64])
    wt = self.load_w("w_na", 768, 576)
    bias = kb.sb("na_bias", [64, 3, 15 * 64])
    mask = kb.sb("na_mask", [64, 15 * 64])
    ones = kb.sb("na_ones", [64, 64], BF16)
    PTs = Rot([(kb.sb("na_pt%d" % i, [64, 768], BF16), "na_pt%d" % i) for i in range(2)])
    recs = Rot([(kb.sb("na_rec%d" % i, [64, 64]), "na_rec%d" % i) for i in range(2)])
    kb.dma(bias[:], bias_d.rearrange("j h d w -> j h (d w)"), w=["na_bias"])
    kb.dma(mask[:], mask_d[:, :], w=["na_mask"])
    kb.memset("pool", ones[:], 1.0, w=["na_ones"])
    onesf = kb.sb("na_onesf", [64, 64])
    kb.memset("pool", onesf[:], 1.0, w=["na_onesf"])
    ptsums = Rot([(kb.sb("na_ptsum%d" % i, [64, 64]), "na_ptsum%d" % i) for i in range(2)])
    for hd in range(3):
        kb.tt("pool", bias[:, hd, :], bias[:, hd, :], mask[:], ALU.add, r=["na_bias", "na_mask"], w=["na_bias"])
    FPb = self.FP[:].bitcast(BF16)
    HB = 2 * T
    Vall = FPb[0:64, 0:68 * 192]
    qT = FPb[0:64, 2 * HB:2 * HB + T]
    kT = FPb[0:64, 2 * HB + T:3 * HB]
    yo = FPb[0:64, 3 * HB:3 * HB + T]
    sA = Rot(self.PS[0:2])
    sB = Rot(self.PS[2:4])
    sCD = Rot(self.PS[4:6])
    sP = Rot(self.PS[6:8])
    for u2 in range(34):
        pb, pbn = sP.next()
        for uu in range(2):
            u = u2 * 2 + uu
            for k in range(8):
                kb.mm(pb[0:64, uu * 192:(uu + 1) * 192], self.uT[:, k, u * 64:(u + 1) * 64], wt[:, k, 384:576], k == 0, k == 7,
                      r=["w_na", "uT.%d" % (u // 2)], w=[pbn])
        kb.copy("act" if u2 % 2 == 0 else "dve", Vall[:, u2 * 384:(u2 + 1) * 384], pb[0:64, 0:384], r=[pbn], w=["na_V.%d" % u2])
    for hd in range(3):
        for bi, blk in enumerate(BLKS):
            t0, t1 = blk
            pb, pbn = sP.next()
            self.proj(wt, "w_na", hd * 64, 64, blk, pb, pbn)
            kb.act(qT[:, t0:t1], pb[0:64, 0:t1 - t0], AF.Copy, r=[pbn], w=["na_q.%d" % bi], scale=0.125)
            pb, pbn = sP.next()
            self.proj(wt, "w_na", 192 + hd * 64, 64, blk, pb, pbn)
            kb.copy("dve", kT[:, t0:t1], pb[0:64, 0:t1 - t0], r=[pbn], w=["na_k.%d" % bi])

        def blk_of(t0, t1, pref):
            return ["%s.%d" % (pref, bi) for bi, (a, b) in enumerate(BLKS) if a < t1 and b > t0]

        def attend(q0, win_units, use_bias_dr0):
            qres = blk_of(q0, q0 + 64, "na_q")
            pt, ptn = PTs.next()
            units = []
            if win_units:
                pa_, pan = sA.next()
                for i, u in enumerate(win_units):
                    kb.mm(pa_[0:64, i * 64:(i + 1) * 64], kT[:, u * 64:(u + 1) * 64], qT[:, q0:q0 + 64], True, True,
                          r=qres + blk_of(u * 64, u * 64 + 64, "na_k"), w=[pan])
                tmp, tmpn = self.CP.next()
                dr0 = use_bias_dr0
                kb.tt("dve", tmp[0:64, :], pa_[0:64, :], bias[:, hd, dr0 * 64:(dr0 + 8) * 64], ALU.add, r=[pan, "na_bias"], w=[tmpn])
                kb.act(pt[:, 0:512], tmp[0:64, :], AF.Exp, r=[tmpn], w=[ptn])
                units += [(u, i * 64) for i, u in enumerate(win_units)]
            pb_, pbn_ = sB.next()
            for u in range(4):
                kb.mm(pb_[0:64, u * 64:(u + 1) * 64], kT[:, u * 64:(u + 1) * 64], qT[:, q0:q0 + 64], True, True,
                      r=qres + ["na_k.0"], w=[pbn_])
            kb.act(pt[:, 512:768], pb_[0:64, 0:256], AF.Exp, r=[pbn_], w=[ptn])
            units += [(u, 512 + u * 64) for u in range(4)]
            return (q0, pt, ptn, units)

        def attend2(ctx_):
            q0, pt, ptn, units = ctx_
            _sub = 9
            pcd, pcdn = sCD.next()
            nu = len(units)
            for i, (u, off) in enumerate(units):
                kb.mm(pcd[0:64, 0:64], Vall[:, u * 192 + hd * 64:u * 192 + (hd + 1) * 64], pt[:, off:off + 64], i == 0, i == nu - 1,
                      r=[ptn, "na_V.%d" % (u // 2)], w=[pcdn])
            if _sub < 2:
                return
            psm, psmn = ptsums.next()
            lo = units[0][1]
            pv = pt[:, lo:lo + nu * 64].rearrange("p (u q) -> p q u", q=64)
            kb.S.op("dve", lambda e, psm=psm, pv=pv: e.tensor_reduce(out=psm[:], in_=pv, axis=mybir.AxisListType.X, op=ALU.add),
                    reads=[ptn], writes=[psmn])
            kb.mm(pcd[0:64, 64:128], onesf[:], psm[:], True, True, r=[psmn, "na_onesf"], w=[pcdn])
            if _sub < 3:
                return
            rc, rcn = recs.next()
            kb.copy("act", rc[:], pcd[0:64, 64:128], r=[pcdn], w=[rcn])
            kb.recip(rc[:], rc[:], r=[rcn], w=[rcn])
            kb.tt("dve", rc[:], pcd[0:64, 0:64], rc[:], ALU.mult, r=[pcdn, rcn], w=[rcn])
            kb.copy("act", yo[:, q0:q0 + 64], rc[:], r=[rcn], w=["na_yo.%d" % (q0 // 512)])

        jobs = []
        if self.with_ctx:
            jobs += [(cu * 64, [], None) for cu in range(4)]
        for r in range(64):
            rs = min(max(r - 4, 0), 56)
            jobs.append((CTXL + 64 * r, [4 + rs + i for i in range(8)], rs - r + 7))
        prev = None
        for jb in jobs:
            cur = attend(*jb)
            if prev is not None:
                attend2(prev)
            prev = cur
        attend2(prev)
        t_lo = 0 if self.with_ctx else CTXL
        kb.dma(self.ybuf[320 + hd * 64:320 + (hd + 1) * 64, t_lo:T], yo[:, t_lo:T],
               r=["na_yo.%d" % i for i in range(9)], final=True)


PhaseA.na = _phasea_na


HY_MAX_DECAY = math.log(1e-2) / 0.3
HY_MIN_DECAY = math.log(1e-2) / 1.5
_HYC = {}


def hy_consts():
    if _HYC:
        return _HYC
    c = _HYC

    def feat(L):
        t = np.arange(L, dtype=np.float32)
        tn = t / np.float32(L)
        bands = np.arange(1, 17, dtype=np.float32)
        ang = np.float32(2.0 * math.pi / L) * t[:, None] * bands[None, :]
        return np.concatenate([tn[:, None], np.cos(ang), np.sin(ang)], axis=-1).astype(np.float32).T.copy()

    c["hy_feat_l_h"] = feat(SEQ)
    c["hy_feat_c_h"] = feat(CTXL)
    c["hy_t_h"] = np.tile(np.arange(SEQ, dtype=np.float32)[None, :], (128, 1))
    N = 8192
    n1 = np.arange(32)[:, None]
    k1 = np.arange(64)[None, :]
    a = 2 * np.pi * n1 * k1 / 64
    c["hy_W1f_h"] = np.concatenate([np.cos(a), -np.sin(a)], axis=1).astype(np.float32)
    n2 = np.arange(128)[:, None]
    a = 2 * np.pi * n2 * k1 / N
    T1 = np.stack([np.cos(a), -np.sin(a)], axis=0)
    c["hy_T1_h"] = np.ascontiguousarray(np.tile(T1[:, :, None, :], (1, 1, 16, 1)).transpose(1, 0, 2, 3)).astype(np.float32)
    k2 = np.arange(128)[None, :]
    a = 2 * np.pi * n2 * k2 / 128
    c["hy_W2_h"] = np.stack([np.cos(a), -np.sin(a), np.sin(a)], axis=1).astype(np.float32)
    G1 = np.concatenate([np.cos(a), np.sin(a)], axis=1)
    G2 = np.concatenate([-np.sin(a), np.cos(a)], axis=1)
    c["hy_G_h"] = np.stack([G1, G2], axis=1).astype(np.float32)
    kk1 = np.arange(64)[:, None]
    m2 = np.arange(128)[None, :]
    a = 2 * np.pi * kk1 * m2 / N
    T2 = np.stack([np.cos(a), np.sin(a)], axis=0)
    c["hy_T2_h"] = np.ascontiguousarray(np.tile(T2[:, :, None, :], (1, 1, 16, 1)).transpose(1, 0, 2, 3)).astype(np.float32)
    m1 = np.arange(32)[None, :]
    a = 2 * np.pi * kk1 * m1 / 64
    c["hy_W1i_h"] = (np.stack([np.cos(a), -np.sin(a)], axis=1) / N).astype(np.float32)
    t = np.arange(256)[:, None]
    k = np.arange(512)[None, :]
    a = 2 * np.pi * t * k / 512
    c["hy_Fc_h"] = np.stack([np.cos(a), -np.sin(a)], axis=1).astype(np.float32)
    a = 2 * np.pi * np.arange(512)[:, None] * np.arange(256)[None, :] / 512
    c["hy_Gc_h"] = (np.stack([np.cos(a), -np.sin(a)], axis=1) / 512).astype(np.float32)
    deltas = np.abs(np.linspace(HY_MIN_DECAY, HY_MAX_DECAY, HY_W, dtype=np.float32))
    c["_deltas"] = deltas
    return c


def prep_A_hy(inp, l, h, m):
    c = hy_consts()
    for k_, v_ in c.items():
        if not k_.startswith("_"):
            m[k_] = v_
    cw = []
    for j in range(3):
        idx = j * 256 + h * 128 + np.arange(128)
        cw.append(np.concatenate([inp["hy_conv_w"][l][:, idx].T, inp["hy_conv_b"][l][idx][:, None]], axis=1))
    m["hy_cw_h"] = _f32(np.concatenate(cw, axis=0))
    m["hy_w1_h"] = _f32(inp["hy_w1"][l])
    m["hy_w2_h"] = _f32(inp["hy_w2"][l])
    m["hy_vec_h"] = _f32(np.stack([inp["hy_b1"][l], inp["hy_b2"][l], inp["hy_freq"][l]], axis=1))
    cols = np.concatenate([(q * 256 + h * 128 + np.arange(128)) for q in range(4)])
    m["hy_w3_h"] = _f32(inp["hy_w3"][l][:, cols])
    m["hy_b3_h"] = _f32(inp["hy_b3"][l][cols].reshape(4, 128).T)
    m["hy_bias_h"] = _f32(inp["hy_bias"][l][:, h * 128:(h + 1) * 128].T)
    dl = c["_deltas"][h * 128:(h + 1) * 128]
    m["hy_delta_h"] = _f32(np.stack([-dl / np.float32(SEQ), -dl / np.float32(CTXL)], axis=1))


def _cmul(kb, ore, oim, are, aim, bre, bim, tmp, conj, r, wre, wim, wtmp):
    kb.tt("dve", ore, are, bre, ALU.mult, r=r, w=wre)
    kb.tt("dve", tmp, aim, bim, ALU.mult, r=r, w=wtmp)
    kb.tt("dve", ore, ore, tmp, ALU.add if conj else ALU.subtract, r=wre + wtmp, w=wre)
    kb.tt("dve", oim, are, bim, ALU.mult, r=r + wre + wtmp, w=wim)
    kb.tt("dve", tmp, aim, bre, ALU.mult, r=r + wre, w=wtmp)
    if conj:
        kb.tt("dve", oim, tmp, oim, ALU.subtract, r=wim + wtmp, w=wim)
    else:
        kb.tt("dve", oim, oim, tmp, ALU.add, r=wim + wtmp, w=wim)


def _phasea_hyena(self):
    kb = self.kb
    D_ = {n: kb.din(n, s) for n, s in [
        ("hy_feat_l_h", [33, SEQ]), ("hy_feat_c_h", [33, CTXL]), ("hy_t_h", [128, SEQ]), ("hy_W1f_h", [32, 128]),
        ("hy_T1_h", [128, 2, 16, 64]), ("hy_W2_h", [128, 3, 128]), ("hy_G_h", [128, 2, 256]), ("hy_T2_h", [64, 2, 16, 128]),
        ("hy_W1i_h", [64, 2, 32]), ("hy_Fc_h", [256, 2, 512]), ("hy_Gc_h", [512, 2, 256]),
        ("hy_cw_h", [384, 4]), ("hy_w1_h", [33, 64]), ("hy_w2_h", [64, 64]), ("hy_vec_h", [64, 3]), ("hy_w3_h", [64, 512]),
        ("hy_b3_h", [128, 4]), ("hy_bias_h", [128, 2]), ("hy_delta_h", [128, 2])]}
    hz = kb.dscratch("hy_hz", [3, 128, SEQ])
    hk = kb.dscratch("hy_hk", [4, 128, SEQ])
    wt = self.load_w("w_hy", 384, 384)

    def ld(name, shape, src):
        t_ = kb.sb(name, shape)
        kb.dma(t_[:], src, w=[name])
        return t_

    cw = ld("hy_cw", [128, 3, 4], D_["hy_cw_h"].rearrange("(j p) c -> p j c", p=128))
    w1 = ld("hy_w1", [33, 64], D_["hy_w1_h"][:, :])
    w2 = ld("hy_w2", [64, 64], D_["hy_w2_h"][:, :])
    vec = ld("hy_vec", [64, 3], D_["hy_vec_h"][:, :])
    w3 = ld("hy_w3", [64, 512], D_["hy_w3_h"][:, :])
    b3 = ld("hy_b3", [128, 4], D_["hy_b3_h"][:, :])
    hbias = ld("hy_bias", [128, 2], D_["hy_bias_h"][:, :])
    delta = ld("hy_delta", [128, 2], D_["hy_delta_h"][:, :])
    sm = kb.sb("hy_sm", [128, 24])
    kb.memset("dve", sm[:], 0.0, w=["hy_sm"])
    sm2 = kb.sb("hy_sm2", [128, 16])
    kb.ts("dve", sm[0:64, 0:1], vec[:, 2:3], 1.0 / TWO_PI, ALU.mult, r=["hy_vec"], w=["hy_sm"])
    psr = Rot(self.PS[0:4])

    zct = kc = None
    if self.with_ctx:
        zct = kb.sb("hy_zct", [128, 2, 3, 128])
        kc = kb.sb("hy_kc", [128, 4, CTXL])
    for j in range(3):
        for bi, blk in enumerate(BLKS):
            t0, t1 = blk
            pb, pbn = psr.next()
            self.proj(wt, "w_hy", j * 128, 128, blk, pb, pbn)
            kb.copy("act" if bi % 2 == 0 else "dve", self.F(3, t0, t1), pb[:, 0:t1 - t0], r=[pbn], w=["F3.%d" % bi])
        for (s0, s1) in [(0, CTXL), (CTXL, T)]:
            rr = self.fres(3, s0, s1)
            ww = self.fres(j, s0, s1)
            kb.ts("dve", self.F(j, s0, s1), self.F(3, s0, s1), cw[:, j, 1:2], ALU.mult, cw[:, j, 3:4], ALU.add, r=rr + ["hy_cw"], w=ww)
            kb.stt(self.F(j, s0 + 1, s1), self.F(3, s0, s1 - 1), cw[:, j, 0:1], self.F(j, s0 + 1, s1), ALU.mult, ALU.add, r=rr + ww + ["hy_cw"], w=ww)
            kb.stt(self.F(j, s0, s1 - 1), self.F(3, s0 + 1, s1), cw[:, j, 2:3], self.F(j, s0, s1 - 1), ALU.mult, ALU.add, r=rr + ww + ["hy_cw"], w=ww)
        kb.dma(hz[j], self.F(j, CTXL, T), r=self.fres(j, CTXL, T), w=["hz.%d" % j])
        if self.with_ctx:
            for tt_ in range(2):
                pb, pbn = psr.next()
                kb.tr(pb[:, 0:128], self.F(j, tt_ * 128, (tt_ + 1) * 128), self.ident[:], r=["F%d.0" % j, "ident"], w=[pbn])
                kb.copy("act", zct[:, tt_, j, :], pb[:, 0:128], r=[pbn], w=["hy_zct"])

    def sin_arg(dst, src_ps, P_, n, bcol, r, w):
        t0_, t0n = self.CP.next()
        t1_, t1n = self.CP.next()
        kb.ts("dve", t0_[0:P_, 0:n], src_ps, vec[0:P_, bcol:bcol + 1], ALU.add, sm[0:P_, 0:1], ALU.mult, r=r + ["hy_vec", "hy_sm"], w=[t0n])
        kb.ts("dve", t1_[0:P_, 0:n], t0_[0:P_, 0:n], MAGIC, ALU.add, r=[t0n], w=[t1n])
        kb.ts("dve", t1_[0:P_, 0:n], t1_[0:P_, 0:n], MAGIC, ALU.subtract, r=[t1n], w=[t1n])
        kb.tt("pool", t0_[0:P_, 0:n], t0_[0:P_, 0:n], t1_[0:P_, 0:n], ALU.subtract, r=[t0n, t1n], w=[t0n])
        kb.act(dst, t0_[0:P_, 0:n], AF.Sin, r=[t0n], w=w, scale=TWO_PI)

    cases = [("l", SEQ, 0)] + ([("c", CTXL, 1)] if self.with_ctx else [])
    for (cname, L, dcol) in cases:
        kb.S.barrier()
        nblk = max(1, L // 512)
        bw = min(L, 512)
        kb.dma(self.F(0, 0, L)[0:33], D_["hy_feat_%s_h" % cname][:, :], w=["hyF0"])
        for b_ in range(nblk):
            pb, pbn = psr.next()
            kb.mm(pb[0:64, 0:bw], w1[:, :], self.F(0, b_ * bw, (b_ + 1) * bw)[0:33], True, True, r=["hy_w1", "hyF0"], w=[pbn])
            sin_arg(self.F(1, b_ * bw, (b_ + 1) * bw)[0:64], pb[0:64, 0:bw], 64, bw, 0, [pbn], ["hyF1"])
        kb.S.barrier()
        for b_ in range(nblk):
            pb, pbn = psr.next()
            kb.mm(pb[0:64, 0:bw], w2[:, :], self.F(1, b_ * bw, (b_ + 1) * bw)[0:64], True, True, r=["hy_w2", "hyF1"], w=[pbn])
            sin_arg(self.F(0, b_ * bw, (b_ + 1) * bw)[0:64], pb[0:64, 0:bw], 64, bw, 1, [pbn], ["hyH2"])
        kb.S.barrier()
        kb.dma(self.F(3, 0, L), D_["hy_t_h"][:, 0:L], w=["hyF3"])
        kb.act(self.F(1, 0, L), self.F(3, 0, L), AF.Exp, r=["hyF3", "hy_delta"], w=["hyWin"], scale=delta[:, dcol:dcol + 1])
        kb.S.barrier()
        for o in range(2):
            for d in range(2):
                q = d * 2 + o
                for b_ in range(nblk):
                    pb, pbn = psr.next()
                    kb.mm(pb[:, 0:bw], w3[:, q * 128:(q + 1) * 128], self.F(0, b_ * bw, (b_ + 1) * bw)[0:64], True, True,
                          r=["hy_w3", "hyH2"], w=[pbn])
                    kb.stt(self.F(2 + d, b_ * bw, (b_ + 1) * bw), pb[:, 0:bw], b3[:, q:q + 1], self.F(1, b_ * bw, (b_ + 1) * bw),
                           ALU.add, ALU.mult, r=[pbn, "hy_b3", "hyWin"], w=["hyK%d" % d])
            kb.memset("dve", self.F(3, 0, 1), 0.0, r=["hyK1"], w=["hyK1"])
            for d in range(2):
                for b_ in range(nblk):
                    tb, tbn = self.CP.next()
                    xin_ = self.F(2 + d, b_ * bw, (b_ + 1) * bw)
                    kb.stt(tb[:, 0:bw], xin_, -1.0, xin_, ALU.mult, ALU.max, r=["hyK%d" % d], w=[tbn])
                    kb.S.op("dve", lambda e, d=d, b_=b_, tb=tb, bw=bw: e.tensor_reduce(out=sm[:, 8 + d * 8 + b_:9 + d * 8 + b_], in_=tb[:, 0:bw], axis=mybir.AxisListType.X, op=ALU.add),
                            reads=[tbn], writes=["hy_sm"])
            if nblk == 8:
                kb.tt("dve", sm2[:, 0:8], sm[:, 8:16], sm[:, 16:24], ALU.add, r=["hy_sm"], w=["hy_sm2"])
                kb.tt("dve", sm2[:, 8:12], sm2[:, 0:4], sm2[:, 4:8], ALU.add, r=["hy_sm2"], w=["hy_sm2"])
                kb.tt("dve", sm2[:, 12:14], sm2[:, 8:10], sm2[:, 10:12], ALU.add, r=["hy_sm2"], w=["hy_sm2"])
                kb.tt("dve", sm[:, 4:5], sm2[:, 12:13], sm2[:, 13:14], ALU.add, r=["hy_sm", "hy_sm2"], w=["hy_sm"])
            else:
                kb.tt("dve", sm[:, 4:5], sm[:, 8:9], sm[:, 16:17], ALU.add, r=["hy_sm"], w=["hy_sm"])
            if self.dbg and cname == "l":
                if o == 0:
                    self.dbg_sm = kb.dout("dbg_sm", [2, 128, 24])
                kb.dma(self.dbg_sm[o], sm[:, :], r=["hy_sm"], w=["dbg_sm%d" % o], final=True)
            kb.ts("dve", sm[:, 4:5], sm[:, 4:5], 1e-6, ALU.add, r=["hy_sm"], w=["hy_sm"])
            kb.recip(sm[:, 4:5], sm[:, 4:5], r=["hy_sm"], w=["hy_sm"])
            for d in range(2):
                kb.ts("dve" if d == 0 else "pool", self.F(2 + d, 0, L), self.F(2 + d, 0, L), sm[:, 4:5], ALU.mult, r=["hyK%d" % d, "hy_sm"], w=["hyK%d" % d])
            kb.tt("dve", self.F(2, 0, 1), self.F(2, 0, 1), hbias[:, o:o + 1], ALU.add, r=["hyK0", "hy_bias"], w=["hyK0"])
            for d in range(2):
                if cname == "l":
                    kb.dma(hk[o * 2 + d], self.F(2 + d, 0, L), r=["hyK%d" % d], w=["hk.%d" % (o * 2 + d)])
                else:
                    kb.copy("act", kc[:, o * 2 + d, :], self.F(2 + d, 0, L), r=["hyK%d" % d], w=["hy_kc"])
    kb.S.barrier()
    if self.dbg:
        return hz, hk, zct, kc, D_
    _hyena_conv(self, hz, hk, zct, kc, D_)


PhaseA.hyena = _phasea_hyena


def _hyena_conv(self, hz, hk, zct, kc, D_):
    kb = self.kb

    def ld(name, shape, src):
        t_ = kb.sb(name, shape)
        kb.dma(t_[:], src, w=[name])
        return t_

    W1f = ld("hy_W1f", [32, 128], D_["hy_W1f_h"][:, :])
    T1 = ld("hy_T1", [128, 2, 4, 64], D_["hy_T1_h"][:, :, 0:4, :])
    W2 = ld("hy_W2", [128, 3, 128], D_["hy_W2_h"][:, :, :])
    G = ld("hy_G", [128, 2, 256], D_["hy_G_h"][:, :, :])
    T2 = ld("hy_T2", [64, 2, 2, 128], D_["hy_T2_h"][:, :, 0:2, :])
    W1i = ld("hy_W1i", [64, 2, 32], D_["hy_W1i_h"][:, :, :])
    CONST = ["hy_W1f", "hy_T1", "hy_W2", "hy_G", "hy_T2", "hy_W1i"]
    pall = Rot(self.PS)
    off = [0]

    def carve(n):
        a = off[0]
        off[0] += n
        assert off[0] <= 4 * T
        return a

    def v3(a, P_, s, k):
        return self.FP[0:P_, a:a + s * k].rearrange("p (s k) -> p s k", k=k)

    srcs = Rot([(v3(carve(2048), 32, 16, 128), "hy_src%d" % i) for i in range(4)])
    AR = v3(carve(1024), 128, 16, 64)
    AI = v3(carve(1024), 128, 16, 64)
    KR = [v3(carve(1024), 128, 16, 64) for _ in range(2)]
    KI = [v3(carve(1024), 128, 16, 64) for _ in range(2)]
    XR = v3(carve(1024), 128, 16, 64)
    XI = v3(carve(1024), 128, 16, 64)
    tmpA = v3(carve(256), 128, 4, 64)
    tmpX = v3(carve(512), 128, 8, 64)
    Bb = Rot([(kb.sb("hy_Bb%d" % i, [64, 2, 4, 128]), "hy_Bb%d" % i) for i in range(1)])
    tmpB = kb.sb("hy_tmpB", [64, 2, 128])
    o32 = Rot([(kb.sb("hy_o32_%d" % i, [32, 4, 128]), "hy_o32_%d" % i) for i in range(1)])
    o16 = Rot([(kb.sb("hy_o16_%d" % i, [32, 16, 128], BF16), "hy_o16_%d" % i) for i in range(1)])

    def fft_fwd(src, srcn, consume):
        for sb4 in range(4):
            pA, pAn = pall.next()
            for s in range(4):
                kb.mm(pA[:, s * 128:(s + 1) * 128], src[:, sb4 * 4 + s, :], W1f[:, :], True, True, r=[srcn, "hy_W1f"], w=[pAn])
            v = pA[:, :].rearrange("p (s c k) -> p s c k", s=4, c=2)
            _cmul(kb, AR[:, sb4 * 4:(sb4 + 1) * 4, :], AI[:, sb4 * 4:(sb4 + 1) * 4, :], v[:, :, 0, :], v[:, :, 1, :],
                  T1[:, 0, :, :], T1[:, 1, :, :], tmpA, False, [pAn, "hy_T1"], ["hyAR"], ["hyAI"], ["hy_tmpA"])
        for b8 in range(2):
            pXr, pXrn = pall.next()
            pXi, pXin = pall.next()
            a_re = AR[:, b8 * 8:(b8 + 1) * 8, :].rearrange("p s k -> p (s k)")
            a_im = AI[:, b8 * 8:(b8 + 1) * 8, :].rearrange("p s k -> p (s k)")
            kb.mm(pXr[:, :], W2[:, 0, :], a_re, True, False, r=["hy_W2", "hyAR"], w=[pXrn])
            kb.mm(pXr[:, :], W2[:, 2, :], a_im, False, True, r=["hy_W2", "hyAI"], w=[pXrn])
            kb.mm(pXi[:, :], W2[:, 1, :], a_re, True, False, r=["hy_W2", "hyAR"], w=[pXin])
            kb.mm(pXi[:, :], W2[:, 0, :], a_im, False, True, r=["hy_W2", "hyAI"], w=[pXin])
            consume(b8, pXr[:, :].rearrange("p (s k) -> p s k", k=64), pXi[:, :].rearrange("p (s k) -> p s k", k=64), [pXrn, pXin])

    def ifft(consume):
        for sb4 in range(4):
            bb, bbn = Bb.next()
            for s2 in range(2):
                pB, pBn = pall.next()
                for s in range(2):
                    sig = sb4 * 4 + s2 * 2 + s
                    kb.mm(pB[0:64, s * 256:(s + 1) * 256], XR[:, sig, :], G[:, 0, :], True, False, r=["hyXR", "hy_G"], w=[pBn])
                    kb.mm(pB[0:64, s * 256:(s + 1) * 256], XI[:, sig, :], G[:, 1, :], False, True, r=["hyXI", "hy_G"], w=[pBn])
                v = pB[0:64, :].rearrange("p (s c m) -> p s c m", s=2, c=2)
                _cmul(kb, bb[:, 0, s2 * 2:(s2 + 1) * 2, :], bb[:, 1, s2 * 2:(s2 + 1) * 2, :], v[:, :, 0, :], v[:, :, 1, :],
                      T2[:, 0, :, :], T2[:, 1, :, :], tmpB[:, :, :], False, [pBn, "hy_T2"], [bbn], [bbn], ["hy_tmpB"])
            pY, pYn = pall.next()
            kb.mm(pY[0:32, :], W1i[:, 0, :], bb[:, 0, :, :].rearrange("p s m -> p (s m)"), True, False, r=["hy_W1i", bbn], w=[pYn])
            kb.mm(pY[0:32, :], W1i[:, 1, :], bb[:, 1, :, :].rearrange("p s m -> p (s m)"), False, True, r=["hy_W1i", bbn], w=[pYn])
            consume(sb4, pY[0:32, :].rearrange("p (s m) -> p s m", m=128), pYn)

    def load_src(dram_rows):
        s_, sn = srcs.next()
        kb.dma(s_, dram_rows.rearrange("c (a b) -> a c b", b=128), r=[], w=[sn])
        return s_, sn

    for g in range(8):
        c0 = g * 16
        for o in range(2):
            for d in range(2):
                s_, sn = srcs.next()
                kb.dma(s_, hk[o * 2 + d, c0:c0 + 16, :].rearrange("c (a b) -> a c b", b=128), r=["hk.%d" % (o * 2 + d)], w=[sn])

                def cons_k(b8, xr, xi, names, o=o, d=d):
                    sl = slice(b8 * 8, (b8 + 1) * 8)
                    if d == 0:
                        kb.copy("act", KR[o][:, sl, :], xr, r=names, w=["hyKR%d" % o])
                        kb.copy("act", KI[o][:, sl, :], xi, r=names, w=["hyKI%d" % o])
                    else:
                        kb.tt("dve", KR[o][:, sl, :], xr, KR[o][:, sl, :], ALU.add, r=names + ["hyKR%d" % o], w=["hyKR%d" % o])
                        kb.tt("dve", KI[o][:, sl, :], KI[o][:, sl, :], xi, ALU.subtract, r=names + ["hyKI%d" % o], w=["hyKI%d" % o])

                fft_fwd(s_, sn, cons_k)
        cur, curn = srcs.next()
        kb.dma(cur, hz[0, c0:c0 + 16, :].rearrange("c (a b) -> a c b", b=128), r=["hz.0"], w=[curn])
        for o in range(2):
            def cons_x(b8, xr, xi, names, o=o):
                sl = slice(b8 * 8, (b8 + 1) * 8)
                _cmul(kb, XR[:, sl, :], XI[:, sl, :], xr, xi, KR[o][:, sl, :], KI[o][:, sl, :], tmpX, False,
                      names + ["hyKR%d" % o, "hyKI%d" % o], ["hyXR"], ["hyXI"], ["hy_tmpX"])

            fft_fwd(cur, curn, cons_x)
            gt, gtn = srcs.next()
            kb.dma(gt, hz[1 + o, c0:c0 + 16, :].rearrange("c (a b) -> a c b", b=128), r=["hz.%d" % (1 + o)], w=[gtn])
            if o == 0:
                nxt, nxtn = srcs.next()

                def cons_y(sb4, py, pyn, gt=gt, gtn=gtn, nxt=nxt, nxtn=nxtn):
                    sl = slice(sb4 * 4, (sb4 + 1) * 4)
                    kb.tt("dve", nxt[:, sl, :], py, gt[:, sl, :], ALU.mult, r=[pyn, gtn], w=[nxtn])

                ifft(cons_y)
                cur, curn = nxt, nxtn
            else:
                ob, obn = o16.next()

                def cons_o(sb4, py, pyn, gt=gt, gtn=gtn, ob=ob, obn=obn):
                    sl = slice(sb4 * 4, (sb4 + 1) * 4)
                    t32, t32n = o32.next()
                    kb.tt("dve", t32[:, :, :], py, gt[:, sl, :], ALU.mult, r=[pyn, gtn], w=[t32n])
                    kb.copy("act", ob[:, sl, :], t32[:, :, :], r=[t32n], w=[obn])

                ifft(cons_o)
                kb.dma(self.ybuf[192 + c0:192 + c0 + 16, CTXL:T].rearrange("c (a b) -> a c b", b=128), ob[:, :, :], r=[obn], final=True)
    kb.S.barrier()
    if self.with_ctx:
        _hyena_ctx(self, zct, kc, D_)


def _hyena_ctx(self, zct, kc, D_):
    kb = self.kb
    pall = Rot(self.PS)
    Fc = self.FP[:, 0:2048].rearrange("p (t c k) -> p t c k", t=2, c=2)
    Gc = self.FP[:, 2048:4096].rearrange("p (q c t) -> p q c t", q=4, c=2)
    kb.dma(Fc, D_["hy_Fc_h"].rearrange("(t p) c k -> p t c k", p=128), w=["hyFc"])
    kb.dma(Gc, D_["hy_Gc_h"].rearrange("(q p) c t -> p q c t", p=128), w=["hyGc"])
    kct = self.FP[:, 4096:5120].rearrange("p (t q c) -> p t q c", t=2, q=4)
    KcR = [self.FP[:, 5120 + i * 512:5632 + i * 512].rearrange("p (q c) -> p q c", q=4) for i in range(2)]
    KcI = [self.FP[:, 6144 + i * 512:6656 + i * 512].rearrange("p (q c) -> p q c", q=4) for i in range(2)]
    YR = self.FP[:, 7168:7680].rearrange("p (q c) -> p q c", q=4)
    YI = self.FP[:, 7680:8192].rearrange("p (q c) -> p q c", q=4)
    tmp = self.FP[:, 8192:8704].rearrange("p (q c) -> p q c", q=4)
    y1 = self.FP[:, 8704:8960].rearrange("p (t c) -> p t c", t=2)
    yo = self.FP[:, 8960:9216].rearrange("p (t c) -> p t c", t=2)
    for q in range(4):
        for tt_ in range(2):
            pb, pbn = pall.next()
            kb.tr(pb[:, 0:128], kc[:, q, tt_ * 128:(tt_ + 1) * 128], self.ident[:], r=["hy_kc", "ident"], w=[pbn])
            kb.copy("act", kct[:, tt_, q, :], pb[:, 0:128], r=[pbn], w=["hy_kct"])

    def fwd(xfn, xres):
        pr, prn = pall.next()
        pi, pin = pall.next()
        for c_, (pp, ppn) in enumerate([(pr, prn), (pi, pin)]):
            for kq in range(4):
                for tt_ in range(2):
                    kb.mm(pp[:, kq * 128:(kq + 1) * 128], Fc[:, tt_, c_, kq * 128:(kq + 1) * 128], xfn(tt_), tt_ == 0, tt_ == 1,
                          r=["hyFc"] + xres, w=[ppn])
        return (pr[:, :].rearrange("p (q c) -> p q c", q=4), prn), (pi[:, :].rearrange("p (q c) -> p q c", q=4), pin)

    def inv(consume):
        pY, pYn = pall.next()
        for tt_ in range(2):
            for kq in range(4):
                kb.mm(pY[:, tt_ * 128:(tt_ + 1) * 128], Gc[:, kq, 0, tt_ * 128:(tt_ + 1) * 128], YR[:, kq, :], kq == 0, False, r=["hyGc", "hyYR"], w=[pYn])
                kb.mm(pY[:, tt_ * 128:(tt_ + 1) * 128], Gc[:, kq, 1, tt_ * 128:(tt_ + 1) * 128], YI[:, kq, :], False, kq == 3, r=["hyGc", "hyYI"], w=[pYn])
        consume(pY[:, 0:256].rearrange("p (t c) -> p t c", t=2), pYn)

    for o in range(2):
        (xr, xrn), (xi, xin) = fwd(lambda tt_, o=o: kct[:, tt_, o * 2 + 0, :], ["hy_kct"])
        kb.copy("act", KcR[o], xr, r=[xrn], w=["hyKcR%d" % o])
        kb.copy("act", KcI[o], xi, r=[xin], w=["hyKcI%d" % o])
        (xr, xrn), (xi, xin) = fwd(lambda tt_, o=o: kct[:, tt_, o * 2 + 1, :], ["hy_kct"])
        kb.tt("dve", KcR[o], xr, KcR[o], ALU.add, r=[xrn, "hyKcR%d" % o], w=["hyKcR%d" % o])
        kb.tt("dve", KcI[o], KcI[o], xi, ALU.subtract, r=[xin, "hyKcI%d" % o], w=["hyKcI%d" % o])
    cur = lambda tt_: zct[:, tt_, 0, :]
    cres = ["hy_zct"]
    for o in range(2):
        (xr, xrn), (xi, xin) = fwd(cur, cres)
        _cmul(kb, YR, YI, xr, xi, KcR[o], KcI[o], tmp, False, [xrn, xin, "hyKcR%d" % o, "hyKcI%d" % o], ["hyYR"], ["hyYI"], ["hyTmpc"])
        if o == 0:
            inv(lambda py, pyn: kb.tt("dve", y1, py, zct[:, :, 1, :], ALU.mult, r=[pyn, "hy_zct"], w=["hyY1"]))
            cur = lambda tt_: y1[:, tt_, :]
            cres = ["hyY1"]
        else:
            inv(lambda py, pyn: kb.tt("dve", yo, py, zct[:, :, 2, :], ALU.mult, r=[pyn, "hy_zct"], w=["hyYo"]))
    ob, obn = self.ost.next()
    for tt_ in range(2):
        pb, pbn = pall.next()
        kb.tr(pb[:, 0:128], yo[:, tt_, :], self.ident[:], r=["hyYo", "ident"], w=[pbn])
        kb.copy("act", ob[:, tt_ * 128:(tt_ + 1) * 128], pb[:, 0:128], r=[pbn], w=[obn])
    kb.dma(self.ybuf[192:320, 0:CTXL], ob[:, 0:CTXL], r=[obn], final=True)


class PhaseB:
    def __init__(self, layer, dbg=False, kb=None, xtok=None, yload=None, xout=None, xtok_fn=None, xout_fn=None, xload=None, modT=None):
        self.layer = layer
        self.with_ctx = layer == 0
        self.moe = layer % 2 == 1
        self.standalone = kb is None
        kb = self.kb = kb if kb is not None else KB()
        NTB = self.NTB = 17 if self.with_ctx else 16
        NTOK = self.NTOK = NTB * 128
        if xtok_fn is None and xload is None:
            self.xtok = xtok if xtok is not None else kb.din("xtok", [NTOK, D])
            xtok_fn = lambda i: self.xtok[i * 128:(i + 1) * 128, :]
        self.xtok_fn = xtok_fn
        self.xload = xload
        self.yload = yload
        if yload is None:
            self.yT = kb.din("yT", [1280, NTOK], BF16)
        if modT is None:
            self.cvT = kb.din("cvT", [D, 2])
            self.wmod = kb.din("wmodB", [D, 6144])
            self.bmodT = kb.din("bmodT", [128, 48])
        self.ident_d = kb.din("ident_d", [128, 128])
        self.w_g_d = kb.din("w_g_h", [D, 4096])
        self.w_br_d = kb.din("w_br_h", [14, 128, D])
        self.w_out_d = kb.din("w_out_h", [D, D])
        self.wglu_d = kb.din("s5_wglu_h", [256, 256])
        self.bglu_d = kb.din("s5_bglu_h", [128, 2])
        self.lnp_d = kb.din("lnp_h", [4, 128, D])
        if xout_fn is None:
            self.xout = xout if xout is not None else kb.dout("xout", [NTOK, D])
            xout_fn = lambda i: self.xout[i * 128:(i + 1) * 128, :]
        self.xout_fn = xout_fn
        self.x1buf = kb.dscratch("x1buf%d" % kb.__dict__.setdefault("_nb", 0), [NTOK, D])
        self.u2T_d = kb.dscratch("u2T_d%d" % kb.__dict__["_nb"], [128, 8, NTOK], BF16)
        kb.__dict__["_nb"] += 1
        self.ident = kb.sb("ident", [128, 128])
        kb.dma(self.ident[:], self.ident_d[:, :], w=["ident"])
        self.identb = kb.sb("identb", [128, 128], BF16)
        kb.copy("dve", self.identb[:], self.ident[:], r=["ident"], w=["identb"])
        self.PS = [(kb.ps("PS%d" % i), "PS%d" % i) for i in range(7)]
        self.pT = kb.ps("PSTb", [128, 1024], BF16)
        self.pT32 = self.pT[:, :].bitcast(F32)
        self.modT = modT if modT is not None else kb.sb("modT", [128, 48, 2])
        if self.moe:
            self.gates = kb.sb("gates", [128, NTB, NEXP])
        if modT is None:
            with kb.scope():
                wmb = Rot([(kb.sb("wmb%d" % i, [128, 8, 256]), "wmb%d" % i) for i in range(2)])
                emit_modT(kb, self.wmod, self.bmodT, self.cvT, 48, self.modT, self.PS[6][0], "PS6", wmb)
                for c0 in (8, 32):
                    kb.ts("dve", self.modT[:, c0:c0 + 8, :], self.modT[:, c0:c0 + 8, :], 1.0, ALU.add, r=["modT"], w=["modT"])
        with kb.scope():
            self.load_gl(0)
            self.mix()
        with kb.scope():
            self.u2T = kb.sb("u2T", [128, 8, NTOK], BF16)
            for i in range(NTB):
                kb.dma(self.u2T[:, :, i * 128:(i + 1) * 128], self.u2T_d[:, :, i * 128:(i + 1) * 128], r=["u2Td.%d" % i], w=["u2T.%d" % i])
            self.load_gl(1)
            if self.moe:
                self.ffn_moe()
            else:
                self.ffn_dense()
        if self.standalone:
            self.nc = kb.finish()

    def load_gl(self, which):
        kb = self.kb
        sfx = "_%d" % which
        self.gbc = kb.sb("gbc" + sfx, [128, 2, D])
        self.gbcn = "gbc" + sfx
        self.lnp = kb.sb("lnp" + sfx, [128, 2, D])
        self.lnpn = "lnp" + sfx
        kb.dma(self.lnp[:], self.lnp_d[which * 2:which * 2 + 2].rearrange("a p d -> p a d"), w=[self.lnpn])
        cbase = (16, 40)[which]
        with kb.scope():
            ones = kb.sb("ones_f" + sfx, [128, 128])
            kb.memset("dve", ones[:], 1.0, w=["ones_f"])
            gb = Rot([(kb.sb("gb%d" % i + sfx, [128, 128]), "gb%d" % i) for i in range(2)])
            for j in range(2):
                for half in range(2):
                    pb, pbn = self.PS[half]
                    for kk in range(4):
                        k = half * 4 + kk
                        g_, gn = gb.next()
                        kb.act(g_[:], ones[:], AF.Copy, r=["ones_f", "modT"], w=[gn], scale=self.modT[:, cbase + k, j:j + 1])
                        kb.mm(pb[:, kk * 128:(kk + 1) * 128], g_[:], self.ident[:], True, True, r=[gn, "ident"], w=[pbn])
                    kb.copy("act", self.gbc[:, j, half * 512:(half + 1) * 512], pb[:, :], r=[pbn], w=[self.gbcn])

    def jof(self, i):
        return 1 if (self.with_ctx and i == 0) else 0

    def mix(self):
        kb = self.kb
        NTB = self.NTB
        w_g = kb.sb("w_g", [128, 8, 4096], BF16)
        for nb in range(8):
            kb.dma_cast(w_g[:, :, nb * 512:(nb + 1) * 512], self.w_g_d[:, nb * 512:(nb + 1) * 512].rearrange("(k p) n -> p k n", p=128), w=["w_g.%d" % nb])
        w_br = kb.sb("w_br", [128, 14, D], BF16)
        kb.dma_cast(w_br[:], self.w_br_d.rearrange("c p n -> p c n"), w=["w_br"])
        w_out = kb.sb("w_out", [128, 8, D], BF16)
        kb.dma_cast(w_out[:], self.w_out_d.rearrange("(k p) n -> p k n", p=128), w=["w_out"])
        wglu = kb.sb("wglu", [128, 2, 256], BF16)
        kb.dma_cast(wglu[:], self.wglu_d.rearrange("(k p) n -> p k n", p=128), w=["wglu"])
        bglu = kb.sb("bglu", [128, 2])
        kb.dma(bglu[:], self.bglu_d[:, :], w=["bglu"])
        lnt = LNT(kb, self.ident, [self.PS[0], self.PS[1]], xn_bufs=1)
        if self.moe:
            _pb_router_setup(self)
        uTs = Rot([(kb.sb("uTt%d" % i, [128, 8, 128], BF16), "uTt%d" % i) for i in range(1)])
        u2ts = Rot([(kb.sb("u2t%d" % i, [128, 8, 128], BF16), "u2t%d" % i) for i in range(1)])
        Gs = Rot([(kb.sb("G%d" % i, [128, 4096], BF16), "G%d" % i) for i in range(1)])
        yts = Rot([(kb.sb("yt%d" % i, [128, 10, 128], BF16), "yt%d" % i) for i in range(1)])
        if self.xload is not None:
            self.ybl = Rot([(kb.sb("ybl%d" % i, [128, 10, 128], BF16), "ybl%d" % i) for i in range(2)])
        yds = Rot([(kb.sb("yd%d" % i, [128, 2, 128], BF16), "yd%d" % i) for i in range(1)])
        mgs = Rot([(kb.sb("mg%d" % i, [128, D]), "mg%d" % i) for i in range(1)])
        mgb = Rot([(kb.sb("mgb%d" % i, [128, D], BF16), "mgb%d" % i) for i in range(1)])
        mTs = Rot([(kb.sb("mT%d" % i, [128, 8, 128], BF16), "mT%d" % i) for i in range(1)])
        tmps = Rot([(kb.sb("tmpB%d" % i, [128, 512]), "tmpB%d" % i) for i in range(2)])
        x1s = Rot([(kb.sb("x1t%d" % i, [128, D]), "x1t%d" % i) for i in range(1)])
        pT = self.pT
        pg = Rot(self.PS[2:5])
        po = Rot(self.PS[5:7])
        BRP = [[(0, 0), (1, 1), (2, 5), (3, 6)],
               [(4, 1), (5, 2), (6, 6), (7, 7)],
               [(8, 2), (9, 3), (10, 7), (11, 8)],
               [(12, 4), (13, 9)]]
        for i in range(NTB):
            j = self.jof(i)
            uT, uTn = uTs.next()
            if self.xload is not None:
                xg_ = self.xload(self, i, lnt)
                xt, xtn, _, _ = lnt.run(None, [], lambda k, uT=uT: uT[:, k, :], [uTn],
                                        lambda k, j=j: self.modT[:, 8 + k, j:j + 1], lambda k, j=j: self.modT[:, k, j:j + 1], xt_given=xg_)
            else:
                xt, xtn, _, _ = lnt.run(self.xtok_fn(i), ["xsrc"], lambda k, uT=uT: uT[:, k, :], [uTn],
                                        lambda k, j=j: self.modT[:, 8 + k, j:j + 1], lambda k, j=j: self.modT[:, k, j:j + 1])
            xtn = list(xtn) if isinstance(xtn, (list, tuple)) else [xtn]
            G, Gn = Gs.next()
            for nb in range(8):
                pb, pbn = pg.next()
                for k in range(8):
                    kb.mm(pb[:, :], uT[:, k, :], w_g[:, k, nb * 512:(nb + 1) * 512], k == 0, k == 7, r=[uTn, "w_g.%d" % nb], w=[pbn])
                kb.act(G[:, nb * 512:(nb + 1) * 512], pb[:, :], AF.Sigmoid, r=[pbn], w=[Gn])
            yt, ytn = yts.next()
            if self.yload is not None:
                self.yload(self, i, yt, ytn)
            else:
                kb.dma(yt[:], self.yT[:, i * 128:(i + 1) * 128].rearrange("(c p) t -> p c t", p=128), w=[ytn])
            yd, ydn = yds.next()
            for oc in range(2):
                pb, pbn = pg.next()
                for k in range(2):
                    kb.mm(pb[:, 0:128], wglu[:, k, oc * 128:(oc + 1) * 128], yt[:, 4 + 5 * k, :], k == 0, k == 1, r=["wglu", ytn], w=[pbn])
                tb, tbn = tmps.next()
                kb.act(tb[:, 0:128], pb[:, 0:128], AF.Sigmoid, r=[pbn, "bglu"], w=[tbn], bias=bglu[:, oc:oc + 1])
                kb.tt("pool", yd[:, oc, :], tb[:, 0:128], yt[:, 4 + 5 * oc, :], ALU.mult, r=[tbn, ytn], w=[ydn])
            mg, mgn = mgs.next()
            for bi_, pieces in enumerate(BRP):
                for nb in range(2):
                    pb, pbn = pg.next()
                    for cc, (pc, ch) in enumerate(pieces):
                        lhs = yd[:, ch // 5, :] if bi_ == 3 else yt[:, ch, :]
                        kb.mm(pb[:, :], lhs, w_br[:, pc, nb * 512:(nb + 1) * 512], cc == 0, cc == len(pieces) - 1,
                              r=[ydn if bi_ == 3 else ytn, "w_br"], w=[pbn])
                    gsl = G[:, bi_ * 1024 + nb * 512:bi_ * 1024 + (nb + 1) * 512]
                    if bi_ == 0:
                        kb.tt("dve", mg[:, nb * 512:(nb + 1) * 512], pb[:, :], gsl, ALU.mult, r=[pbn, Gn], w=[mgn + ".%d" % nb])
                    else:
                        tb, tbn = tmps.next()
                        kb.tt("dve", tb[:, :], pb[:, :], gsl, ALU.mult, r=[pbn, Gn], w=[tbn])
                        kb.tt("pool", mg[:, nb * 512:(nb + 1) * 512], mg[:, nb * 512:(nb + 1) * 512], tb[:, :], ALU.add,
                              r=[tbn, mgn + ".%d" % nb], w=[mgn + ".%d" % nb])
            mb, mbn = mgb.next()
            kb.copy("act", mb[:], mg[:], r=[mgn + ".0", mgn + ".1"], w=[mbn])
            mT, mTn = mTs.next()
            for k in range(8):
                kb.tr(pT[:, k * 128:(k + 1) * 128], mb[:, k * 128:(k + 1) * 128], self.identb[:], r=[mbn, "identb"], w=["PSTb"])
            kb.copy("dve", mT[:].rearrange("p k t -> p (k t)"), pT[:, :], r=["PSTb"], w=[mTn])
            x1, x1n = x1s.next()
            for nb in range(2):
                pb, pbn = po.next()
                for k in range(8):
                    kb.mm(pb[:, :], mT[:, k, :], w_out[:, k, nb * 512:(nb + 1) * 512], k == 0, k == 7, r=[mTn, "w_out"], w=[pbn])
                sl = slice(nb * 512, (nb + 1) * 512)
                kb.tt("dve", x1[:, sl], pb[:, :], self.gbc[:, j, sl], ALU.mult, r=[pbn, self.gbcn], w=[x1n + ".%d" % nb])
                kb.stt(x1[:, sl], xt[:, sl], ALPHA, x1[:, sl], ALU.mult, ALU.add, r=xtn + [x1n + ".%d" % nb], w=[x1n + ".%d" % nb])
            x1res = [x1n + ".0", x1n + ".1"]
            self.ln_affine(lnt, x1, x1res, 0)
            kb.dma(self.x1buf[i * 128:(i + 1) * 128, :], x1[:], r=x1res, w=["x1buf.%d" % i])
            d32 = d32r = None
            if self.moe and not _os.environ.get("B1_NO32"):
                u32, u32n = self.u32.next()
                self.cur_u32 = (u32, u32n)
                d32, d32r = (lambda k, u32=u32: u32[:, k, :]), [u32n]
            u2t, u2tn = u2ts.next()
            lnt.run(None, [], lambda k, u2t=u2t: u2t[:, k, :], [u2tn],
                    lambda k, j=j: self.modT[:, 32 + k, j:j + 1], lambda k, j=j: self.modT[:, 24 + k, j:j + 1], xt_given=(x1, x1res),
                    dst32_fn=d32, dst32_res=d32r)
            kb.dma(self.u2T_d[:, :, i * 128:(i + 1) * 128], u2t[:], r=[u2tn], w=["u2Td.%d" % i])
            if self.moe and not _os.environ.get("B1_NOROUTER"):
                self.router(i, lnt)

    def ln_affine(self, lnt, x, xres, which):
        kb = self.kb
        st, stn = lnt.stats_multi(x, xres)
        kb.ts("dve", x[:], x[:], st[:, 12:13], ALU.subtract, st[:, 14:15], ALU.mult, r=xres + [stn], w=xres)
        kb.tt("pool", x[:], x[:], self.lnp[:, 0, :], ALU.mult, r=xres + [self.lnpn], w=xres)
        kb.tt("dve", x[:], x[:], self.lnp[:, 1, :], ALU.add, r=xres + [self.lnpn], w=xres)


def _pb_router_setup(self):
    kb = self.kb
    self.wr_d = kb.din("moe_router_h", [D, NEXP])
    self.wr = kb.sb("wr", [128, 8, NEXP])
    kb.dma(self.wr[:], self.wr_d.rearrange("(k p) e -> p k e", p=128), w=["wr"])
    self.u32 = Rot([(kb.sb("u32_%d" % i, [128, 8, 128]), "u32_%d" % i) for i in range(1)])
    self.rt = kb.sb("rt", [128, 40])


def _pb_router(self, i):
    kb = self.kb
    u32, u32n = self.cur_u32
    pb, pbn = self.PS[2]
    for k in range(8):
        kb.mm(pb[:, 0:NEXP], u32[:, k, :], self.wr[:, k, :], k == 0, k == 7, r=[u32n, "wr"], w=[pbn])
    rt = self.rt
    R_ = ["rt"]
    lg, m1, mk1, lg2, m2, mk2, e1, w1, w2 = (rt[:, 0:8], rt[:, 8:9], rt[:, 9:17], rt[:, 17:25], rt[:, 25:26], rt[:, 26:34],
                                               rt[:, 34:35], rt[:, 35:36], rt[:, 36:37])
    kb.copy("dve", lg, pb[:, 0:NEXP], r=[pbn], w=R_)
    kb.S.op("dve", lambda e: e.tensor_reduce(out=m1, in_=lg, axis=mybir.AxisListType.X, op=ALU.max), reads=R_, writes=R_)
    kb.ts("dve", mk1, lg, m1, ALU.is_equal, r=R_, w=R_)
    kb.stt(lg2, mk1, -1e30, lg, ALU.mult, ALU.add, r=R_, w=R_)
    kb.S.op("dve", lambda e: e.tensor_reduce(out=m2, in_=lg2, axis=mybir.AxisListType.X, op=ALU.max), reads=R_, writes=R_)
    kb.ts("dve", mk2, lg2, m2, ALU.is_equal, r=R_, w=R_)
    kb.tt("dve", e1, m2, m1, ALU.subtract, r=R_, w=R_)
    kb.act(e1, e1, AF.Exp, r=R_, w=R_)
    kb.ts("dve", w1, e1, 1.0, ALU.add, r=R_, w=R_)
    kb.recip(w1, w1, r=R_, w=R_)
    kb.tt("dve", w2, e1, w1, ALU.mult, r=R_, w=R_)
    g = self.gates[:, i, :]
    kb.ts("dve", g, mk1, w1, ALU.mult, r=R_, w=["gates.%d" % i])
    kb.stt(g, mk2, w2, g, ALU.mult, ALU.add, r=R_ + ["gates.%d" % i], w=["gates.%d" % i])


def _pb_final(self, lnt, i, ffn_ap, ffn_res, x1s, tmps):
    kb = self.kb
    j = self.jof(i)
    x1, x1n = x1s.next()
    kb.dma(x1[:], self.x1buf[i * 128:(i + 1) * 128, :], r=["x1buf.%d" % i], w=[x1n])
    for nb in range(2):
        sl = slice(nb * 512, (nb + 1) * 512)
        tb, tbn = tmps.next()
        kb.tt("dve", tb[:, :], ffn_ap(nb), self.gbc[:, j, sl], ALU.mult, r=ffn_res(nb) + [self.gbcn], w=[tbn])
        kb.stt(x1[:, sl], x1[:, sl], ALPHA, tb[:, :], ALU.mult, ALU.add, r=[tbn, x1n], w=[x1n])
    self.ln_affine(lnt, x1, [x1n], 1)
    kb.dma(self.xout_fn(i), x1[:], r=[x1n], w=["xout.%d" % i], final=True)


def _pb_ffn_dense(self):
    kb = self.kb
    NTB = self.NTB
    wg_d = kb.din("ff_w_gate", [D, FF_DENSE])
    wu_d = kb.din("ff_w_up", [D, FF_DENSE])
    wd_d = kb.din("ff_w_down", [FF_DENSE, D])
    NF = FF_DENSE // 128
    wg = kb.sb("ffwg", [128, 8, FF_DENSE], BF16)
    wu = kb.sb("ffwu", [128, 8, FF_DENSE], BF16)
    wd = kb.sb("ffwd", [128, NF, D], BF16)
    for c in range(0, NF, 2):
        sl = slice(c * 128, min(NF, c + 2) * 128)
        kb.dma_cast(wg[:, :, sl], wg_d[:, sl].rearrange("(k p) n -> p k n", p=128), w=["ffwg.%d" % (c // 2)])
        kb.dma_cast(wu[:, :, sl], wu_d[:, sl].rearrange("(k p) n -> p k n", p=128), w=["ffwu.%d" % (c // 2)])
        kb.dma_cast(wd[:, c:c + 2, :], wd_d[sl, :].rearrange("(c p) n -> p c n", p=128), w=["ffwd.%d" % (c // 2)])
    lnt = LNT(kb, self.ident, [self.PS[0], self.PS[1]], light=True)
    x1s = Rot([(kb.sb("x1f%d" % i, [128, D]), "x1f%d" % i) for i in range(1)])
    tmps = Rot([(kb.sb("tmpF%d" % i, [128, 512]), "tmpF%d" % i) for i in range(2)])
    hTs = Rot([(kb.sb("hT%d" % i, [128, 256], BF16), "hT%d" % i) for i in range(1)])
    pgu = Rot(self.PS[4:7])
    groups = [(i0, min(2, NTB - i0)) for i0 in range(0, NTB, 2)]
    for (i0, n) in groups:
        t0, nt = i0 * 128, n * 128
        ures = ["u2T.%d" % (i0 + q) for q in range(n)]
        for f in range(NF):
            pgt, pgtn = pgu.next()
            put, putn = pgu.next()
            for k in range(8):
                kb.mm(pgt[:, 0:nt], wg[:, k, f * 128:(f + 1) * 128], self.u2T[:, k, t0:t0 + nt], k == 0, k == 7, r=["ffwg.%d" % (f // 2)] + ures, w=[pgtn])
            for k in range(8):
                kb.mm(put[:, 0:nt], wu[:, k, f * 128:(f + 1) * 128], self.u2T[:, k, t0:t0 + nt], k == 0, k == 7, r=["ffwu.%d" % (f // 2)] + ures, w=[putn])
            s_, sn = tmps.next()
            u_, un = tmps.next()
            kb.act(s_[:, 0:nt], pgt[:, 0:nt], AF.Silu, r=[pgtn], w=[sn])
            kb.copy("act", u_[:, 0:nt], put[:, 0:nt], r=[putn], w=[un])
            hT, hTn = hTs.next()
            kb.tt("pool", hT[:, 0:nt], s_[:, 0:nt], u_[:, 0:nt], ALU.mult, r=[sn, un], w=[hTn])
            for q in range(n):
                for nb in range(2):
                    pa_, pan = self.PS[q * 2 + nb]
                    kb.mm(pa_[:, :], hT[:, q * 128:(q + 1) * 128], wd[:, f, nb * 512:(nb + 1) * 512], f == 0, f == NF - 1,
                          r=[hTn, "ffwd.%d" % (f // 2)], w=[pan])
        for q in range(n):
            _pb_final(self, lnt, i0 + q, lambda nb, q=q: self.PS[q * 2 + nb][0][:, :], lambda nb, q=q: [self.PS[q * 2 + nb][1]], x1s, tmps)


def _pb_ffn_moe(self):
    kb = self.kb
    NTB = self.NTB
    if int(_os.environ.get("B1_NEXP", NEXP)) > 0:
        wg_d = kb.din("moe_w_gate", [NEXP, D, FF_EXP])
        wu_d = kb.din("moe_w_up", [NEXP, D, FF_EXP])
        wd_d = kb.din("moe_w_down", [NEXP, FF_EXP, D])
    FG = 4
    NG = FF_EXP // (FG * 128)
    acc = kb.sb("moe_acc", [128, NTB, D])
    for i in range(NTB):
        kb.memset("pool" if i % 2 else "dve", acc[:, i, :], 0.0, w=["acc.%d" % i])
    wgs = Rot([(kb.sb("mwg%d" % i, [128, 8, FG * 128], BF16), "mwg%d" % i) for i in range(2)])
    wus = Rot([(kb.sb("mwu%d" % i, [128, 8, FG * 128], BF16), "mwu%d" % i) for i in range(2)])
    wds = Rot([(kb.sb("mwd%d" % i, [128, FG, D], BF16), "mwd%d" % i) for i in range(2)])
    tmps = Rot([(kb.sb("tmpF%d" % i, [128, 512]), "tmpF%d" % i) for i in range(4)])
    hTs = Rot([(kb.sb("hT%d" % i, [128, FG, 512], BF16), "hT%d" % i) for i in range(2)])
    pgu = Rot(self.PS[0:3])
    pac = Rot([(self.PS[3], self.PS[4]), (self.PS[5], self.PS[6])])
    nblk = NTB // 4
    def _load(e, fg):
        c0 = fg * FG * 128
        wg, wgn = wgs.next()
        wu, wun = wus.next()
        wd, wdn = wds.next()
        kb.dma_cast(wg[:], wg_d[e, :, c0:c0 + FG * 128].rearrange("(k p) n -> p k n", p=128), w=[wgn])
        kb.dma_cast(wu[:], wu_d[e, :, c0:c0 + FG * 128].rearrange("(k p) n -> p k n", p=128), w=[wun])
        kb.dma_cast(wd[:], wd_d[e, c0:c0 + FG * 128, :].rearrange("(c p) n -> p c n", p=128), w=[wdn])
        return wg, wgn, wu, wun, wd, wdn

    _groups = [(e, fg) for e in range(int(_os.environ.get("B1_NEXP", NEXP))) for fg in range(NG)]
    _nxt = _load(*_groups[0]) if _groups else None
    for _gi, (e, fg) in enumerate(_groups):
        if True:
            wg, wgn, wu, wun, wd, wdn = _nxt
            if _gi + 1 < len(_groups):
                _nxt = _load(*_groups[_gi + 1])
            for blk in range(nblk):
                t0 = blk * 512
                ures = ["u2T.%d" % (blk * 4 + q) for q in range(4)]
                hT, hTn = hTs.next()
                for c in range(FG):
                    pgt, pgtn = pgu.next()
                    put, putn = pgu.next()
                    for k in range(8):
                        kb.mm(pgt[:, :], wg[:, k, c * 128:(c + 1) * 128], self.u2T[:, k, t0:t0 + 512], k == 0, k == 7, r=[wgn] + ures, w=[pgtn])
                    for k in range(8):
                        kb.mm(put[:, :], wu[:, k, c * 128:(c + 1) * 128], self.u2T[:, k, t0:t0 + 512], k == 0, k == 7, r=[wun] + ures, w=[putn])
                    s_, sn = tmps.next()
                    u_, un = tmps.next()
                    kb.act(s_[:, :], pgt[:, :], AF.Silu, r=[pgtn], w=[sn])
                    kb.copy("act", u_[:, :], put[:, :], r=[putn], w=[un])
                    kb.tt("pool", hT[:, c, :], s_[:, :], u_[:, :], ALU.mult, r=[sn, un], w=[hTn + ".%d" % c])
                for q in range(4):
                    i = blk * 4 + q
                    banks = pac.next()
                    for nb in range(2):
                        pa_, pan = banks[nb]
                        for c in range(FG):
                            kb.mm(pa_[:, 0:512], hT[:, c, q * 128:(q + 1) * 128], wd[:, c, nb * 512:(nb + 1) * 512], c == 0, c == FG - 1,
                                  r=[hTn + ".%d" % c, wdn], w=[pan])
                        sl = slice(nb * 512, (nb + 1) * 512)
                        kb.stt(acc[:, i, sl], pa_[:, 0:512], self.gates[:, i, e:e + 1], acc[:, i, sl], ALU.mult, ALU.add,
                               r=[pan, "gates.%d" % i, "acc.%d" % i], w=["acc.%d" % i])
    lnt = LNT(kb, self.ident, [self.PS[0], self.PS[1]], light=True)
    x1s = Rot([(kb.sb("x1f%d" % i, [128, D]), "x1f%d" % i) for i in range(2)])
    for i in range(NTB):
        _pb_final(self, lnt, i, lambda nb, i=i: acc[:, i, nb * 512:(nb + 1) * 512], lambda nb, i=i: ["acc.%d" % i], x1s, tmps)


PhaseB.router = lambda self, i, lnt: _pb_router(self, i)
PhaseB.ffn_dense = _pb_ffn_dense
PhaseB.ffn_moe = _pb_ffn_moe


def prep_B(inp, l, b, h, xs, ctxs, yfull):
    with_ctx = l == 0
    m = {}
    lat = slice(h * 2048, (h + 1) * 2048)
    if with_ctx:
        cs = slice(h * 128, (h + 1) * 128)
        m["xtok"] = _f32(np.concatenate([ctxs[b][cs], xs[b][lat]], axis=0))
        cols = np.concatenate([np.arange(h * 128, (h + 1) * 128), CTXL + np.arange(h * 2048, (h + 1) * 2048)])
    else:
        m["xtok"] = _f32(xs[b][lat])
        cols = CTXL + np.arange(h * 2048, (h + 1) * 2048)
    if yfull is not None:
        m["yT"] = np.ascontiguousarray(yfull[:, cols])
    m["cvT"] = _f32(np.stack([inp["c"][b], inp["c_ctx"]], axis=1))
    m["wmodB"] = _f32(inp["w_mod"][l])
    m["bmodT"] = _f32(inp["b_mod"][l].reshape(48, 128).T)
    m["ident_d"] = np.eye(128, dtype=np.float32)
    m["w_g_h"] = _f32(inp["w_in"][l][:, OFF_G:])
    wbr = {"a": inp["w_br_a"][l], "b": inp["w_br_b"][l], "c": inp["w_br_c"][l], "d": inp["w_br_d"][l]}
    bounds = {"a": (0, 192), "b": (192, 320), "c": (320, 512), "d": (512, 640)}
    width = {"a": 192, "b": 128, "c": 192, "d": 128}
    pieces = []
    for br in "abcd":
        lo, hi = bounds[br]
        for hh in range(2):
            for ch in range(5):
                r0, r1 = max(lo, ch * 128), min(hi, (ch + 1) * 128)
                if r0 >= r1:
                    continue
                pc = np.zeros((128, D), np.float32)
                pc[r0 - ch * 128:r1 - ch * 128] = wbr[br][hh * width[br] + (r0 - lo):hh * width[br] + (r1 - lo)]
                pieces.append(pc)
    m["w_br_h"] = _f32(np.stack(pieces, axis=0))
    m["w_out_h"] = _f32(inp["w_out"][l])
    m["s5_wglu_h"] = _f32(inp["s5_w_glu"][l])
    m["s5_bglu_h"] = _f32(inp["s5_b_glu"][l].reshape(2, 128).T)
    m["lnp_h"] = _f32(np.stack([np.tile(inp[k][l][None, :], (128, 1)) for k in ("ln1_g", "ln1_b", "ln2_g", "ln2_b")], axis=0))
    if l % 2 == 0:
        m["ff_w_gate"] = _f32(inp["ff_w_gate"][l // 2])
        m["ff_w_up"] = _f32(inp["ff_w_up"][l // 2])
        m["ff_w_down"] = _f32(inp["ff_w_down"][l // 2])
    else:
        m["moe_router_h"] = _f32(inp["moe_router"][l // 2])
        m["moe_w_gate"] = _f32(inp["moe_w_gate"][l // 2])
        m["moe_w_up"] = _f32(inp["moe_w_up"][l // 2])
        m["moe_w_down"] = _f32(inp["moe_w_down"][l // 2])
    return m


def assemble_y(yb0, yb1):
    return np.concatenate([yb0, yb1], axis=0)


PAIRS = [[0, 1], [2, 3], [4, 5], [6, 7]]


class Fused:
    def __init__(self):
        kb = self.kb = KB()
        S = kb.S
        kb.pfx = ""
        hsel_d = kb.din("hsel_h", [128, 2])
        out = kb.dout("out", [2048, D])
        hsel = kb.sb("hsel", [128, 2])
        kb.dma(hsel[:], hsel_d[:, :], w=["hsel"])
        ybuf = [kb.dscratch("ybuf%d" % l, [640, T], BF16) for l in range(2)]
        ygath = [kb.dscratch("ygath%d" % l, [1280, T], BF16) for l in range(2)]
        xo0 = kb.dscratch("xo0", [2176, D])
        xg = kb.dscratch("xg", [4352, D])

        def allgather(src, dst, wres):
            S.barrier()
            S.coll(kb.es, lambda e: e.collective_compute("AllGather", ALU.bypass, replica_groups=PAIRS, ins=[src], outs=[dst]),
                   reads=[], writes=wres)

        def make_yload(l, with_ctx):
            def yload(pb, i, yt, ytn):
                if with_ctx:
                    c0, c1 = (0, 128) if i == 0 else (CTXL + (i - 1) * 128, CTXL + 2048 + (i - 1) * 128)
                else:
                    c0, c1 = CTXL + i * 128, CTXL + 2048 + i * 128
                (ya, yan), (yb, ybn) = pb.ybl.next(), pb.ybl.next()
                kb.dma(ya[:], ygath[l][:, c0:c0 + 128].rearrange("(c p) t -> p c t", p=128), r=["ygath%d" % l], w=[yan])
                kb.dma(yb[:], ygath[l][:, c1:c1 + 128].rearrange("(c p) t -> p c t", p=128), r=["ygath%d" % l], w=[ybn])
                kb.ts("pool", yt[:], ya[:], hsel[:, 0:1], ALU.mult, r=[yan, "hsel"], w=[ytn])
                kb.stt(yt[:], yb[:], hsel[:, 1:2], yt[:], ALU.mult, ALU.add, r=[ybn, "hsel", ytn], w=[ytn])
            return yload

        kb.pfx = "L0_"
        with kb.scope():
            PhaseA(True, kb=kb, ybuf=ybuf[0])
        allgather(ybuf[0][:, :], ygath[0][:, :], ["ygath0"])
        with kb.scope():
            PhaseB(0, kb=kb, yload=make_yload(0, True), xout=xo0)
        allgather(xo0[:, :], xg[:, :], ["xsrc"])
        kb.pfx = "L1_"

        def xin1(t):
            if t < 2:
                r0 = t * 2176
            else:
                n = t - 2
                r0 = 128 + n * 128 if n < 16 else 2176 + 128 + (n - 16) * 128
            return xg[r0:r0 + 128, :]

        with kb.scope():
            PhaseA(False, kb=kb, xin_fn=xin1, ybuf=ybuf[1])
        allgather(ybuf[1][:, :], ygath[1][:, :], ["ygath1"])
        with kb.scope():
            PhaseB(1, kb=kb, xtok=xo0[128:2176, :], yload=make_yload(1, False), xout=out)
        self.nc = kb.finish()


_PROGS = {}


def _prog(kind, layer):
    key = (kind, layer)
    if key not in _PROGS:
        _PROGS[key] = PhaseA(with_ctx=(layer == 0)) if kind == "A" else PhaseB(layer)
    return _PROGS[key]


def kernel_unfused(**inputs):
    inp = {k: np.asarray(v) for k, v in inputs.items()}
    xs = [_f32(inp["x"][b]) for b in range(NB)]
    ctxs = [_f32(inp["ctx"][b]) for b in range(NB)]
    cores = [(b, h) for b in range(NB) for h in range(2)]
    for l in range(2):
        pa = _prog("A", l)
        maps = []
        for (b, h) in cores:
            m = prep_A(inp, l, b, h, xs, ctxs)
            maps.append({k: m[k] for k in pa.kb.in_names})
        res = run_bass_kernel_spmd(pa.nc, maps, core_ids=list(range(8))).results
        del maps
        yfull = [assemble_y(np.asarray(res[2 * b]["ybuf"]), np.asarray(res[2 * b + 1]["ybuf"])) for b in range(NB)]
        pb = _prog("B", l)
        maps = []
        for (b, h) in cores:
            m = prep_B(inp, l, b, h, xs, ctxs, yfull[b])
            maps.append({k: m[k] for k in pb.kb.in_names})
        res = run_bass_kernel_spmd(pb.nc, maps, core_ids=list(range(8))).results
        del maps
        if l == 0:
            ctxs = [np.concatenate([np.asarray(res[2 * b + h]["xout"])[0:128] for h in range(2)], axis=0) for b in range(NB)]
            xs = [np.concatenate([np.asarray(res[2 * b + h]["xout"])[128:] for h in range(2)], axis=0) for b in range(NB)]
        else:
            xs = [np.concatenate([np.asarray(res[2 * b + h]["xout"]) for h in range(2)], axis=0) for b in range(NB)]
    return np.stack(xs, axis=0).astype(np.float32)


class Fused2:
    def __init__(self):
        kb = self.kb = KB()
        S = kb.S
        kb.pfx = ""
        hsel_d = kb.din("hsel_h", [128, 2])
        out = kb.dout("out", [2048, D])
        hsel = kb.sb("hsel", [128, 2])
        kb.dma(hsel[:], hsel_d[:, :], w=["hsel"])
        yfull = [kb.dscratch("yfull%d" % l, [1280, T], BF16) for l in range(2)]
        xfull = kb.dscratch("xfull", [T, D])
        kb.pfx = "L0_"
        xin0 = kb.din("xin", [T, D])

        def urow(hh, i, with_ctx):
            if with_ctx:
                return hh * 128 if i == 0 else CTXL + hh * 2048 + (i - 1) * 128
            return CTXL + hh * 2048 + i * 128

        def static_yload(l, hh, with_ctx):
            def yload(pb, i, yt, ytn):
                c0 = urow(hh, i, with_ctx)
                kb.dma(yt[:], yfull[l][:, c0:c0 + 128].rearrange("(c p) t -> p c t", p=128), r=["yfull%d" % l], w=[ytn])
            return yload

        def layer_mod(l):
            kb.pfx = "L%d_" % l
            modT = kb.sb("modT_L%d" % l, [128, 48, 2])
            cvT = kb.din("cvT", [D, 2])
            wmod = kb.din("wmodB", [D, 6144])
            bmodT = kb.din("bmodT", [128, 48])
            with kb.scope():
                pm = kb.ps("PSmod")
                wmb = Rot([(kb.sb("wmb%d" % i, [128, 8, 256]), "wmb%d" % i) for i in range(2)])
                emit_modT(kb, wmod, bmodT, cvT, 48, modT, pm, "PSmod", wmb)
                for c0 in (8, 32):
                    kb.ts("dve", modT[:, c0:c0 + 8, :], modT[:, c0:c0 + 8, :], 1.0, ALU.add, r=["modT"], w=["modT"])
            return modT

        modT0 = layer_mod(0)
        with kb.scope():
            uT = kb.sb("uT_sh", [128, 8, T], BF16)
            for h in range(2):
                kb.pfx = "L0h%d_" % h
                with kb.scope():
                    PhaseA(True, kb=kb, xin_fn=lambda t: xin0[t * 128:(t + 1) * 128, :], ybuf=yfull[0][h * 640:(h + 1) * 640, :],
                           modT=modT0, uT=uT, compute_uT=(h == 0))
        S.barrier()
        _stop = int(_os.environ.get("FZ_STOP", "9"))
        kb.pfx = "L0_"
        for hh in range(2 if _stop >= 2 else 0):
            with kb.scope():
                PhaseB(0, kb=kb, yload=static_yload(0, hh, True), modT=modT0,
                       xtok_fn=lambda i, hh=hh: xin0[urow(hh, i, True):urow(hh, i, True) + 128, :],
                       xout_fn=lambda i, hh=hh: xfull[urow(hh, i, True):urow(hh, i, True) + 128, :])
        S.barrier()
        modT1 = layer_mod(1)
        with kb.scope():
            uT = kb.sb("uT_sh", [128, 8, T], BF16)
            for h in range(2 if _stop >= 3 else 0):
                kb.pfx = "L1h%d_" % h
                with kb.scope():
                    PhaseA(False, kb=kb, xin_fn=lambda t: xfull[t * 128:(t + 1) * 128, :], ybuf=yfull[1][h * 640:(h + 1) * 640, :],
                           modT=modT1, uT=uT, compute_uT=(h == 0))
        S.barrier()
        kb.pfx = "L1_"

        def yload1(pb, i, yt, ytn):
            (ya, yan), (yb, ybn) = pb.ybl.next(), pb.ybl.next()
            c0, c1 = urow(0, i, False), urow(1, i, False)
            kb.dma(ya[:], yfull[1][:, c0:c0 + 128].rearrange("(c p) t -> p c t", p=128), w=[yan])
            kb.dma(yb[:], yfull[1][:, c1:c1 + 128].rearrange("(c p) t -> p c t", p=128), w=[ybn])
            kb.ts("pool", yt[:], ya[:], hsel[:, 0:1], ALU.mult, r=[yan, "hsel"], w=[ytn])
            kb.stt(yt[:], yb[:], hsel[:, 1:2], yt[:], ALU.mult, ALU.add, r=[ybn, "hsel", ytn], w=[ytn])

        def xload1(pb, i, lnt):
            (xa, xan), (xb, xbn) = lnt.xt.next(), lnt.xt.next()
            r0, r1 = urow(0, i, False), urow(1, i, False)
            kb.dma(xa[:], xfull[r0:r0 + 128, :], w=[xan])
            kb.dma(xb[:], xfull[r1:r1 + 128, :], w=[xbn])
            kb.ts("pool", xa[:], xa[:], hsel[:, 0:1], ALU.mult, r=[xan, "hsel"], w=[xan])
            kb.stt(xa[:], xb[:], hsel[:, 1:2], xa[:], ALU.mult, ALU.add, r=[xbn, "hsel", xan], w=[xan])
            return xa, [xan]

        if _stop >= 4:
            with kb.scope():
                PhaseB(1, kb=kb, yload=yload1, xload=xload1, xout=out, modT=modT1)
        self.nc = kb.finish()


_FUSED = []


def kernel(**inputs):
    inp = {k: np.asarray(v) for k, v in inputs.items()}
    xs = [_f32(inp["x"][b]) for b in range(NB)]
    ctxs = [_f32(inp["ctx"][b]) for b in range(NB)]
    if not _FUSED:
        _FUSED.append(Fused2())
    fz = _FUSED[0]
    names = set(fz.kb.in_names)
    maps = []
    for b in range(NB):
        shared = {}
        for l in range(2):
            for hp in range(2):
                for k, v in prep_A(inp, l, b, hp, xs, ctxs).items():
                    kk = "L%dh%d_%s" % (l, hp, k)
                    if kk in names:
                        shared[kk] = v
                    kk = "L%d_%s" % (l, k)
                    if kk in names and kk not in shared:
                        shared[kk] = v
            for k, v in prep_B(inp, l, b, 0, xs, ctxs, None).items():
                kk = "L%d_%s" % (l, k)
                if kk in names and kk not in shared:
                    shared[kk] = v
        for h in range(2):
            m = dict(shared)
            m["hsel_h"] = np.tile(np.array([[1.0 - h, float(h)]], np.float32), (128, 1))
            missing = names - set(m)
            assert not missing, missing
            maps.append(m)
    res = run_bass_kernel_spmd(fz.nc, maps, core_ids=list(range(8))).results
    outs = [np.concatenate([np.asarray(res[2 * b + h]["out"]) for h in range(2)], axis=0) for b in range(NB)]
    return np.stack(outs, axis=0).astype(np.float32)
```
